# Optimizing a Trainium2 kernel written in Bass

```python
import jax, jax.numpy as jnp
from jax import lax
import numpy as np

D_MODEL = 1024
BATCH = 4
SEQ = 8192
DEPTH = 4

N_BRANCH = 4
BRANCH_WIDTH = 512
ML_HEADS = 4
ML_DK = 128
ML_DV = 128
FX_HEADS = 4
FX_DH = 128
LRU_WIDTH = 512
LRU_BLOCKS = 8
LRU_BLOCK = 64
CONV_WIDTH = 4
LRU_C = 8.0
GLA_HEADS = 4
GLA_DK = 64
GLA_DV = 128
GLA_RANK = 16
GLA_TAU = 16.0
D_FF = 4 * D_MODEL
CHUNK = 64
Q_BLOCK = 128
EPS = 1e-6

IN_WIDTHS = (
    ML_HEADS * ML_DK, ML_HEADS * ML_DK, ML_HEADS * ML_DV, ML_HEADS * ML_DV, ML_HEADS, ML_HEADS,
    FX_HEADS * FX_DH, FX_HEADS * FX_DH, FX_HEADS * FX_DH, FX_HEADS,
    LRU_WIDTH, LRU_WIDTH,
    GLA_HEADS * GLA_DK, GLA_HEADS * GLA_DK, GLA_HEADS * GLA_DV, GLA_HEADS * GLA_DV, GLA_RANK,
    N_BRANCH * D_MODEL,
)
N_IN = sum(IN_WIDTHS)
ML_F_COL = 5
FX_F_COL = 9

kernel_name = "hybrid_mlstm_fox_rglru_gla_block"


def rms_norm(x, g):
    xf = x.astype(jnp.float32)
    y = xf * lax.rsqrt(jnp.mean(xf * xf, axis=-1, keepdims=True) + EPS)
    return (y * g.astype(jnp.float32)).astype(x.dtype)


def split_cols(z):
    offs = []
    acc = 0
    for w in IN_WIDTHS[:-1]:
        acc += w
        offs.append(acc)
    return jnp.split(z, offs, axis=-1)


def mlstm(q, k, v, i_pre, f_pre):
    f32 = jnp.float32
    B, S, H, dk = q.shape
    dv = v.shape[-1]
    nc = S // CHUNK

    def to_chunks(t):
        t = t.astype(f32).reshape((B, nc, CHUNK) + t.shape[2:])
        return jnp.swapaxes(t, 2, 3)

    qc = to_chunks(q) * (dk ** -0.5)
    kc = to_chunks(k)
    vc = to_chunks(v)
    log_i = to_chunks(i_pre)
    log_f = jax.nn.log_sigmoid(to_chunks(f_pre))
    b = jnp.cumsum(log_f, axis=-1)
    b_last = b[..., -1]

    w_end = b_last[..., None] - b + log_i
    m_loc = jnp.max(w_end, axis=-1)
    e_end = jnp.exp(w_end - m_loc[..., None])
    c_loc = jnp.einsum('bnhsk,bnhsv->bnhkv', kc * e_end[..., None], vc)
    n_loc = jnp.einsum('bnhs,bnhsk->bnhk', e_end, kc)

    def step(carry, inp):
        c, n, m = carry
        g, ml, cl, nl = inp
        m_new = jnp.maximum(g + m, ml)
        a = jnp.exp(g + m - m_new)
        bb = jnp.exp(ml - m_new)
        c_new = a[..., None, None] * c + bb[..., None, None] * cl
        n_new = a[..., None] * n + bb[..., None] * nl
        return (c_new, n_new, m_new), (c, n, m)

    init = (jnp.zeros((B, H, dk, dv), f32), jnp.zeros((B, H, dk), f32), jnp.zeros((B, H), f32))
    xs = (jnp.moveaxis(b_last, 1, 0), jnp.moveaxis(m_loc, 1, 0),
          jnp.moveaxis(c_loc, 1, 0), jnp.moveaxis(n_loc, 1, 0))
    _, (c_prev, n_prev, m_prev) = lax.scan(step, init, xs)
    c_prev = jnp.moveaxis(c_prev, 0, 1)
    n_prev = jnp.moveaxis(n_prev, 0, 1)
    m_prev = jnp.moveaxis(m_prev, 0, 1)

    causal = jnp.tril(jnp.ones((CHUNK, CHUNK), dtype=bool))
    d_ts = jnp.where(causal, b[..., :, None] - b[..., None, :] + log_i[..., None, :], -jnp.inf)
    inter_log = b + m_prev[..., None]
    m_t = jnp.maximum(inter_log, jnp.max(d_ts, axis=-1))
    s_qk = jnp.einsum('bnhtk,bnhsk->bnhts', qc, kc) * jnp.exp(d_ts - m_t[..., None])
    w_inter = jnp.exp(inter_log - m_t)
    num = (jnp.einsum('bnhts,bnhsv->bnhtv', s_qk, vc)
           + w_inter[..., None] * jnp.einsum('bnhtk,bnhkv->bnhtv', qc, c_prev))
    den = s_qk.sum(-1) + w_inter * jnp.einsum('bnhtk,bnhk->bnht', qc, n_prev)
    h = num / jnp.maximum(jnp.abs(den), jnp.exp(-m_t))[..., None]
    return jnp.swapaxes(h, 2, 3).reshape(B, S, H, dv)


def forgetting_attention(q, k, v, f_pre, gq, gk):
    f32 = jnp.float32
    B, S, H, Dh = q.shape
    q = rms_norm(q.astype(f32), gq) * (Dh ** -0.5)
    k = rms_norm(k.astype(f32), gk)
    v = v.astype(f32)
    F = jnp.cumsum(jax.nn.log_sigmoid(f_pre.astype(f32)), axis=1)
    Fk = jnp.transpose(F, (0, 2, 1))
    nb = S // Q_BLOCK
    qb = jnp.moveaxis(q.reshape(B, nb, Q_BLOCK, H, Dh), 1, 0)
    Fq = jnp.moveaxis(Fk.reshape(B, H, nb, Q_BLOCK), 2, 0)
    pos_k = jnp.arange(S)

    def block(args):
        qi, Fi, idx = args
        pos_q = idx * Q_BLOCK + jnp.arange(Q_BLOCK)
        s = jnp.einsum('bqhd,bkhd->bhqk', qi, k) + (Fi[..., :, None] - Fk[:, :, None, :])
        s = jnp.where(pos_k[None, :] <= pos_q[:, None], s, -jnp.inf)
        p = jax.nn.softmax(s, axis=-1)
        return jnp.einsum('bhqk,bkhd->bqhd', p, v)

    out = lax.map(block, (qb, Fq, jnp.arange(nb)))
    return jnp.moveaxis(out, 0, 1).reshape(B, S, H, Dh)


def rg_lru_branch(xb, yb, conv_w, conv_b, wa, ba, wx, bx, lam):
    f32 = jnp.float32
    B, S, W = xb.shape
    xf = xb.astype(f32)
    xc = lax.conv_general_dilated(xf, conv_w.astype(f32)[:, None, :], window_strides=(1,),
                                  padding=[(CONV_WIDTH - 1, 0)],
                                  dimension_numbers=('NWC', 'WIO', 'NWC'),
                                  feature_group_count=W) + conv_b.astype(f32)
    xg = xc.reshape(B, S, LRU_BLOCKS, LRU_BLOCK)
    r_gate = jax.nn.sigmoid(jnp.einsum('bsgi,gij->bsgj', xg, wa.astype(f32)).reshape(B, S, W) + ba.astype(f32))
    i_gate = jax.nn.sigmoid(jnp.einsum('bsgi,gij->bsgj', xg, wx.astype(f32)).reshape(B, S, W) + bx.astype(f32))
    log_a = -LRU_C * r_gate * jax.nn.softplus(-lam.astype(f32))
    a = jnp.exp(log_a)
    u = jnp.sqrt(-jnp.expm1(2.0 * log_a)) * (i_gate * xc)

    def combine(left, right):
        a1, b1 = left
        a2, b2 = right
        return a1 * a2, a2 * b1 + b2

    _, h = lax.associative_scan(combine, (a, u), axis=1)
    return h * jax.nn.gelu(yb.astype(f32))


def gla(q, k, v, log_alpha):
    f32 = jnp.float32
    B, S, H, dk = q.shape
    dv = v.shape[-1]
    nc = S // CHUNK

    def to_chunks(t):
        t = t.astype(f32).reshape((B, nc, CHUNK) + t.shape[2:])
        return jnp.swapaxes(t, 2, 3)

    qc = to_chunks(q) * (dk ** -0.5)
    kc = to_chunks(k)
    vc = to_chunks(v)
    bc = jnp.cumsum(to_chunks(log_alpha), axis=3)
    b_last = bc[..., -1, :]
    q_t = qc * jnp.exp(bc)
    k_t = kc * jnp.exp(-bc)
    causal = jnp.tril(jnp.ones((CHUNK, CHUNK), dtype=bool))
    attn = jnp.where(causal, jnp.einsum('bnhtk,bnhsk->bnhts', q_t, k_t), 0.0)
    o_intra = jnp.einsum('bnhts,bnhsv->bnhtv', attn, vc)
    ds = jnp.einsum('bnhsk,bnhsv->bnhkv', kc * jnp.exp(b_last[..., None, :] - bc), vc)

    def step(s, inp):
        decay, d = inp
        return decay[..., None] * s + d, s

    init = jnp.zeros((B, H, dk, dv), f32)
    _, s_prev = lax.scan(step, init, (jnp.moveaxis(jnp.exp(b_last), 1, 0), jnp.moveaxis(ds, 1, 0)))
    s_prev = jnp.moveaxis(s_prev, 0, 1)
    o = o_intra + jnp.einsum('bnhtk,bnhkv->bnhtv', q_t, s_prev)
    return jnp.swapaxes(o, 2, 3).reshape(B, S, H, dv)


def hybrid_layer(x, norm1_g, w_in, b_in, ml_norm_g, fx_qnorm_g, fx_knorm_g,
                 lru_conv_w, lru_conv_b, lru_wa, lru_ba, lru_wx, lru_bx, lru_lambda,
                 gla_w_alpha, gla_b_alpha, gla_norm_g, w_branch, w_o, norm2_g, w_up, w_down):
    f32 = jnp.float32
    B, S, _ = x.shape
    h = rms_norm(x, norm1_g)
    z = h @ w_in + b_in
    (mq, mk, mv, mo, mi, mf, fq, fk, fv, ff, rx, ry,
     gq, gk, gv, gg, ga, gates) = split_cols(z)

    y_ml = mlstm(mq.reshape(B, S, ML_HEADS, ML_DK), mk.reshape(B, S, ML_HEADS, ML_DK),
                 mv.reshape(B, S, ML_HEADS, ML_DV), mi, mf)
    y_ml = rms_norm(y_ml, ml_norm_g.reshape(ML_HEADS, ML_DV)).reshape(B, S, BRANCH_WIDTH)
    y_ml = y_ml * jax.nn.sigmoid(mo.astype(f32))

    y_fx = forgetting_attention(fq.reshape(B, S, FX_HEADS, FX_DH), fk.reshape(B, S, FX_HEADS, FX_DH),
                                fv.reshape(B, S, FX_HEADS, FX_DH), ff, fx_qnorm_g, fx_knorm_g)
    y_fx = y_fx.reshape(B, S, BRANCH_WIDTH)

    y_lru = rg_lru_branch(rx, ry, lru_conv_w, lru_conv_b, lru_wa, lru_ba, lru_wx, lru_bx, lru_lambda)

    log_alpha = jax.nn.log_sigmoid(ga.astype(f32) @ gla_w_alpha.astype(f32) + gla_b_alpha.astype(f32)) / GLA_TAU
    y_gla = gla(gq.reshape(B, S, GLA_HEADS, GLA_DK), gk.reshape(B, S, GLA_HEADS, GLA_DK),
                gv.reshape(B, S, GLA_HEADS, GLA_DV), log_alpha.reshape(B, S, GLA_HEADS, GLA_DK))
    y_gla = rms_norm(y_gla, gla_norm_g.reshape(GLA_HEADS, GLA_DV)).reshape(B, S, BRANCH_WIDTH)
    y_gla = y_gla * jax.nn.silu(gg.astype(f32))

    ycat = jnp.stack([y_ml, y_fx, y_lru, y_gla], axis=2).astype(x.dtype)
    yb = jnp.einsum('bsnw,nwd->bsnd', ycat, w_branch)
    g = jax.nn.sigmoid(gates.reshape(B, S, N_BRANCH, D_MODEL))
    merged = jnp.sum(g * yb, axis=2)
    x = x + merged @ w_o

    h2 = rms_norm(x, norm2_g)
    x = x + jnp.square(jax.nn.relu(h2 @ w_up)) @ w_down
    return x


def setup_inputs(seed: int = 0) -> dict:
    key = jax.random.key(seed)
    ks = jax.random.split(key, 24)
    f32 = jnp.float32

    def nrm(k, shape, scale):
        return jax.random.normal(k, shape, f32) * scale

    x = nrm(ks[0], (BATCH, SEQ, D_MODEL), 1.0)
    norm1_g = 1.0 + nrm(ks[1], (DEPTH, D_MODEL), 0.02)
    w_in = nrm(ks[2], (DEPTH, D_MODEL, N_IN), D_MODEL ** -0.5)
    b_in = nrm(ks[3], (DEPTH, N_IN), 0.02)
    ml_f_off = sum(IN_WIDTHS[:ML_F_COL])
    fx_f_off = sum(IN_WIDTHS[:FX_F_COL])
    b_in = b_in.at[:, ml_f_off:ml_f_off + ML_HEADS].add(jnp.linspace(3.0, 6.0, ML_HEADS, dtype=f32))
    b_in = b_in.at[:, fx_f_off:fx_f_off + FX_HEADS].add(jnp.linspace(3.0, 6.0, FX_HEADS, dtype=f32))
    ml_norm_g = 1.0 + nrm(ks[4], (DEPTH, ML_HEADS * ML_DV), 0.02)
    fx_qnorm_g = 1.0 + nrm(ks[5], (DEPTH, FX_DH), 0.02)
    fx_knorm_g = 1.0 + nrm(ks[6], (DEPTH, FX_DH), 0.02)
    lru_conv_w = nrm(ks[7], (DEPTH, CONV_WIDTH, LRU_WIDTH), CONV_WIDTH ** -0.5)
    lru_conv_b = nrm(ks[8], (DEPTH, LRU_WIDTH), 0.02)
    lru_wa = nrm(ks[9], (DEPTH, LRU_BLOCKS, LRU_BLOCK, LRU_BLOCK), LRU_BLOCK ** -0.5)
    lru_ba = nrm(ks[10], (DEPTH, LRU_WIDTH), 0.02)
    lru_wx = nrm(ks[11], (DEPTH, LRU_BLOCKS, LRU_BLOCK, LRU_BLOCK), LRU_BLOCK ** -0.5)
    lru_bx = nrm(ks[12], (DEPTH, LRU_WIDTH), 0.02)
    u = jax.random.uniform(ks[13], (DEPTH, LRU_WIDTH), f32, 0.9, 0.999)
    a0 = u ** (1.0 / LRU_C)
    lru_lambda = jnp.log(a0) - jnp.log1p(-a0)
    gla_w_alpha = nrm(ks[14], (DEPTH, GLA_RANK, GLA_HEADS * GLA_DK), GLA_RANK ** -0.5)
    gla_b_alpha = nrm(ks[15], (DEPTH, GLA_HEADS * GLA_DK), 0.02)
    gla_norm_g = 1.0 + nrm(ks[16], (DEPTH, GLA_HEADS * GLA_DV), 0.02)
    w_branch = nrm(ks[17], (DEPTH, N_BRANCH, BRANCH_WIDTH, D_MODEL), BRANCH_WIDTH ** -0.5)
    w_o = nrm(ks[18], (DEPTH, D_MODEL, D_MODEL), D_MODEL ** -0.5)
    norm2_g = 1.0 + nrm(ks[19], (DEPTH, D_MODEL), 0.02)
    w_up = nrm(ks[20], (DEPTH, D_MODEL, D_FF), D_MODEL ** -0.5)
    w_down = nrm(ks[21], (DEPTH, D_FF, D_MODEL), D_FF ** -0.5)
    return {"x": x, "norm1_g": norm1_g, "w_in": w_in, "b_in": b_in, "ml_norm_g": ml_norm_g,
            "fx_qnorm_g": fx_qnorm_g, "fx_knorm_g": fx_knorm_g, "lru_conv_w": lru_conv_w,
            "lru_conv_b": lru_conv_b, "lru_wa": lru_wa, "lru_ba": lru_ba, "lru_wx": lru_wx,
            "lru_bx": lru_bx, "lru_lambda": lru_lambda, "gla_w_alpha": gla_w_alpha,
            "gla_b_alpha": gla_b_alpha, "gla_norm_g": gla_norm_g, "w_branch": w_branch, "w_o": w_o,
            "norm2_g": norm2_g, "w_up": w_up, "w_down": w_down}


def reference(x, norm1_g, w_in, b_in, ml_norm_g, fx_qnorm_g, fx_knorm_g, lru_conv_w, lru_conv_b,
              lru_wa, lru_ba, lru_wx, lru_bx, lru_lambda, gla_w_alpha, gla_b_alpha, gla_norm_g,
              w_branch, w_o, norm2_g, w_up, w_down):
    for l in range(DEPTH):
        x = hybrid_layer(x, norm1_g[l], w_in[l], b_in[l], ml_norm_g[l], fx_qnorm_g[l], fx_knorm_g[l],
                         lru_conv_w[l], lru_conv_b[l], lru_wa[l], lru_ba[l], lru_wx[l], lru_bx[l],
                         lru_lambda[l], gla_w_alpha[l], gla_b_alpha[l], gla_norm_g[l],
                         w_branch[l], w_o[l], norm2_g[l], w_up[l], w_down[l])
    return x
```

```python
import os
import numpy as np
import concourse.bass as bass
import concourse.mybir as mybir
from concourse.bass_utils import run_bass_kernel_spmd
from contextlib import ExitStack

F32 = mybir.dt.float32
BF16 = mybir.dt.bfloat16
AF = mybir.ActivationFunctionType
ALU = mybir.AluOpType

D = 1024
NIN = 10268
DFF = 4096
EPS = 1e-6
C_MQ, C_MK, C_MV, C_MO, C_MI, C_MF = 0, 512, 1024, 1536, 2048, 2052
C_FQ, C_FK, C_FV, C_FF = 2056, 2568, 3080, 3592
C_RX, C_RY = 3596, 4108
C_GQ, C_GK, C_GV, C_GG, C_GA = 4620, 4876, 5132, 5644, 6156
C_GT = 6172


class DSem:
    def __init__(self, h):
        self.h = h
        self.count = 0


class Sync:
    ENG = ('pe', 'act', 'dve', 'pool', 'sp')

    def __init__(self, nc, es):
        self.nc = nc
        self.es = es
        self.esem = {k: es.enter_context(nc.semaphore('s_' + k)) for k in self.ENG}
        self.ecnt = {k: 0 for k in self.ENG}
        self.seen = {k: {} for k in self.ENG}
        self.prog = {k: [] for k in self.ENG}
        self.lastw = {}
        self.readers = {}
        self.nwaits = 0
        self.ndsem = 0
        self.dsems = []

    def dsem(self, name=None, nobar=False):
        self.ndsem += 1
        d = DSem(self.es.enter_context(self.nc.semaphore(name or ('d%d' % self.ndsem))))
        if not nobar:
            self.dsems.append(d)
        return d

    def barrier(self):
        for e in self.ENG:
            for o in self.ENG:
                if o != e and self.ecnt[o] > 0:
                    self._wait(e, (self.esem[o], self.ecnt[o]))
            for d in self.dsems:
                if d.count > 0:
                    self._wait(e, (d.h, d.count))

    def _wait(self, eng, tok):
        sem, val = tok
        if eng == 'pe' and sem is self.esem['pe']:
            return
        s = self.seen[eng]
        if s.get(id(sem), 0) >= val:
            return
        self.prog[eng].append(('w', sem, val))
        self.nwaits += 1
        s[id(sem)] = val

    def _deps(self, eng, R, W):
        for k in R:
            t = self.lastw.get(k)
            if t:
                self._wait(eng, t)
        for k in W:
            t = self.lastw.get(k)
            if t:
                self._wait(eng, t)
            for t in self.readers.get(k, {}).values():
                self._wait(eng, t)

    def _record(self, tok, R, W):
        for k in W:
            self.lastw[k] = tok
            self.readers[k] = {}
        for k in R:
            self.readers.setdefault(k, {})[id(tok[0])] = tok

    def op(self, eng, meth, R, W, *args, **kw):
        self._deps(eng, R, W)
        self.ecnt[eng] += 1
        self.prog[eng].append(('i', meth, args, kw, self.esem[eng], 1))
        self._record((self.esem[eng], self.ecnt[eng]), R, W)

    def dma(self, q, ds, out, in_, R=(), W=(), **kw):
        self._deps(q, R, W)
        ds.count += 16
        self.prog[q].append(('i', 'dma_start', (), dict(out=out, in_=in_, **kw), ds.h, 16))
        self._record((ds.h, ds.count), R, W)

    def wait_all(self, eng, keys):
        for k in keys:
            t = self.lastw.get(k)
            if t:
                self._wait(eng, t)

    def emit(self):
        nc = self.nc

        def replay(name, e):
            for it in self.prog[name]:
                if it[0] == 'w':
                    e.wait_ge(it[1], it[2])
                else:
                    _, meth, args, kw, sem, inc = it
                    getattr(e, meth)(*args, **kw).then_inc(sem, inc)

        with nc.Block() as block:
            @block.sync
            def _(e):
                replay('sp', e)

            @block.tensor
            def _(e):
                replay('pe', e)

            @block.scalar
            def _(e):
                replay('act', e)

            @block.vector
            def _(e):
                replay('dve', e)

            @block.gpsimd
            def _(e):
                replay('pool', e)


PNAMES = ["norm1_g", "w_in", "b_in", "ml_norm_g", "fx_qnorm_g", "fx_knorm_g", "lru_conv_w", "lru_conv_b",
          "lru_wa", "lru_ba", "lru_wx", "lru_bx", "lru_lambda", "gla_w_alpha", "gla_b_alpha", "gla_norm_g",
          "w_branch", "w_o", "norm2_g", "w_up", "w_down"]


def build(S_, NL, dbg=False):
    NT = S_ // 512
    NB = S_ // 128
    nc = bass.Bass("TRN2", target_bir_lowering=False)
    es = ExitStack()
    es.enter_context(nc.allow_non_contiguous_dma(reason="small param loads"))
    S = Sync(nc, es)

    def dram(name, shape, dt, kind="Internal"):
        return nc.dram_tensor(name, shape, dt, kind=kind).ap()

    x_in = dram("x", [S_, D], F32, "ExternalInput")
    cst = dram("cst", [128, 384], F32, "ExternalInput")
    shp = dict(norm1_g=[NL, D], w_in=[NL, D, NIN], b_in=[NL, NIN], ml_norm_g=[NL, 512], fx_qnorm_g=[NL, 128],
               fx_knorm_g=[NL, 128], lru_conv_w=[NL, 4, 512], lru_conv_b=[NL, 512], lru_wa=[NL, 8, 64, 64],
               lru_ba=[NL, 512], lru_wx=[NL, 8, 64, 64], lru_bx=[NL, 512], lru_lambda=[NL, 512],
               gla_w_alpha=[NL, 16, 256], gla_b_alpha=[NL, 256], gla_norm_g=[NL, 512],
               w_branch=[NL, 4, 512, D], w_o=[NL, D, D], norm2_g=[NL, D], w_up=[NL, D, DFF], w_down=[NL, DFF, D])
    P = {k: dram(k, shp[k], F32, "ExternalInput") for k in PNAMES}
    out = dram("out", [S_, D], F32, "ExternalOutput")
    dbg_y = dram("dbg_y", [16, 128, S_], BF16, "ExternalOutput") if dbg else None

    wb_in = dram("wb_in", [NL, D, NIN], BF16)
    wb_br = dram("wb_br", [NL, 2048, D], BF16)
    wb_o = dram("wb_o", [NL, D, D], BF16)
    wb_up = dram("wb_up", [NL, D, DFF], BF16)
    wb_dn = dram("wb_dn", [NL, DFF, D], BF16)
    xs = [dram("xs0", [S_, D], F32), dram("xs1", [S_, D], F32)]
    yT = dbg_y if dbg else dram("yT", [16, 128, S_], BF16)
    QTd = dram("QTd", [4, 128, S_], BF16)
    KTd = dram("KTd", [4, 128, S_], BF16)
    Vd = dram("Vd", [4, 128, NB, 130], BF16)

    def sb(name, shape, dt):
        return es.enter_context(nc.sbuf_tensor(name, shape, dt))

    def ps(name, shape, dt):
        return es.enter_context(nc.psum_tensor(name, shape, dt))

    def cast_w(src, dst, key):
        ds = S.dsem(nobar=True)
        n = 1
        for s_ in src.shape:
            n *= s_
        rows = n // 2048
        pat = " ".join("abcd"[:len(src.shape)])
        sv = src.rearrange(f"{pat} -> ({pat})").rearrange("(r c) -> r c", c=2048)
        pat = " ".join("abcd"[:len(dst.shape)])
        dv = dst.rearrange(f"{pat} -> ({pat})").rearrange("(r c) -> r c", c=2048)
        r0 = 0
        while r0 < rows:
            r1 = min(rows, r0 + 2048)
            S.dma('pool', ds, dv[r0:r1, :], sv[r0:r1, :], W=[key])
            r0 = r1

    for l in range(NL):
        cast_w(P["w_in"][l], wb_in[l], ('wb_in', l))
        cast_w(P["w_branch"][l], wb_br[l], ('wb_br', l))
        cast_w(P["w_o"][l], wb_o[l], ('wb_o', l))
        cast_w(P["w_up"][l], wb_up[l], ('wb_up', l))
        cast_w(P["w_down"][l], wb_dn[l], ('wb_dn', l))

    cst_f = sb("cst_f", [128, 384], F32)
    d_c = S.dsem()
    S.dma('sp', d_c, cst_f[:], cst, W=['cst_f'])
    ident_f = cst_f[:, 0:128]
    tri_f = cst_f[:, 128:256]
    ones_f = cst_f[:, 256:384]
    ident_b = sb("ident_b", [128, 128], BF16)
    mask4 = sb("mask4", [128, 4, 128], BF16)
    S.op('dve', 'tensor_copy', ['cst_f'], ['ident_b'], out=ident_b[:], in_=ident_f)
    for h in range(4):
        S.op('dve', 'tensor_copy', ['cst_f'], ['mask4'], out=mask4[:, h, :], in_=tri_f)

    xt = sb("xt", [128, 4, D], F32)
    junk = sb("junk", [128, D], F32)
    hT = sb("hT", [128, 8, 512], BF16)
    wsl = [sb("wsl0", [128, 8, 512], BF16), sb("wsl1", [128, 8, 512], BF16)]
    d_wsl = [S.dsem(), S.dsem()]
    d_x = S.dsem()
    st1 = sb("st1", [128, 16], F32)
    bfe = sb("bfe", [128, 64], F32)
    g1 = sb("g1", [128, 8], F32)
    g2 = sb("g2", [128, 8], F32)
    sm = sb("sm", [128, 160], F32)
    yTs = sb("yTs", [128, 4, 512], BF16)
    ytok = sb("ytok", [128, 512], BF16)
    Fall = sb("Fall", [128, NB, 4], F32)
    rI = sb("rI", [128, NT, 4], F32)
    d_p = S.dsem()
    d_y = S.dsem()
    bb = sb("bb", [128, 4108], F32)
    mif = sb("mif", [128, 4, 8], F32)
    Cst = sb("Cst", [128, 4, 130], F32)
    Cbf = sb("Cbf", [128, 4, 130], BF16)
    gbc = sb("gbc", [128, 512], F32)
    gqk = sb("gqk", [128, 2, 128], F32)
    Fcar = sb("Fcar", [128, 4], F32)
    rxy = sb("rxy", [128, 4, 515], F32)
    lcar = sb("lcar", [128, 4], F32)
    lpar = sb("lpar", [128, 64], F32)
    wbd = sb("wbd", [128, 2, 4, 128], BF16)
    wal_f = sb("wal_f", [16, 256], F32)
    wal = sb("wal", [16, 256], BF16)
    gpar = sb("gpar", [128, 8], F32)
    Sst = sb("Sst", [128, 2, 128], F32)
    Sbf = sb("Sbf", [128, 2, 128], BF16)
    d_f = S.dsem()
    d_f2 = S.dsem()
    bigB = sb("bigB", [128, 20480], BF16)
    bigF = sb("bigF", [128, 8192], F32)

    def vB(off, shape):
        n = 1
        for s_ in shape[1:]:
            n *= s_
        v = bigB[:, off:off + n]
        if len(shape) == 3:
            v = v.rearrange("p (a b) -> p a b", a=shape[1])
        elif len(shape) == 4:
            v = v.rearrange("p (a b c) -> p a b c", a=shape[1], b=shape[2])
        return v

    def vF(off, shape):
        n = 1
        for s_ in shape[1:]:
            n *= s_
        v = bigF[:, off:off + n]
        if len(shape) == 3:
            v = v.rearrange("p (a b) -> p a b", a=shape[1])
        return v

    PJ = [ps("PJ0", [128, 512], F32), ps("PJ1", [128, 512], F32)]
    PT = ps("PT", [128, 512], F32)
    PB = ps("PB", [128, 1024], BF16)
    PA = ps("PA", [128, 512], F32)
    PO = [ps("PO0", [128, 512], F32), ps("PO1", [128, 512], F32)]
    PS = ps("PS", [128, 512], F32)

    state = dict(wslot=0, pj=0)

    def load_w(src_ap, ncols, rkey, kch=8):
        i = state['wslot']
        state['wslot'] ^= 1
        S.dma('sp', d_wsl[i], wsl[i][:, 0:kch, 0:ncols], src_ap.rearrange("(c p) n -> p c n", p=128),
              R=[rkey], W=[('wsl', i)])
        return i

    def next_pj():
        i = state['pj']
        state['pj'] ^= 1
        return i

    def norm_from_xt(gt, gkey):
        for j in range(4):
            S.op('act', 'activation', ['xt'], ['junk', ('st1', j)], out=junk[:], in_=xt[:, j, :], func=AF.Square,
                 accum_out=st1[:, j:j + 1])
        S.op('act', 'activation', [('st1', j) for j in range(4)], ['st1b'], out=st1[:, 4:8], in_=st1[:, 0:4],
             func=AF.Sqrt, scale=1.0 / D, bias=EPS)
        S.op('dve', 'reciprocal', ['st1b'], ['st1c'], out=st1[:, 8:12], in_=st1[:, 4:8])
        for j in range(4):
            S.op('dve', 'tensor_scalar', ['xt', 'st1c'], ['junk'], out=junk[:], in0=xt[:, j, :],
                 scalar1=st1[:, 8 + j:9 + j], scalar2=None, op0=ALU.mult)
            for half in range(2):
                for q in range(4):
                    c = half * 4 + q
                    S.op('pe', 'transpose', ['junk', 'cst_f'], ['PT'], out=PT[:, q * 128:(q + 1) * 128],
                         in_=junk[:, c * 128:(c + 1) * 128], identity=ident_f)
                for q in range(4):
                    c = half * 4 + q
                    S.op('dve', 'tensor_scalar', [gkey], ['PT', ('hT', c)], out=hT[:, c, j * 128:(j + 1) * 128],
                         in0=PT[:, q * 128:(q + 1) * 128], scalar1=gt[:, c:c + 1], scalar2=None, op0=ALU.mult)

    def load_x(src_dram, t):
        S.dma('sp', d_x, xt[:], src_dram[t * 512:(t + 1) * 512, :].rearrange("(j p) d -> p j d", p=128),
              R=[('xs', id(src_dram))], W=['xt'])

    def proj_feat(wsrc, wkey, c0, ncols, evac):
        done = 0
        while done < ncols:
            n = min(512, ncols - done)
            wi = load_w(wsrc[:, c0 + done:c0 + done + n], n, wkey)
            mc = 0
            while mc * 128 < n:
                m = min(128, n - mc * 128)
                pj = next_pj()
                for kc in range(8):
                    S.op('pe', 'matmul', [('wsl', wi), ('hT', kc)], [('PJ', pj)], PJ[pj][0:m, :],
                         lhsT=wsl[wi][:, kc, mc * 128:mc * 128 + m], rhs=hT[:, kc, :], start=(kc == 0), stop=(kc == 7))
                evac((done + mc * 128) // 128, m, PJ[pj], ('PJ', pj))
                mc += 1
            done += n

    def proj_tok(wsrc, wkey, c0, ncols, evac):
        done = 0
        while done < ncols:
            n = min(512, ncols - done)
            wi = load_w(wsrc[:, c0 + done:c0 + done + n], n, wkey)
            for j in range(4):
                pj = next_pj()
                for kc in range(8):
                    S.op('pe', 'matmul', [('wsl', wi), ('hT', kc)], [('PJ', pj)], PJ[pj][:, 0:n],
                         lhsT=hT[:, kc, j * 128:(j + 1) * 128], rhs=wsl[wi][:, kc, 0:n], start=(kc == 0), stop=(kc == 7))
                evac(j, done, n, PJ[pj], ('PJ', pj))
            done += n

    def yT_store(br, t):
        S.dma('pool', d_y, yT[br * 4:(br + 1) * 4, :, t * 512:(t + 1) * 512].rearrange("c p s -> p c s"),
              yTs[:], R=['yTs'], W=[('yT', br)])

    def tok_to_yTs(j):
        for c in range(4):
            S.op('pe', 'transpose', ['ytok', 'ident_b'], ['PB'], out=PB[:, c * 128:(c + 1) * 128],
                 in_=ytok[:, c * 128:(c + 1) * 128], identity=ident_b[:])
        S.op('act', 'activation', [], ['PB', 'yTs'], out=yTs[:, :, j * 128:(j + 1) * 128],
             in_=PB[:, 0:512].rearrange("p (c s) -> p c s", c=4), func=AF.Copy)

    for l in range(NL):
        x_src = x_in if l == 0 else xs[(l - 1) % 2]
        x_dst = out if l == NL - 1 else xs[l % 2]
        WIN = wb_in[l]
        kin = ('wb_in', l)
        S.barrier()
        bi = P["b_in"][l]
        S.dma('sp', d_p, g1[:], P["norm1_g"][l].rearrange("(c p) -> p c", p=128), W=['g1'])
        S.dma('sp', d_p, g2[:], P["norm2_g"][l].rearrange("(c p) -> p c", p=128), W=['g2'])
        S.dma('sp', d_p, bb[:, 0:1544], bi[512:2056].partition_broadcast(128), W=['bb'])
        S.dma('sp', d_p, bb[:, 1544:3084], bi[2056:3596].partition_broadcast(128), W=['bb'])
        S.dma('sp', d_p, bb[:, 3084:4108], bi[5132:6156].partition_broadcast(128), W=['bb'])
        S.dma('sp', d_p, bfe[:, 0:8], bi[0:1024].rearrange("(c p) -> p c", p=128), W=['bfe'])
        S.dma('sp', d_p, bfe[:, 8:16], bi[C_RX:C_RX + 1024].rearrange("(c p) -> p c", p=128), W=['bfe'])
        S.dma('sp', d_p, bfe[:, 16:20], bi[C_GQ:C_GQ + 512].rearrange("(c p) -> p c", p=128), W=['bfe'])
        S.dma('sp', d_p, bfe[0:16, 20:21], bi[C_GA:C_GA + 16].rearrange("(p c) -> p c", c=1), W=['bfe'])
        S.dma('sp', d_p, bfe[:, 21:53], bi[C_GT:C_GT + 4096].rearrange("(c p) -> p c", p=128), W=['bfe'])
        S.dma('sp', d_p, gqk[:, 0, :], P["fx_qnorm_g"][l].partition_broadcast(128), W=['gqk'])
        S.dma('sp', d_p, gqk[:, 1, :], P["fx_knorm_g"][l].partition_broadcast(128), W=['gqk'])
        for k_ in range(4):
            S.dma('sp', d_p, lpar[:, 4 * k_:4 * k_ + 4], P["lru_conv_w"][l, k_].rearrange("(c p) -> p c", p=128), W=['lpar'])
        for i_, nm in enumerate(["lru_conv_b", "lru_ba", "lru_bx", "lru_lambda"]):
            S.dma('sp', d_p, lpar[:, 16 + 4 * i_:20 + 4 * i_], P[nm][l].rearrange("(c p) -> p c", p=128), W=['lpar'])
        LP = ['lpar', 'lpard', 'lpare']
        wbd_f = junk[:, :].rearrange("p (w c k) -> p w c k", w=2, c=4)
        S.op('pool', 'memset', [], ['junk'], junk[:], 0.0)
        for wi_, nm in enumerate(["lru_wa", "lru_wx"]):
            for g in range(8):
                c, po = g // 2, (g % 2) * 64
                S.dma('sp', d_p, wbd_f[po:po + 64, wi_, c, po:po + 64], P[nm][l, g], R=[], W=['junk'])
        S.dma('sp', d_p, wal_f[:], P["gla_w_alpha"][l], W=['wal_f'])
        S.dma('sp', d_p, gpar[:, 0:2], P["gla_b_alpha"][l].rearrange("(c p) -> p c", p=128), W=['gpar'])
        S.barrier()
        S.op('pool', 'tensor_scalar', ['gqk'], ['gqk'], out=gqk[:, 0, :], in0=gqk[:, 0, :], scalar1=float(128 ** -0.5),
             scalar2=None, op0=ALU.mult)
        S.op('act', 'activation', ['lpar'], ['lparb'], out=lpar[:, 40:44], in_=lpar[:, 28:32], func=AF.Exp, scale=-1.0)
        S.op('act', 'activation', ['lparb'], ['lparc'], out=lpar[:, 44:48], in_=lpar[:, 40:44], func=AF.Ln, bias=1.0)
        S.op('dve', 'tensor_scalar', ['lparc'], ['lpard'], out=lpar[:, 32:36], in0=lpar[:, 44:48], scalar1=-8.0,
             scalar2=None, op0=ALU.mult)
        S.op('dve', 'tensor_scalar', ['lparc'], ['lpare'], out=lpar[:, 36:40], in0=lpar[:, 44:48], scalar1=-16.0,
             scalar2=None, op0=ALU.mult)
        S.op('pool', 'tensor_copy', ['junk'], ['wbd'], out=wbd[:], in_=wbd_f)
        S.op('pool', 'tensor_copy', ['wal_f'], ['wal'], out=wal[:], in_=wal_f[:])
        S.op('pool', 'tensor_scalar', ['gpar'], ['gparb'], out=gpar[:, 2:4], in0=gpar[:, 0:2], scalar1=-1.0,
             scalar2=None, op0=ALU.mult)
        for tl, key in [(Cst, 'Cst'), (Cbf, 'Cbf'), (Sst, 'Sst'), (Sbf, 'Sbf'), (lcar, 'lcar'), (Fcar, 'Fcar'), (rxy, 'rxy')]:
            S.op('pool', 'memset', [], [key], tl[:], 0.0)
        S.barrier()

        for t in range(NT):
            load_x(x_src, t)
            norm_from_xt(g1, 'g1')

            S.barrier()
            mqT = vB(0, [128, 4, 512])
            mkT = vB(2048, [128, 4, 512])
            mk_tok = vB(4096, [128, 4, 512])
            mv_aug = vB(6144, [128, 4, 4, 130])
            STb = vB(8224, [128, 4, 128])
            kd = vB(8736, [128, 4, 128])
            gs_ml = vF(0, [128, 4, 512])
            ytmp = vF(2048, [128, 4, 130])
            S.op('pool', 'memset', [], ['mv_aug'], mv_aug, 1.0)
            S.dma('sp', d_p, gbc[:], P["ml_norm_g"][l].partition_broadcast(128), W=['gbc'])

            def ev_mq(mc, m, pt_, pk):
                if mc < 4:
                    S.op('dve', 'tensor_scalar', ['bfe'], [pk, ('mqk', mc)], out=mqT[:, mc, :], in0=pt_[:, :],
                         scalar1=bfe[:, mc:mc + 1], scalar2=float(128 ** -0.5), op0=ALU.add, op1=ALU.mult)
                else:
                    S.op('act', 'activation', ['bfe'], [pk, ('mqk', mc)], out=mkT[:, mc - 4, :], in_=pt_[:, :],
                         func=AF.Identity, bias=bfe[:, mc:mc + 1])
            proj_feat(WIN, kin, C_MQ, 1024, ev_mq)

            def ev_mtok(j, off, n, pt_, pk):
                if off == 0:
                    S.op('dve', 'tensor_tensor', ['bb'], [pk, 'mk_tok'], out=mk_tok[:, j, :], in0=pt_[:, 0:512],
                         in1=bb[:, 0:512], op=ALU.add)
                elif off == 512:
                    S.op('dve', 'tensor_tensor', ['bb'], [pk, 'mv_aug'], out=mv_aug[:, j, :, 0:128],
                         in0=pt_[:, 0:512].rearrange("p (h e) -> p h e", h=4),
                         in1=bb[:, 512:1024].rearrange("p (h e) -> p h e", h=4), op=ALU.add)
                elif off == 1024:
                    S.op('dve', 'tensor_tensor', ['bb'], [pk, 'gs_ml'], out=gs_ml[:, j, :], in0=pt_[:, 0:512],
                         in1=bb[:, 1024:1536], op=ALU.add)
                    S.op('act', 'activation', ['gs_ml'], ['gs_ml'], out=gs_ml[:, j, :], in_=gs_ml[:, j, :], func=AF.Sigmoid)
                    S.op('pool', 'tensor_tensor', ['gs_ml', 'gbc'], ['gs_ml'], out=gs_ml[:, j, :], in0=gs_ml[:, j, :],
                         in1=gbc[:], op=ALU.mult)
                else:
                    S.op('dve', 'tensor_tensor', ['bb'], [pk, 'mif'], out=mif[:, j, :], in0=pt_[:, 0:8],
                         in1=bb[:, 1536:1544], op=ALU.add)
            proj_tok(WIN, kin, C_MK, 1544, ev_mtok)

            S.op('act', 'activation', ['mif'], ['sm_a'], out=sm[:, 0:16].rearrange("p (j h) -> p j h", h=4),
                 in_=mif[:, :, 4:8], func=AF.Exp, scale=-1.0)
            S.op('act', 'activation', ['sm_a'], ['sm_a'], out=sm[:, 0:16], in_=sm[:, 0:16], func=AF.Ln, bias=1.0)
            S.op('pe', 'matmul', ['sm_a', 'cst_f'], ['PA'], PA[:, 0:16], lhsT=tri_f, rhs=sm[:, 0:16], start=True, stop=True)
            S.op('pe', 'matmul', ['sm_a', 'cst_f'], ['PA'], PA[:, 16:32], lhsT=ones_f, rhs=sm[:, 0:16], start=True, stop=True)
            S.op('dve', 'tensor_copy', [], ['PA', 'sm_b'], out=sm[:, 16:48], in_=PA[:, 0:32])
            S.op('act', 'activation', ['sm_b'], ['sm_c'], out=sm[:, 48:80], in_=sm[:, 16:48], func=AF.Exp, scale=-1.0)
            S.op('dve', 'tensor_tensor', ['sm_b', 'mif'], ['sm_d'], out=sm[:, 96:112].rearrange("p (j h) -> p j h", h=4),
                 in0=sm[:, 16:32].rearrange("p (j h) -> p j h", h=4), in1=mif[:, :, 0:4], op=ALU.add)
            S.op('dve', 'tensor_tensor', ['sm_d', 'sm_b'], ['sm_e'], out=sm[:, 80:96], in0=sm[:, 96:112], in1=sm[:, 32:48],
                 op=ALU.subtract)
            S.op('act', 'activation', ['sm_e'], ['sm_e'], out=sm[:, 80:96], in_=sm[:, 80:96], func=AF.Exp)
            S.op('act', 'activation', ['sm_d'], ['sm_d'], out=sm[:, 96:112], in_=sm[:, 96:112], func=AF.Exp)
            for j in range(4):
                js = slice(j * 128, (j + 1) * 128)
                for h in range(4):
                    S.op('pe', 'matmul', [('mqk', h), ('mqk', 4 + h)], ['PA'], PA[:, h * 128:(h + 1) * 128],
                         lhsT=mkT[:, h, js], rhs=mqT[:, h, js], start=True, stop=True)
                for h in range(4):
                    col = 96 + j * 4 + h
                    S.op('dve', 'scalar_tensor_tensor', ['sm_d', 'mask4'], ['PA', 'STb'], out=STb[:, h, :],
                         in0=PA[:, h * 128:(h + 1) * 128], scalar=sm[:, col:col + 1], in1=mask4[:, h, :],
                         op0=ALU.mult, op1=ALU.mult)
                for h in range(4):
                    pk = ('PO', h // 2)
                    o_ap = PO[h // 2][:, (h % 2) * 130:(h % 2) * 130 + 130]
                    S.op('pe', 'matmul', ['STb', 'mv_aug'], [pk], o_ap, lhsT=STb[:, h, :], rhs=mv_aug[:, j, h, :],
                         start=(h % 2 == 0), stop=False, skip_group_check=True)
                    S.op('pe', 'matmul', [('mqk', h), 'Cbf'], [pk], o_ap, lhsT=mqT[:, h, js], rhs=Cbf[:, h, :],
                         start=False, stop=True, skip_group_check=True)
                for h in range(4):
                    col = 80 + j * 4 + h
                    S.op('pool', 'tensor_scalar', ['mk_tok', 'sm_e'], ['kd'], out=kd[:, h, :],
                         in0=mk_tok[:, j, h * 128:(h + 1) * 128], scalar1=sm[:, col:col + 1], scalar2=None, op0=ALU.mult)
                for h in range(4):
                    S.op('pe', 'matmul', ['kd', 'mv_aug'], ['PS'], PS[:, 0:130], lhsT=kd[:, h, :], rhs=mv_aug[:, j, h, :],
                         start=True, stop=True)
                    col = 64 + j * 4 + h
                    S.op('dve', 'scalar_tensor_tensor', ['sm_c'], ['PS', 'Cst'], out=Cst[:, h, :], in0=Cst[:, h, :],
                         scalar=sm[:, col:col + 1], in1=PS[:, 0:130], op0=ALU.mult, op1=ALU.add)
                for hp in range(2):
                    S.op('act', 'activation', [], [('PO', hp), 'ytmp'], out=ytmp[:, 2 * hp:2 * hp + 2, :],
                         in_=PO[hp][:, 0:260].rearrange("p (h e) -> p h e", h=2), func=AF.Copy)
                S.op('pool', 'tensor_copy', ['Cst'], ['Cbf'], out=Cbf[:], in_=Cst[:])
                eb4 = sm[:, 48 + j * 4:52 + j * 4]
                S.op('dve', 'tensor_tensor', ['ytmp', 'sm_c'], ['sm_f'], out=sm[:, 112:116], in0=ytmp[:, :, 128], in1=eb4,
                     op=ALU.mult)
                S.op('act', 'activation', ['sm_f'], ['sm_f'], out=sm[:, 112:116], in_=sm[:, 112:116], func=AF.Abs)
                S.op('dve', 'tensor_scalar', ['sm_f'], ['sm_f'], out=sm[:, 112:116], in0=sm[:, 112:116], scalar1=1.0,
                     scalar2=None, op0=ALU.max)
                S.op('dve', 'reciprocal', ['sm_f'], ['sm_f'], out=sm[:, 112:116], in_=sm[:, 112:116])
                S.op('dve', 'tensor_tensor', ['sm_f', 'sm_c'], ['sm_f'], out=sm[:, 112:116], in0=sm[:, 112:116], in1=eb4,
                     op=ALU.mult)
                for h in range(4):
                    S.op('dve', 'tensor_scalar', ['sm_f', 'ytmp'], ['ytmp'], out=ytmp[:, h, 0:128], in0=ytmp[:, h, 0:128],
                         scalar1=sm[:, 112 + h:113 + h], scalar2=None, op0=ALU.mult)
                for h in range(4):
                    S.op('act', 'activation', ['ytmp'], ['junk', ('sm_g', h)], out=junk[:, 0:128], in_=ytmp[:, h, 0:128],
                         func=AF.Square, accum_out=sm[:, 116 + h:117 + h])
                S.op('act', 'activation', [('sm_g', h) for h in range(4)], ['sm_h'], out=sm[:, 120:124], in_=sm[:, 116:120],
                     func=AF.Sqrt, scale=1.0 / 128, bias=EPS)
                S.op('dve', 'reciprocal', ['sm_h'], ['sm_h'], out=sm[:, 120:124], in_=sm[:, 120:124])
                for h in range(4):
                    S.op('dve', 'scalar_tensor_tensor', ['sm_h', 'ytmp', 'gs_ml'], ['ytok'], out=ytok[:, h * 128:(h + 1) * 128],
                         in0=ytmp[:, h, 0:128], scalar=sm[:, 120 + h:121 + h], in1=gs_ml[:, j, h * 128:(h + 1) * 128],
                         op0=ALU.mult, op1=ALU.mult)
                tok_to_yTs(j)
            yT_store(0, t)

            S.barrier()
            fqn = vB(0, [128, 512])
            fT = vB(512, [128, 4, 512])
            fv_aug = vB(2560, [128, 4, 4, 130])
            fq_t = vF(0, [128, 512])
            S.op('pool', 'memset', [], ['fv_aug'], fv_aug, 1.0)

            def ev_ftok(j, off, n, pt_, pk):
                if off < 1024:
                    w = off // 512
                    S.op('dve', 'tensor_tensor', ['bb'], [pk, 'fq_t'], out=fq_t, in0=pt_[:, 0:512],
                         in1=bb[:, 1544 + off:1544 + off + 512], op=ALU.add)
                    for h in range(4):
                        S.op('act', 'activation', ['fq_t'], ['junk', ('sm_i', h)], out=junk[:, 0:128],
                             in_=fq_t[:, h * 128:(h + 1) * 128], func=AF.Square, accum_out=sm[:, 128 + h:129 + h])
                    S.op('act', 'activation', [('sm_i', h) for h in range(4)], ['sm_j'], out=sm[:, 132:136],
                         in_=sm[:, 128:132], func=AF.Sqrt, scale=1.0 / 128, bias=EPS)
                    S.op('dve', 'reciprocal', ['sm_j'], ['sm_j'], out=sm[:, 132:136], in_=sm[:, 132:136])
                    for h in range(4):
                        S.op('dve', 'scalar_tensor_tensor', ['sm_j', 'fq_t', 'gqk'], ['fqn'], out=fqn[:, h * 128:(h + 1) * 128],
                             in0=fq_t[:, h * 128:(h + 1) * 128], scalar=sm[:, 132 + h:133 + h], in1=gqk[:, w, :],
                             op0=ALU.mult, op1=ALU.mult)
                    for h in range(4):
                        S.op('pe', 'transpose', ['fqn', 'ident_b'], ['PB'], out=PB[:, h * 128:(h + 1) * 128],
                             in_=fqn[:, h * 128:(h + 1) * 128], identity=ident_b[:])
                    S.op('act', 'activation', [], ['PB', 'fT'], out=fT[:, :, j * 128:(j + 1) * 128],
                         in_=PB[:, 0:512].rearrange("p (c s) -> p c s", c=4), func=AF.Copy)
                    if j == 3:
                        dst = QTd if w == 0 else KTd
                        S.dma('pool', d_f, dst[:, :, t * 512:(t + 1) * 512].rearrange("h p s -> p h s"), fT,
                              R=['fT'], W=[('QK', w)])
                elif off == 1024:
                    S.op('dve', 'tensor_tensor', ['bb'], [pk, 'fv_aug'], out=fv_aug[:, :, j, 0:128],
                         in0=pt_[:, 0:512].rearrange("p (h e) -> p h e", h=4),
                         in1=bb[:, 1544 + 1024:1544 + 1536].rearrange("p (h e) -> p h e", h=4), op=ALU.add)
                    if j == 3:
                        S.dma('pool', d_f2, Vd[:, :, t * 4:(t + 1) * 4, :].rearrange("h p j e -> p h j e"), fv_aug,
                              R=['fv_aug'], W=['Vd'])
                else:
                    S.op('dve', 'tensor_tensor', ['bb'], [pk, 'sm_k'], out=sm[:, 136:140], in0=pt_[:, 0:4],
                         in1=bb[:, 1544 + 1536:1544 + 1540], op=ALU.add)
                    S.op('act', 'activation', ['sm_k'], ['sm_k'], out=sm[:, 136:140], in_=sm[:, 136:140], func=AF.Exp, scale=-1.0)
                    S.op('act', 'activation', ['sm_k'], ['sm_k'], out=sm[:, 136:140], in_=sm[:, 136:140], func=AF.Ln, bias=1.0)
                    S.op('pe', 'matmul', ['sm_k', 'cst_f'], ['PA'], PA[:, 0:4], lhsT=tri_f, rhs=sm[:, 136:140], start=True, stop=True)
                    S.op('pe', 'matmul', ['sm_k', 'cst_f'], ['PA'], PA[:, 4:8], lhsT=ones_f, rhs=sm[:, 136:140], start=True, stop=True)
                    if j == 0:
                        S.op('dve', 'tensor_scalar', ['Fcar'], ['rI'], out=rI[:, t, :], in0=Fcar[:], scalar1=-1.0, scalar2=None,
                             op0=ALU.mult)
                    S.op('dve', 'tensor_tensor', ['Fcar'], ['PA', 'Fall'], out=Fall[:, t * 4 + j, :], in0=PA[:, 0:4], in1=Fcar[:],
                         op=ALU.add)
                    S.op('dve', 'tensor_tensor', [], ['PA', 'Fcar'], out=Fcar[:], in0=PA[:, 4:8], in1=Fcar[:], op=ALU.add)
            proj_tok(WIN, kin, C_FQ, 1540, ev_ftok)

            S.barrier()
            ryT = vF(0, [128, 4, 512])
            xc = vF(2048, [128, 4, 512])
            lr = vF(4096, [128, 512])
            li = vF(4608, [128, 512])
            la_ = vF(5120, [128, 512])
            lu = vF(5632, [128, 512])
            lh = vF(6144, [128, 512])
            xcb = vB(0, [128, 4, 512])

            def ev_rx(mc, m, pt_, pk):
                if mc < 4:
                    S.op('act', 'activation', ['bfe'], [pk, 'rxy'], out=rxy[:, mc, 3:515], in_=pt_[:, :], func=AF.Identity,
                         bias=bfe[:, 8 + mc:9 + mc])
                else:
                    S.op('act', 'activation', ['bfe'], [pk, 'ryT'], out=ryT[:, mc - 4, :], in_=pt_[:, :], func=AF.Identity,
                         bias=bfe[:, 8 + mc:9 + mc])
            proj_feat(WIN, kin, C_RX, 1024, ev_rx)
            for c in range(4):
                S.op('dve', 'tensor_scalar', ['rxy'] + LP, ['xc'], out=xc[:, c, :], in0=rxy[:, c, 0:512],
                     scalar1=lpar[:, c:c + 1], scalar2=lpar[:, 16 + c:17 + c], op0=ALU.mult, op1=ALU.add)
                for k in range(1, 4):
                    S.op('dve', 'scalar_tensor_tensor', ['rxy'] + LP, ['xc'], out=xc[:, c, :], in0=rxy[:, c, k:k + 512],
                         scalar=lpar[:, 4 * k + c:4 * k + c + 1], in1=xc[:, c, :], op0=ALU.mult, op1=ALU.add)
            S.op('pool', 'tensor_copy', ['xc'], ['xcb'], out=xcb, in_=xc)
            S.op('pool', 'tensor_copy', [], ['rxy'], out=rxy[:, :, 0:3], in_=rxy[:, :, 512:515])
            for c in range(4):
                pj = next_pj()
                S.op('pe', 'matmul', ['wbd', 'xcb'], [('PJ', pj)], PJ[pj][:, :], lhsT=wbd[:, 0, c, :], rhs=xcb[:, c, :],
                     start=True, stop=True)
                S.op('act', 'activation', LP, [('PJ', pj), 'lr'], out=lr, in_=PJ[pj][:, :], func=AF.Sigmoid,
                     bias=lpar[:, 20 + c:21 + c])
                pj = next_pj()
                S.op('pe', 'matmul', ['wbd', 'xcb'], [('PJ', pj)], PJ[pj][:, :], lhsT=wbd[:, 1, c, :], rhs=xcb[:, c, :],
                     start=True, stop=True)
                S.op('act', 'activation', LP, [('PJ', pj), 'li'], out=li, in_=PJ[pj][:, :], func=AF.Sigmoid,
                     bias=lpar[:, 24 + c:25 + c])
                S.op('act', 'activation', ['lr'] + LP, ['la_'], out=la_, in_=lr, func=AF.Exp, scale=lpar[:, 32 + c:33 + c])
                S.op('act', 'activation', ['lr'] + LP, ['lu'], out=lu, in_=lr, func=AF.Exp, scale=lpar[:, 36 + c:37 + c])
                S.op('dve', 'tensor_scalar', ['lu'], ['lu'], out=lu, in0=lu, scalar1=-1.0, scalar2=1.0, op0=ALU.mult, op1=ALU.add)
                S.op('dve', 'tensor_scalar', ['lu'], ['lu'], out=lu, in0=lu, scalar1=0.0, scalar2=None, op0=ALU.max)
                S.op('act', 'activation', ['lu'], ['lu'], out=lu, in_=lu, func=AF.Sqrt)
                S.op('dve', 'tensor_tensor', ['li', 'xc'], ['li'], out=li, in0=li, in1=xc[:, c, :], op=ALU.mult)
                S.op('dve', 'tensor_tensor', ['li', 'lu'], ['lu'], out=lu, in0=lu, in1=li, op=ALU.mult)
                S.op('dve', 'tensor_tensor_scan', ['la_', 'lu', 'lcar'], ['lh'], out=lh, data0=la_, data1=lu,
                     initial=lcar[:, c:c + 1], op0=ALU.mult, op1=ALU.add)
                S.op('pool', 'tensor_copy', ['lh'], ['lcar'], out=lcar[:, c:c + 1], in_=lh[:, 511:512])
                yv = ryT[:, c, :]
                S.op('pool', 'tensor_tensor', ['ryT'], ['li'], out=li, in0=yv, in1=yv, op=ALU.mult)
                S.op('dve', 'tensor_scalar', ['li'], ['li'], out=li, in0=li, scalar1=0.044715, scalar2=1.0, op0=ALU.mult,
                     op1=ALU.add)
                S.op('dve', 'tensor_tensor', ['li', 'ryT'], ['li'], out=li, in0=li, in1=yv, op=ALU.mult)
                S.op('act', 'activation', ['li'], ['li'], out=li, in_=li, func=AF.Sigmoid, scale=1.5957691216)
                S.op('dve', 'tensor_tensor', ['li', 'ryT'], ['li'], out=li, in0=li, in1=yv, op=ALU.mult)
                S.op('dve', 'tensor_tensor', ['li', 'lh'], ['yTs'], out=yTs[:, c, :], in0=li, in1=lh, op=ALU.mult)
            yT_store(2, t)

            S.barrier()
            gqT = vF(0, [128, 2, 512])
            gkT = vF(1024, [128, 2, 512])
            gsp = vF(2048, [128, 2, 512])
            gcs = vF(3072, [128, 2, 512])
            geq = vF(4096, [128, 2, 512])
            gek = vF(5120, [128, 2, 512])
            gsil = vF(6144, [128, 4, 512])
            qtT = vB(0, [128, 2, 512])
            ktT = vB(1024, [128, 2, 512])
            kt_tok = vB(2048, [128, 256])
            gv_t = vB(2304, [128, 4, 512])
            Ab = vB(4352, [128, 4, 128])
            gaT = vB(4864, [128, 512])
            S.dma('sp', d_p, gbc[:], P["gla_norm_g"][l].partition_broadcast(128), W=['gbc'])

            def ev_gq(mc, m, pt_, pk):
                dst = gqT if mc < 2 else gkT
                S.op('act', 'activation', ['bfe'], [pk, ('gqk_', mc)], out=dst[:, mc % 2, :], in_=pt_[:, :], func=AF.Identity,
                     bias=bfe[:, 16 + mc:17 + mc])
            proj_feat(WIN, kin, C_GQ, 512, ev_gq)

            def ev_ga(mc, m, pt_, pk):
                S.op('act', 'activation', ['bfe'], [pk, 'gaT'], out=gaT[0:16, :], in_=pt_[0:16, :], func=AF.Identity,
                     bias=bfe[0:16, 20:21])
            proj_feat(WIN, kin, C_GA, 16, ev_ga)

            def ev_gtok(j, off, n, pt_, pk):
                if off == 0:
                    S.op('dve', 'tensor_tensor', ['bb'], [pk, 'gv_t'], out=gv_t[:, j, :], in0=pt_[:, 0:512],
                         in1=bb[:, 3084:3596], op=ALU.add)
                else:
                    S.op('dve', 'tensor_tensor', ['bb'], [pk, 'gsil'], out=gsil[:, j, :], in0=pt_[:, 0:512],
                         in1=bb[:, 3596:4108], op=ALU.add)
                    S.op('act', 'activation', ['gsil'], ['junk'], out=junk[:, 0:512], in_=gsil[:, j, :], func=AF.Sigmoid)
                    S.op('dve', 'tensor_tensor', ['junk'], ['gsil'], out=gsil[:, j, :], in0=gsil[:, j, :],
                         in1=junk[:, 0:512], op=ALU.mult)
                    S.op('pool', 'tensor_tensor', ['gbc'], ['gsil'], out=gsil[:, j, :], in0=gsil[:, j, :], in1=gbc[:],
                         op=ALU.mult)
            proj_tok(WIN, kin, C_GV, 1024, ev_gtok)
            for c in range(2):
                pj = next_pj()
                S.op('pe', 'matmul', ['wal', 'gaT'], [('PJ', pj)], PJ[pj][:, :], lhsT=wal[:, c * 128:(c + 1) * 128],
                     rhs=gaT[0:16, :], start=True, stop=True)
                S.op('act', 'activation', ['gparb'], [('PJ', pj), 'gsp'], out=gsp[:, c, :], in_=PJ[pj][:, :], func=AF.Exp,
                     scale=-1.0, bias=gpar[:, 2 + c:3 + c])
                S.op('act', 'activation', ['gsp'], ['gsp'], out=gsp[:, c, :], in_=gsp[:, c, :], func=AF.Ln, bias=1.0)
                for j in range(4):
                    js = slice(j * 128, (j + 1) * 128)
                    S.op('dve', 'tensor_tensor_scan', ['gsp', 'cst_f'], ['gcs'], out=gcs[:, c, js], data0=ones_f,
                         data1=gsp[:, c, js], initial=0.0, op0=ALU.mult, op1=ALU.add)
                S.op('act', 'activation', ['gcs'], ['geq'], out=geq[:, c, :], in_=gcs[:, c, :], func=AF.Exp, scale=-1.0 / 16)
                S.op('act', 'activation', ['gcs'], ['gek'], out=gek[:, c, :], in_=gcs[:, c, :], func=AF.Exp, scale=1.0 / 16)
                S.op('dve', 'scalar_tensor_tensor', [('gqk_', c), 'geq'], ['qtT'], out=qtT[:, c, :], in0=gqT[:, c, :],
                     scalar=0.125, in1=geq[:, c, :], op0=ALU.mult, op1=ALU.mult)
                S.op('dve', 'tensor_tensor', [('gqk_', 2 + c), 'gek'], ['ktT'], out=ktT[:, c, :], in0=gkT[:, c, :],
                     in1=gek[:, c, :], op=ALU.mult)
            PAR = [PA, PS]
            PARK = ['PA', 'PS']
            for j in range(4):
                js = slice(j * 128, (j + 1) * 128)
                for h in range(4):
                    c, par = h // 2, h % 2
                    po_ = par * 64
                    S.op('pe', 'matmul', ['ktT', 'qtT'], [PARK[par]], PAR[par][:, c * 128:(c + 1) * 128],
                         lhsT=ktT[po_:po_ + 64, c, js], rhs=qtT[po_:po_ + 64, c, js], start=True, stop=True)
                for par in range(2):
                    S.op('dve', 'tensor_tensor', ['mask4'], [PARK[par], 'Ab'], out=Ab[:, 2 * par:2 * par + 2, :],
                         in0=PAR[par][:, 0:256].rearrange("p (h s) -> p h s", h=2), in1=mask4[:, 0:2, :], op=ALU.mult)
                for h in range(4):
                    c, par = h // 2, h % 2
                    po_ = par * 64
                    o_ap = PO[par][:, c * 128:(c + 1) * 128]
                    S.op('pe', 'matmul', ['Ab', 'gv_t'], [('PO', par)], o_ap, lhsT=Ab[:, 2 * par + c, :],
                         rhs=gv_t[:, j, h * 128:(h + 1) * 128], start=(c == 0), stop=False, skip_group_check=True)
                    S.op('pe', 'matmul', ['qtT', 'Sbf'], [('PO', par)], o_ap, lhsT=qtT[po_:po_ + 64, c, js],
                         rhs=Sbf[po_:po_ + 64, c, :], start=False, stop=True, skip_group_check=True)
                for c in range(2):
                    S.op('pe', 'transpose', ['ktT', 'ident_b'], ['PB'], out=PB[:, c * 128:(c + 1) * 128], in_=ktT[:, c, js],
                         identity=ident_b[:])
                S.op('act', 'activation', [], ['PB', 'kt_tok'], out=kt_tok, in_=PB[:, 0:256], func=AF.Copy)
                for h in range(4):
                    c, po_ = h // 2, (h % 2) * 64
                    S.op('pe', 'matmul', ['kt_tok', 'gv_t'], ['PS'], PS[po_:po_ + 64, c * 128:(c + 1) * 128],
                         lhsT=kt_tok[:, h * 64:(h + 1) * 64], rhs=gv_t[:, j, h * 128:(h + 1) * 128], start=(h < 2), stop=True,
                         skip_group_check=True)
                for c in range(2):
                    S.op('dve', 'tensor_tensor', [], ['PS', 'Sst'], out=Sst[:, c, :], in0=PS[:, c * 128:(c + 1) * 128],
                         in1=Sst[:, c, :], op=ALU.add)
                    S.op('dve', 'tensor_scalar', ['geq'], ['Sst'], out=Sst[:, c, :], in0=Sst[:, c, :],
                         scalar1=geq[:, c, j * 128 + 127:j * 128 + 128], scalar2=None, op0=ALU.mult)
                for h in range(4):
                    c, par = h // 2, h % 2
                    S.op('act', 'activation', [], [('PO', par), 'junk', ('sm_l', h)], out=junk[:, 0:128],
                         in_=PO[par][:, c * 128:(c + 1) * 128], func=AF.Square, accum_out=sm[:, 140 + h:141 + h])
                S.op('pool', 'tensor_copy', ['Sst'], ['Sbf'], out=Sbf[:], in_=Sst[:])
                S.op('act', 'activation', [('sm_l', h) for h in range(4)], ['sm_m'], out=sm[:, 144:148], in_=sm[:, 140:144],
                     func=AF.Sqrt, scale=1.0 / 128, bias=EPS)
                S.op('dve', 'reciprocal', ['sm_m'], ['sm_m'], out=sm[:, 144:148], in_=sm[:, 144:148])
                for h in range(4):
                    c, par = h // 2, h % 2
                    S.op('dve', 'scalar_tensor_tensor', ['sm_m', 'gsil'], [('PO', par), 'ytok'], out=ytok[:, h * 128:(h + 1) * 128],
                         in0=PO[par][:, c * 128:(c + 1) * 128], scalar=sm[:, 144 + h:145 + h], in1=gsil[:, j, h * 128:(h + 1) * 128],
                         op0=ALU.mult, op1=ALU.mult)
                tok_to_yTs(j)
            yT_store(3, t)

        S.barrier()
        KTs = vB(0, [128, S_])
        Vs = vB(S_, [128, NB, 130])
        o0 = S_ + NB * 130
        QTs = vB(o0, [128, 512])
        Pb = vB(o0 + 512, [128, 2, 512])
        ob = vB(o0 + 1536, [128, 512])
        biasI = vF(0, [128, NB])
        rd = vF(NB, [128, 4])
        d_k = d_f
        d_v = d_f2
        d_q = d_p
        OB = [PO[0], PO[1], PA, PS]
        OK = [('PO', 0), ('PO', 1), 'PA', 'PS']
        pcnt = 0
        for h in range(4):
            S.dma('sp', d_k, KTs, KTd[h], R=[('QK', 1)], W=['KTs'])
            S.dma('sp', d_v, Vs, Vd[h], R=['Vd'], W=['Vs'])
            for I in range(NT):
                S.dma('sp', d_q, QTs, QTd[h, :, I * 512:(I + 1) * 512], R=[('QK', 0)], W=['QTs'])
                nj = 4 * I + 4
                S.op('dve', 'tensor_scalar', ['Fall', 'rI'], ['biasI'], out=biasI[:, 0:nj], in0=Fall[:, 0:nj, h],
                     scalar1=rI[:, I, h:h + 1], scalar2=None, op0=ALU.add)
                for j in range(nj):
                    jj = j - 4 * I
                    q0 = 0 if jj <= 0 else jj * 128
                    nq = 512 - q0
                    pj = pcnt % 2
                    pcnt += 1
                    S.op('pe', 'matmul', ['KTs', 'QTs'], [('PJ', pj)], PJ[pj][:, 0:nq], lhsT=KTs[:, j * 128:(j + 1) * 128],
                         rhs=QTs[:, q0:512], start=True, stop=True)
                    S.op('act', 'activation', ['biasI'], [('PJ', pj), ('Pb', pj)], out=Pb[:, pj, 0:nq], in_=PJ[pj][:, 0:nq],
                         func=AF.Exp, bias=biasI[:, j:j + 1])
                    if jj >= 0:
                        S.op('pool', 'tensor_tensor', ['mask4'], [('Pb', pj)], out=Pb[:, pj, 0:128], in0=Pb[:, pj, 0:128],
                             in1=mask4[:, 0, :], op=ALU.mult)
                    for qs in range(q0 // 128, 4):
                        last = (j == 4 * I + qs)
                        S.op('pe', 'matmul', [('Pb', pj), 'Vs'], [OK[qs]], OB[qs][:, 0:130],
                             lhsT=Pb[:, pj, qs * 128 - q0:(qs + 1) * 128 - q0], rhs=Vs[:, j, :], start=(j == 0), stop=last)
                for qs in range(4):
                    S.op('dve', 'reciprocal', [], [OK[qs], ('rd', qs)], out=rd[:, qs:qs + 1], in_=OB[qs][:, 128:129])
                    S.op('dve', 'tensor_scalar', [('rd', qs)], [OK[qs], 'ob'], out=ob[:, qs * 128:(qs + 1) * 128],
                         in0=OB[qs][:, 0:128], scalar1=rd[:, qs:qs + 1], scalar2=None, op0=ALU.mult)
                for qs in range(4):
                    S.op('pe', 'transpose', ['ob', 'ident_b'], ['PB'], out=PB[:, qs * 128:(qs + 1) * 128],
                         in_=ob[:, qs * 128:(qs + 1) * 128], identity=ident_b[:])
                S.op('act', 'activation', [], ['PB', 'yTs'], out=yTs[:, 0, :], in_=PB[:, 0:512], func=AF.Copy)
                S.dma('pool', d_y, yT[4 + h, :, I * 512:(I + 1) * 512], yTs[:, 0, :], R=['yTs'], W=[('yT', 1)])

        S.barrier()
        ybr = vB(0, [128, 16, 512])
        mgb = vB(8192, [128, 8, 512])
        aT = vB(12288, [128, 16, 512])
        mg = vF(0, [128, 8, 512])
        sg = vF(4096, [128, 512])
        rl = vF(4608, [128, 512])
        d_yb = d_f
        d_o = d_f2
        accs = [PO[0], PO[1], PA, PS]
        acck = [('PO', 0), ('PO', 1), 'PA', 'PS']
        for t in range(NT):
            load_x(x_src, t)
            norm_from_xt(g1, 'g1')
            S.dma('sp', d_yb, ybr, yT[:, :, t * 512:(t + 1) * 512].rearrange("c p s -> p c s"),
                  R=[('yT', b) for b in range(4)], W=['ybr'])
            for n in range(4):
                for half in range(2):
                    i = load_w(wb_br[l][n * 512:(n + 1) * 512, half * 512:(half + 1) * 512], 512, ('wb_br', l), kch=4)
                    gi = load_w(WIN[:, C_GT + n * 1024 + half * 512:C_GT + n * 1024 + half * 512 + 512], 512, kin)
                    for m in range(4):
                        dc = half * 4 + m
                        pj = next_pj()
                        for kc in range(8):
                            S.op('pe', 'matmul', [('wsl', gi), ('hT', kc)], [('PJ', pj)], PJ[pj][:, :],
                                 lhsT=wsl[gi][:, kc, m * 128:(m + 1) * 128], rhs=hT[:, kc, :], start=(kc == 0), stop=(kc == 7))
                        S.op('act', 'activation', ['bfe'], [('PJ', pj), 'sg'], out=sg, in_=PJ[pj][:, :], func=AF.Sigmoid,
                             bias=bfe[:, 21 + n * 8 + dc:22 + n * 8 + dc])
                        pj2 = next_pj()
                        for kc in range(4):
                            S.op('pe', 'matmul', [('wsl', i), 'ybr'], [('PJ', pj2)], PJ[pj2][:, :],
                                 lhsT=wsl[i][:, kc, m * 128:(m + 1) * 128], rhs=ybr[:, n * 4 + kc, :], start=(kc == 0),
                                 stop=(kc == 3))
                        if n == 0:
                            S.op('dve', 'tensor_tensor', ['sg'], [('PJ', pj2), ('mg', dc)], out=mg[:, dc, :], in0=PJ[pj2][:, :],
                                 in1=sg, op=ALU.mult)
                        else:
                            S.op('dve', 'tensor_tensor', [], [('PJ', pj2), 'sg'], out=sg, in0=PJ[pj2][:, :], in1=sg,
                                 op=ALU.mult)
                            S.op('pool', 'tensor_tensor', ['sg'], [('mg', dc)], out=mg[:, dc, :], in0=mg[:, dc, :], in1=sg,
                                 op=ALU.add)
            for dc in range(8):
                S.op('pool', 'tensor_copy', [('mg', dc)], ['mgb'], out=mgb[:, dc, :], in_=mg[:, dc, :])
            for half in range(2):
                wi = load_w(wb_o[l][:, half * 512:(half + 1) * 512], 512, ('wb_o', l))
                for j in range(4):
                    pj = next_pj()
                    for kc in range(8):
                        S.op('pe', 'matmul', [('wsl', wi), 'mgb'], [('PJ', pj)], PJ[pj][:, :],
                             lhsT=mgb[:, kc, j * 128:(j + 1) * 128], rhs=wsl[wi][:, kc, :], start=(kc == 0), stop=(kc == 7))
                    S.op('dve', 'tensor_tensor', [], [('PJ', pj), 'xt'], out=xt[:, j, half * 512:(half + 1) * 512],
                         in0=PJ[pj][:, :], in1=xt[:, j, half * 512:(half + 1) * 512], op=ALU.add)
            norm_from_xt(g2, 'g2')
            for fh in range(2):
                for grp in range(4):
                    wi = load_w(wb_up[l][:, fh * 2048 + grp * 512:fh * 2048 + (grp + 1) * 512], 512, ('wb_up', l))
                    for m in range(4):
                        fc = grp * 4 + m
                        pj = next_pj()
                        for kc in range(8):
                            S.op('pe', 'matmul', [('wsl', wi), ('hT', kc)], [('PJ', pj)], PJ[pj][:, :],
                                 lhsT=wsl[wi][:, kc, m * 128:(m + 1) * 128], rhs=hT[:, kc, :], start=(kc == 0), stop=(kc == 7))
                        S.op('act', 'activation', [], [('PJ', pj), 'rl'], out=rl, in_=PJ[pj][:, :], func=AF.Relu)
                        S.op('pool', 'tensor_tensor', ['rl'], [('aT', fc)], out=aT[:, fc, :], in0=rl, in1=rl, op=ALU.mult)
                for half in range(2):
                    for grp in range(2):
                        r0 = fh * 2048 + grp * 1024
                        wi = load_w(wb_dn[l][r0:r0 + 1024, half * 512:(half + 1) * 512], 512, ('wb_dn', l))
                        for j in range(4):
                            for kc in range(8):
                                fc = grp * 8 + kc
                                S.op('pe', 'matmul', [('wsl', wi), ('aT', fc)], [acck[j]], accs[j][:, :],
                                     lhsT=aT[:, fc, j * 128:(j + 1) * 128], rhs=wsl[wi][:, kc, :], start=(fc == 0), stop=(fc == 15))
                    for j in range(4):
                        S.op('dve', 'tensor_tensor', [], [acck[j], 'xt'], out=xt[:, j, half * 512:(half + 1) * 512],
                             in0=accs[j][:, :], in1=xt[:, j, half * 512:(half + 1) * 512], op=ALU.add)
            S.dma('pool', d_o, x_dst[t * 512:(t + 1) * 512, :].rearrange("(j p) d -> p j d", p=128), xt[:],
                  R=['xt'], W=[('xs', id(x_dst))])

    S.barrier()
    S.emit()
    print("instr counts", S.ecnt, "waits", S.nwaits, "dsems", S.ndsem, flush=True)
    return nc, es


def make_consts():
    c = np.zeros((128, 384), np.float32)
    c[:, 0:128] = np.eye(128, dtype=np.float32)
    c[:, 128:256] = np.triu(np.ones((128, 128), np.float32))
    c[:, 256:384] = 1.0
    return c


_CACHE = {}


def run(inputs, dbg=False):
    x = np.ascontiguousarray(inputs["x"], dtype=np.float32)
    B, S_, _ = x.shape
    NL = inputs["w_in"].shape[0]
    key = (S_, NL, dbg)
    if key not in _CACHE:
        _CACHE[key] = build(S_, NL, dbg)
    nc, _es = _CACHE[key]
    cst = make_consts()
    in_maps = []
    for b in range(B):
        m = {"x": x[b], "cst": cst}
        for k in PNAMES:
            m[k] = np.ascontiguousarray(inputs[k], dtype=np.float32)
        in_maps.append(m)
    res = run_bass_kernel_spmd(nc, in_maps, core_ids=list(range(B)))
    return res


def kernel(**inputs):
    res = run(inputs)
    return np.stack([r["out"] for r in res.results], axis=0).astype(np.float32)
```

```python
import os
import numpy as np
import concourse.bass as bass
import concourse.mybir as mybir
from concourse.bass_utils import run_bass_kernel_spmd
from contextlib import ExitStack

F32 = mybir.dt.float32
BF16 = mybir.dt.bfloat16
AF = mybir.ActivationFunctionType
ALU = mybir.AluOpType

D = 1024
NIN = 10268
DFF = 4096
EPS = 1e-6
C_MQ, C_MK, C_MV, C_MO, C_MI, C_MF = 0, 512, 1024, 1536, 2048, 2052
C_FQ, C_FK, C_FV, C_FF = 2056, 2568, 3080, 3592
C_RX, C_RY = 3596, 4108
C_GQ, C_GK, C_GV, C_GG, C_GA = 4620, 4876, 5132, 5644, 6156
C_GT = 6172


class DSem:
    def __init__(self, h):
        self.h = h
        self.count = 0


class Sync:
    ENG = ('pe', 'act', 'dve', 'pool', 'sp')

    def __init__(self, nc, es):
        self.nc = nc
        self.es = es
        self.esem = {k: es.enter_context(nc.semaphore('s_' + k)) for k in self.ENG}
        self.ecnt = {k: 0 for k in self.ENG}
        self.seen = {k: {} for k in self.ENG}
        self.prog = {k: [] for k in self.ENG}
        self.lastw = {}
        self.readers = {}
        self.nwaits = 0
        self.ndsem = 0
        self.dsems = []

    def dsem(self, name=None, nobar=False):
        self.ndsem += 1
        d = DSem(self.es.enter_context(self.nc.semaphore(name or ('d%d' % self.ndsem))))
        if not nobar:
            self.dsems.append(d)
        return d

    def barrier(self):
        for e in self.ENG:
            for o in self.ENG:
                if o != e and self.ecnt[o] > 0:
                    self._wait(e, (self.esem[o], self.ecnt[o]))
            for d in self.dsems:
                if d.count > 0:
                    self._wait(e, (d.h, d.count))

    def _wait(self, eng, tok):
        sem, val = tok
        if eng == 'pe' and sem is self.esem['pe']:
            return
        s = self.seen[eng]
        if s.get(id(sem), 0) >= val:
            return
        self.prog[eng].append(('w', sem, val))
        self.nwaits += 1
        s[id(sem)] = val

    def _deps(self, eng, R, W):
        for k in R:
            t = self.lastw.get(k)
            if t:
                self._wait(eng, t)
        for k in W:
            t = self.lastw.get(k)
            if t:
                self._wait(eng, t)
            for t in self.readers.get(k, {}).values():
                self._wait(eng, t)

    def _record(self, tok, R, W):
        for k in W:
            self.lastw[k] = tok
            self.readers[k] = {}
        for k in R:
            self.readers.setdefault(k, {})[id(tok[0])] = tok

    def op(self, eng, meth, R, W, *args, **kw):
        self._deps(eng, R, W)
        self.ecnt[eng] += 1
        self.prog[eng].append(('i', meth, args, kw, self.esem[eng], 1))
        self._record((self.esem[eng], self.ecnt[eng]), R, W)

    def dma(self, q, ds, out, in_, R=(), W=(), **kw):
        self._deps(q, R, W)
        ds.count += 16
        self.prog[q].append(('i', 'dma_start', (), dict(out=out, in_=in_, **kw), ds.h, 16))
        self._record((ds.h, ds.count), R, W)

    def wait_all(self, eng, keys):
        for k in keys:
            t = self.lastw.get(k)
            if t:
                self._wait(eng, t)

    def emit(self):
        nc = self.nc

        def replay(name, e):
            for it in self.prog[name]:
                if it[0] == 'w':
                    e.wait_ge(it[1], it[2])
                else:
                    _, meth, args, kw, sem, inc = it
                    getattr(e, meth)(*args, **kw).then_inc(sem, inc)

        with nc.Block() as block:
            @block.sync
            def _(e):
                replay('sp', e)

            @block.tensor
            def _(e):
                replay('pe', e)

            @block.scalar
            def _(e):
                replay('act', e)

            @block.vector
            def _(e):
                replay('dve', e)

            @block.gpsimd
            def _(e):
                replay('pool', e)


PNAMES = ["norm1_g", "w_in", "b_in", "ml_norm_g", "fx_qnorm_g", "fx_knorm_g", "lru_conv_w", "lru_conv_b",
          "lru_wa", "lru_ba", "lru_wx", "lru_bx", "lru_lambda", "gla_w_alpha", "gla_b_alpha", "gla_norm_g",
          "w_branch", "w_o", "norm2_g", "w_up", "w_down"]


def build(S_, NL, dbg=False):
    NT = S_ // 512
    NB = S_ // 128
    nc = bass.Bass("TRN2", target_bir_lowering=False)
    es = ExitStack()
    es.enter_context(nc.allow_non_contiguous_dma(reason="small param loads"))
    S = Sync(nc, es)

    def dram(name, shape, dt, kind="Internal"):
        return nc.dram_tensor(name, shape, dt, kind=kind).ap()

    x_in = dram("x", [S_, D], F32, "ExternalInput")
    cst = dram("cst", [128, 384], F32, "ExternalInput")
    shp = dict(norm1_g=[NL, D], w_in=[NL, D, NIN], b_in=[NL, NIN], ml_norm_g=[NL, 512], fx_qnorm_g=[NL, 128],
               fx_knorm_g=[NL, 128], lru_conv_w=[NL, 4, 512], lru_conv_b=[NL, 512], lru_wa=[NL, 8, 64, 64],
               lru_ba=[NL, 512], lru_wx=[NL, 8, 64, 64], lru_bx=[NL, 512], lru_lambda=[NL, 512],
               gla_w_alpha=[NL, 16, 256], gla_b_alpha=[NL, 256], gla_norm_g=[NL, 512],
               w_branch=[NL, 4, 512, D], w_o=[NL, D, D], norm2_g=[NL, D], w_up=[NL, D, DFF], w_down=[NL, DFF, D])
    P = {k: dram(k, shp[k], F32, "ExternalInput") for k in PNAMES}
    out = dram("out", [S_, D], F32, "ExternalOutput")
    dbg_y = dram("dbg_y", [16, 128, S_], BF16, "ExternalOutput") if dbg else None

    wb_in = dram("wb_in", [NL, D, NIN], BF16)
    wb_br = dram("wb_br", [NL, 2048, D], BF16)
    wb_o = dram("wb_o", [NL, D, D], BF16)
    wb_up = dram("wb_up", [NL, D, DFF], BF16)
    wb_dn = dram("wb_dn", [NL, DFF, D], BF16)
    xs = [dram("xs0", [S_, D], F32), dram("xs1", [S_, D], F32)]
    yT = dbg_y if dbg else dram("yT", [16, 128, S_], BF16)
    QTd = dram("QTd", [4, 128, S_], BF16)
    KTd = dram("KTd", [4, 128, S_], BF16)
    Vd = dram("Vd", [4, 128, NB, 130], BF16)

    def sb(name, shape, dt):
        return es.enter_context(nc.sbuf_tensor(name, shape, dt))

    def ps(name, shape, dt):
        return es.enter_context(nc.psum_tensor(name, shape, dt))

    def cast_w(src, dst, key):
        ds = S.dsem(nobar=True)
        n = 1
        for s_ in src.shape:
            n *= s_
        rows = n // 2048
        pat = " ".join("abcd"[:len(src.shape)])
        sv = src.rearrange(f"{pat} -> ({pat})").rearrange("(r c) -> r c", c=2048)
        pat = " ".join("abcd"[:len(dst.shape)])
        dv = dst.rearrange(f"{pat} -> ({pat})").rearrange("(r c) -> r c", c=2048)
        r0 = 0
        while r0 < rows:
            r1 = min(rows, r0 + 2048)
            S.dma('pool', ds, dv[r0:r1, :], sv[r0:r1, :], W=[key])
            r0 = r1

    for l in range(NL):
        cast_w(P["w_in"][l], wb_in[l], ('wb_in', l))
        cast_w(P["w_branch"][l], wb_br[l], ('wb_br', l))
        cast_w(P["w_o"][l], wb_o[l], ('wb_o', l))
        cast_w(P["w_up"][l], wb_up[l], ('wb_up', l))
        cast_w(P["w_down"][l], wb_dn[l], ('wb_dn', l))

    cst_f = sb("cst_f", [128, 384], F32)
    d_c = S.dsem()
    S.dma('sp', d_c, cst_f[:], cst, W=['cst_f'])
    ident_f = cst_f[:, 0:128]
    tri_f = cst_f[:, 128:256]
    ones_f = cst_f[:, 256:384]
    ident_b = sb("ident_b", [128, 128], BF16)
    mask4 = sb("mask4", [128, 4, 128], BF16)
    S.op('dve', 'tensor_copy', ['cst_f'], ['ident_b'], out=ident_b[:], in_=ident_f)
    for h in range(4):
        S.op('dve', 'tensor_copy', ['cst_f'], ['mask4'], out=mask4[:, h, :], in_=tri_f)

    xts = [sb("xt0", [128, 4, D], F32), sb("xt1", [128, 4, D], F32)]
    junk = sb("junk", [128, D], F32)
    hTs = [sb("hT0", [128, 8, 512], BF16), sb("hT1", [128, 8, 512], BF16)]
    NW = 3
    wsl = [sb("wsl%d" % i, [128, 8, 512], BF16) for i in range(NW)]
    d_wsl = [S.dsem() for _ in range(NW)]
    d_xs = [S.dsem(), S.dsem()]
    cur = dict(hi=0)
    st1 = sb("st1", [128, 16], F32)
    bfe = sb("bfe", [128, 64], F32)
    g1 = sb("g1", [128, 8], F32)
    g2 = sb("g2", [128, 8], F32)
    sm = sb("sm", [128, 160], F32)
    yTs = sb("yTs", [128, 4, 512], BF16)
    ytok = sb("ytok", [128, 512], BF16)
    Fall = sb("Fall", [128, NB, 4], F32)
    rI = sb("rI", [128, NT, 4], F32)
    d_p = S.dsem()
    d_y = S.dsem()
    bb = sb("bb", [128, 4108], F32)
    mif = sb("mif", [128, 4, 8], F32)
    Cst = sb("Cst", [128, 4, 130], F32)
    Cbf = sb("Cbf", [128, 4, 130], BF16)
    gbc = sb("gbc", [128, 512], F32)
    gqk = sb("gqk", [128, 2, 128], F32)
    Fcar = sb("Fcar", [128, 4], F32)
    rxy = sb("rxy", [128, 4, 515], F32)
    lcar = sb("lcar", [128, 4], F32)
    lpar = sb("lpar", [128, 64], F32)
    wbd = sb("wbd", [128, 2, 4, 128], BF16)
    wal_f = sb("wal_f", [16, 256], F32)
    wal = sb("wal", [16, 256], BF16)
    gpar = sb("gpar", [128, 8], F32)
    Sst = sb("Sst", [128, 2, 128], F32)
    Sbf = sb("Sbf", [128, 2, 128], BF16)
    d_f = S.dsem()
    d_f2 = S.dsem()
    bigB = sb("bigB", [128, 20480], BF16)
    bigF = sb("bigF", [128, 8192], F32)

    def vB(off, shape):
        n = 1
        for s_ in shape[1:]:
            n *= s_
        v = bigB[:, off:off + n]
        if len(shape) == 3:
            v = v.rearrange("p (a b) -> p a b", a=shape[1])
        elif len(shape) == 4:
            v = v.rearrange("p (a b c) -> p a b c", a=shape[1], b=shape[2])
        return v

    def vF(off, shape):
        n = 1
        for s_ in shape[1:]:
            n *= s_
        v = bigF[:, off:off + n]
        if len(shape) == 3:
            v = v.rearrange("p (a b) -> p a b", a=shape[1])
        return v

    PJ = [ps("PJ0", [128, 512], F32), ps("PJ1", [128, 512], F32)]
    PT = ps("PT", [128, 512], F32)
    PB = ps("PB", [128, 1024], BF16)
    PA = ps("PA", [128, 512], F32)
    PO = [ps("PO0", [128, 512], F32), ps("PO1", [128, 512], F32)]
    PS = ps("PS", [128, 512], F32)

    state = dict(wslot=0, pj=0)

    def load_w(src_ap, ncols, rkey, kch=8):
        i = state['wslot']
        state['wslot'] = (i + 1) % NW
        S.dma('sp', d_wsl[i], wsl[i][:, 0:kch, 0:ncols], src_ap.rearrange("(c p) n -> p c n", p=128),
              R=[rkey], W=[('wsl', i)])
        return i

    def next_pj():
        i = state['pj']
        state['pj'] ^= 1
        return i

    def norm_from_xt(gt, gkey, xi, hi):
        xt = xts[xi]
        xk = ('xt', xi)
        hT = hTs[hi]
        for j in range(4):
            S.op('act', 'activation', [xk], ['junk', ('st1', j)], out=junk[:], in_=xt[:, j, :], func=AF.Square,
                 accum_out=st1[:, j:j + 1])
        S.op('act', 'activation', [('st1', j) for j in range(4)], ['st1b'], out=st1[:, 4:8], in_=st1[:, 0:4],
             func=AF.Sqrt, scale=1.0 / D, bias=EPS)
        S.op('dve', 'reciprocal', ['st1b'], ['st1c'], out=st1[:, 8:12], in_=st1[:, 4:8])
        for j in range(4):
            S.op('dve', 'tensor_scalar', [xk, 'st1c'], ['junk'], out=junk[:], in0=xt[:, j, :],
                 scalar1=st1[:, 8 + j:9 + j], scalar2=None, op0=ALU.mult)
            for half in range(2):
                for q in range(4):
                    c = half * 4 + q
                    S.op('pe', 'transpose', ['junk', 'cst_f'], ['PT'], out=PT[:, q * 128:(q + 1) * 128],
                         in_=junk[:, c * 128:(c + 1) * 128], identity=ident_f)
                for q in range(4):
                    c = half * 4 + q
                    S.op('dve', 'tensor_scalar', [gkey], ['PT', ('hT', hi, c)], out=hT[:, c, j * 128:(j + 1) * 128],
                         in0=PT[:, q * 128:(q + 1) * 128], scalar1=gt[:, c:c + 1], scalar2=None, op0=ALU.mult)

    def load_x(src_dram, t, xi):
        S.dma('sp', d_xs[xi], xts[xi][:], src_dram[t * 512:(t + 1) * 512, :].rearrange("(j p) d -> p j d", p=128),
              R=[('xs', id(src_dram))], W=[('xt', xi)])

    def proj_feat(wsrc, wkey, c0, ncols, evac):
        done = 0
        while done < ncols:
            n = min(512, ncols - done)
            wi = load_w(wsrc[:, c0 + done:c0 + done + n], n, wkey)
            mc = 0
            while mc * 128 < n:
                m = min(128, n - mc * 128)
                pj = next_pj()
                for kc in range(8):
                    S.op('pe', 'matmul', [('wsl', wi), ('hT', cur['hi'], kc)], [('PJ', pj)], PJ[pj][0:m, :],
                         lhsT=wsl[wi][:, kc, mc * 128:mc * 128 + m], rhs=hTs[cur['hi']][:, kc, :], start=(kc == 0), stop=(kc == 7))
                evac((done + mc * 128) // 128, m, PJ[pj], ('PJ', pj))
                mc += 1
            done += n

    def proj_tok(wsrc, wkey, c0, ncols, evac):
        done = 0
        while done < ncols:
            n = min(512, ncols - done)
            wi = load_w(wsrc[:, c0 + done:c0 + done + n], n, wkey)
            for j in range(4):
                pj = next_pj()
                for kc in range(8):
                    S.op('pe', 'matmul', [('wsl', wi), ('hT', cur['hi'], kc)], [('PJ', pj)], PJ[pj][:, 0:n],
                         lhsT=hTs[cur['hi']][:, kc, j * 128:(j + 1) * 128], rhs=wsl[wi][:, kc, 0:n], start=(kc == 0), stop=(kc == 7))
                evac(j, done, n, PJ[pj], ('PJ', pj))
            done += n

    def yT_store(br, t):
        S.dma('pool', d_y, yT[br * 4:(br + 1) * 4, :, t * 512:(t + 1) * 512].rearrange("c p s -> p c s"),
              yTs[:], R=['yTs'], W=[('yT', br)])

    def tok_to_yTs(j):
        for c in range(4):
            S.op('pe', 'transpose', ['ytok', 'ident_b'], ['PB'], out=PB[:, c * 128:(c + 1) * 128],
                 in_=ytok[:, c * 128:(c + 1) * 128], identity=ident_b[:])
        S.op('act', 'activation', [], ['PB', 'yTs'], out=yTs[:, :, j * 128:(j + 1) * 128],
             in_=PB[:, 0:512].rearrange("p (c s) -> p c s", c=4), func=AF.Copy)

    for l in range(NL):
        x_src = x_in if l == 0 else xs[(l - 1) % 2]
        x_dst = out if l == NL - 1 else xs[l % 2]
        WIN = wb_in[l]
        kin = ('wb_in', l)
        S.barrier()
        bi = P["b_in"][l]
        S.dma('sp', d_p, g1[:], P["norm1_g"][l].rearrange("(c p) -> p c", p=128), W=['g1'])
        S.dma('sp', d_p, g2[:], P["norm2_g"][l].rearrange("(c p) -> p c", p=128), W=['g2'])
        S.dma('sp', d_p, bb[:, 0:1544], bi[512:2056].partition_broadcast(128), W=['bb'])
        S.dma('sp', d_p, bb[:, 1544:3084], bi[2056:3596].partition_broadcast(128), W=['bb'])
        S.dma('sp', d_p, bb[:, 3084:4108], bi[5132:6156].partition_broadcast(128), W=['bb'])
        S.dma('sp', d_p, bfe[:, 0:8], bi[0:1024].rearrange("(c p) -> p c", p=128), W=['bfe'])
        S.dma('sp', d_p, bfe[:, 8:16], bi[C_RX:C_RX + 1024].rearrange("(c p) -> p c", p=128), W=['bfe'])
        S.dma('sp', d_p, bfe[:, 16:20], bi[C_GQ:C_GQ + 512].rearrange("(c p) -> p c", p=128), W=['bfe'])
        S.dma('sp', d_p, bfe[0:16, 20:21], bi[C_GA:C_GA + 16].rearrange("(p c) -> p c", c=1), W=['bfe'])
        S.dma('sp', d_p, bfe[:, 21:53], bi[C_GT:C_GT + 4096].rearrange("(c p) -> p c", p=128), W=['bfe'])
        S.dma('sp', d_p, gqk[:, 0, :], P["fx_qnorm_g"][l].partition_broadcast(128), W=['gqk'])
        S.dma('sp', d_p, gqk[:, 1, :], P["fx_knorm_g"][l].partition_broadcast(128), W=['gqk'])
        for k_ in range(4):
            S.dma('sp', d_p, lpar[:, 4 * k_:4 * k_ + 4], P["lru_conv_w"][l, k_].rearrange("(c p) -> p c", p=128), W=['lpar'])
        for i_, nm in enumerate(["lru_conv_b", "lru_ba", "lru_bx", "lru_lambda"]):
            S.dma('sp', d_p, lpar[:, 16 + 4 * i_:20 + 4 * i_], P[nm][l].rearrange("(c p) -> p c", p=128), W=['lpar'])
        LP = ['lpar', 'lpard', 'lpare']
        wbd_f = junk[:, :].rearrange("p (w c k) -> p w c k", w=2, c=4)
        S.op('pool', 'memset', [], ['junk'], junk[:], 0.0)
        for wi_, nm in enumerate(["lru_wa", "lru_wx"]):
            for g in range(8):
                c, po = g // 2, (g % 2) * 64
                S.dma('sp', d_p, wbd_f[po:po + 64, wi_, c, po:po + 64], P[nm][l, g], R=[], W=['junk'])
        S.dma('sp', d_p, wal_f[:], P["gla_w_alpha"][l], W=['wal_f'])
        S.dma('sp', d_p, gpar[:, 0:2], P["gla_b_alpha"][l].rearrange("(c p) -> p c", p=128), W=['gpar'])
        S.barrier()
        S.op('pool', 'tensor_scalar', ['gqk'], ['gqk'], out=gqk[:, 0, :], in0=gqk[:, 0, :], scalar1=float(128 ** -0.5),
             scalar2=None, op0=ALU.mult)
        S.op('act', 'activation', ['lpar'], ['lparb'], out=lpar[:, 40:44], in_=lpar[:, 28:32], func=AF.Exp, scale=-1.0)
        S.op('act', 'activation', ['lparb'], ['lparc'], out=lpar[:, 44:48], in_=lpar[:, 40:44], func=AF.Ln, bias=1.0)
        S.op('dve', 'tensor_scalar', ['lparc'], ['lpard'], out=lpar[:, 32:36], in0=lpar[:, 44:48], scalar1=-8.0,
             scalar2=None, op0=ALU.mult)
        S.op('dve', 'tensor_scalar', ['lparc'], ['lpare'], out=lpar[:, 36:40], in0=lpar[:, 44:48], scalar1=-16.0,
             scalar2=None, op0=ALU.mult)
        S.op('pool', 'tensor_copy', ['junk'], ['wbd'], out=wbd[:], in_=wbd_f)
        S.op('pool', 'tensor_copy', ['wal_f'], ['wal'], out=wal[:], in_=wal_f[:])
        S.op('pool', 'tensor_scalar', ['gpar'], ['gparb'], out=gpar[:, 2:4], in0=gpar[:, 0:2], scalar1=-1.0,
             scalar2=None, op0=ALU.mult)
        for tl, key in [(Cst, 'Cst'), (Cbf, 'Cbf'), (Sst, 'Sst'), (Sbf, 'Sbf'), (lcar, 'lcar'), (Fcar, 'Fcar'), (rxy, 'rxy')]:
            S.op('pool', 'memset', [], [key], tl[:], 0.0)
        S.barrier()

        load_x(x_src, 0, 0)
        norm_from_xt(g1, 'g1', 0, 0)
        for t in range(NT):
            cur['hi'] = t % 2
            if t + 1 < NT:
                load_x(x_src, t + 1, (t + 1) % 2)

            S.barrier()
            mqT = vB(0, [128, 4, 512])
            mkT = vB(2048, [128, 4, 512])
            mk_tok = vB(4096, [128, 4, 512])
            mv_aug = vB(6144, [128, 4, 4, 130])
            STb = vB(8224, [128, 4, 128])
            kd = vB(8736, [128, 4, 128])
            gs_ml = vF(0, [128, 4, 512])
            ytmp = vF(2048, [128, 4, 130])
            S.op('pool', 'memset', [], ['mv_aug'], mv_aug, 1.0)
            S.dma('sp', d_p, gbc[:], P["ml_norm_g"][l].partition_broadcast(128), W=['gbc'])

            def ev_mq(mc, m, pt_, pk):
                if mc < 4:
                    S.op('dve', 'tensor_scalar', ['bfe'], [pk, ('mqk', mc)], out=mqT[:, mc, :], in0=pt_[:, :],
                         scalar1=bfe[:, mc:mc + 1], scalar2=float(128 ** -0.5), op0=ALU.add, op1=ALU.mult)
                else:
                    S.op('act', 'activation', ['bfe'], [pk, ('mqk', mc)], out=mkT[:, mc - 4, :], in_=pt_[:, :],
                         func=AF.Identity, bias=bfe[:, mc:mc + 1])
            proj_feat(WIN, kin, C_MQ, 1024, ev_mq)

            def ev_mtok(j, off, n, pt_, pk):
                if off == 0:
                    S.op('dve', 'tensor_tensor', ['bb'], [pk, 'mk_tok'], out=mk_tok[:, j, :], in0=pt_[:, 0:512],
                         in1=bb[:, 0:512], op=ALU.add)
                elif off == 512:
                    S.op('dve', 'tensor_tensor', ['bb'], [pk, 'mv_aug'], out=mv_aug[:, j, :, 0:128],
                         in0=pt_[:, 0:512].rearrange("p (h e) -> p h e", h=4),
                         in1=bb[:, 512:1024].rearrange("p (h e) -> p h e", h=4), op=ALU.add)
                elif off == 1024:
                    S.op('dve', 'tensor_tensor', ['bb'], [pk, 'gs_ml'], out=gs_ml[:, j, :], in0=pt_[:, 0:512],
                         in1=bb[:, 1024:1536], op=ALU.add)
                    S.op('act', 'activation', ['gs_ml'], ['gs_ml'], out=gs_ml[:, j, :], in_=gs_ml[:, j, :], func=AF.Sigmoid)
                    S.op('pool', 'tensor_tensor', ['gs_ml', 'gbc'], ['gs_ml'], out=gs_ml[:, j, :], in0=gs_ml[:, j, :],
                         in1=gbc[:], op=ALU.mult)
                else:
                    S.op('dve', 'tensor_tensor', ['bb'], [pk, 'mif'], out=mif[:, j, :], in0=pt_[:, 0:8],
                         in1=bb[:, 1536:1544], op=ALU.add)
            proj_tok(WIN, kin, C_MK, 1544, ev_mtok)

            S.op('act', 'activation', ['mif'], ['sm_a'], out=sm[:, 0:16].rearrange("p (j h) -> p j h", h=4),
                 in_=mif[:, :, 4:8], func=AF.Exp, scale=-1.0)
            S.op('act', 'activation', ['sm_a'], ['sm_a'], out=sm[:, 0:16], in_=sm[:, 0:16], func=AF.Ln, bias=1.0)
            S.op('pe', 'matmul', ['sm_a', 'cst_f'], ['PA'], PA[:, 0:16], lhsT=tri_f, rhs=sm[:, 0:16], start=True, stop=True)
            S.op('pe', 'matmul', ['sm_a', 'cst_f'], ['PA'], PA[:, 16:32], lhsT=ones_f, rhs=sm[:, 0:16], start=True, stop=True)
            S.op('dve', 'tensor_copy', [], ['PA', 'sm_b'], out=sm[:, 16:48], in_=PA[:, 0:32])
            S.op('act', 'activation', ['sm_b'], ['sm_c'], out=sm[:, 48:80], in_=sm[:, 16:48], func=AF.Exp, scale=-1.0)
            S.op('dve', 'tensor_tensor', ['sm_b', 'mif'], ['sm_d'], out=sm[:, 96:112].rearrange("p (j h) -> p j h", h=4),
                 in0=sm[:, 16:32].rearrange("p (j h) -> p j h", h=4), in1=mif[:, :, 0:4], op=ALU.add)
            S.op('dve', 'tensor_tensor', ['sm_d', 'sm_b'], ['sm_e'], out=sm[:, 80:96], in0=sm[:, 96:112], in1=sm[:, 32:48],
                 op=ALU.subtract)
            S.op('act', 'activation', ['sm_e'], ['sm_e'], out=sm[:, 80:96], in_=sm[:, 80:96], func=AF.Exp)
            S.op('act', 'activation', ['sm_d'], ['sm_d'], out=sm[:, 96:112], in_=sm[:, 96:112], func=AF.Exp)
            for j in range(4):
                js = slice(j * 128, (j + 1) * 128)
                for h in range(4):
                    S.op('pe', 'matmul', [('mqk', h), ('mqk', 4 + h)], ['PA'], PA[:, h * 128:(h + 1) * 128],
                         lhsT=mkT[:, h, js], rhs=mqT[:, h, js], start=True, stop=True)
                for h in range(4):
                    col = 96 + j * 4 + h
                    S.op('dve', 'scalar_tensor_tensor', ['sm_d', 'mask4'], ['PA', 'STb'], out=STb[:, h, :],
                         in0=PA[:, h * 128:(h + 1) * 128], scalar=sm[:, col:col + 1], in1=mask4[:, h, :],
                         op0=ALU.mult, op1=ALU.mult)
                for h in range(4):
                    pk = ('PO', h // 2)
                    o_ap = PO[h // 2][:, (h % 2) * 130:(h % 2) * 130 + 130]
                    S.op('pe', 'matmul', ['STb', 'mv_aug'], [pk], o_ap, lhsT=STb[:, h, :], rhs=mv_aug[:, j, h, :],
                         start=(h % 2 == 0), stop=False, skip_group_check=True)
                    S.op('pe', 'matmul', [('mqk', h), 'Cbf'], [pk], o_ap, lhsT=mqT[:, h, js], rhs=Cbf[:, h, :],
                         start=False, stop=True, skip_group_check=True)
                for h in range(4):
                    col = 80 + j * 4 + h
                    S.op('pool', 'tensor_scalar', ['mk_tok', 'sm_e'], ['kd'], out=kd[:, h, :],
                         in0=mk_tok[:, j, h * 128:(h + 1) * 128], scalar1=sm[:, col:col + 1], scalar2=None, op0=ALU.mult)
                for h in range(4):
                    S.op('pe', 'matmul', ['kd', 'mv_aug'], ['PS'], PS[:, 0:130], lhsT=kd[:, h, :], rhs=mv_aug[:, j, h, :],
                         start=True, stop=True)
                    col = 64 + j * 4 + h
                    S.op('dve', 'scalar_tensor_tensor', ['sm_c'], ['PS', 'Cst'], out=Cst[:, h, :], in0=Cst[:, h, :],
                         scalar=sm[:, col:col + 1], in1=PS[:, 0:130], op0=ALU.mult, op1=ALU.add)
                for hp in range(2):
                    S.op('act', 'activation', [], [('PO', hp), 'ytmp'], out=ytmp[:, 2 * hp:2 * hp + 2, :],
                         in_=PO[hp][:, 0:260].rearrange("p (h e) -> p h e", h=2), func=AF.Copy)
                S.op('pool', 'tensor_copy', ['Cst'], ['Cbf'], out=Cbf[:], in_=Cst[:])
                eb4 = sm[:, 48 + j * 4:52 + j * 4]
                S.op('dve', 'tensor_tensor', ['ytmp', 'sm_c'], ['sm_f'], out=sm[:, 112:116], in0=ytmp[:, :, 128], in1=eb4,
                     op=ALU.mult)
                S.op('act', 'activation', ['sm_f'], ['sm_f'], out=sm[:, 112:116], in_=sm[:, 112:116], func=AF.Abs)
                S.op('dve', 'tensor_scalar', ['sm_f'], ['sm_f'], out=sm[:, 112:116], in0=sm[:, 112:116], scalar1=1.0,
                     scalar2=None, op0=ALU.max)
                S.op('dve', 'reciprocal', ['sm_f'], ['sm_f'], out=sm[:, 112:116], in_=sm[:, 112:116])
                S.op('dve', 'tensor_tensor', ['sm_f', 'sm_c'], ['sm_f'], out=sm[:, 112:116], in0=sm[:, 112:116], in1=eb4,
                     op=ALU.mult)
                for h in range(4):
                    S.op('dve', 'tensor_scalar', ['sm_f', 'ytmp'], ['ytmp'], out=ytmp[:, h, 0:128], in0=ytmp[:, h, 0:128],
                         scalar1=sm[:, 112 + h:113 + h], scalar2=None, op0=ALU.mult)
                for h in range(4):
                    S.op('act', 'activation', ['ytmp'], ['junk', ('sm_g', h)], out=junk[:, 0:128], in_=ytmp[:, h, 0:128],
                         func=AF.Square, accum_out=sm[:, 116 + h:117 + h])
                S.op('act', 'activation', [('sm_g', h) for h in range(4)], ['sm_h'], out=sm[:, 120:124], in_=sm[:, 116:120],
                     func=AF.Sqrt, scale=1.0 / 128, bias=EPS)
                S.op('dve', 'reciprocal', ['sm_h'], ['sm_h'], out=sm[:, 120:124], in_=sm[:, 120:124])
                for h in range(4):
                    S.op('dve', 'scalar_tensor_tensor', ['sm_h', 'ytmp', 'gs_ml'], ['ytok'], out=ytok[:, h * 128:(h + 1) * 128],
                         in0=ytmp[:, h, 0:128], scalar=sm[:, 120 + h:121 + h], in1=gs_ml[:, j, h * 128:(h + 1) * 128],
                         op0=ALU.mult, op1=ALU.mult)
                tok_to_yTs(j)
            yT_store(0, t)

            S.barrier()
            fqn = vB(0, [128, 512])
            fT = vB(512, [128, 4, 512])
            fv_aug = vB(2560, [128, 4, 4, 130])
            fq_t = vF(0, [128, 512])
            S.op('pool', 'memset', [], ['fv_aug'], fv_aug, 1.0)

            def ev_ftok(j, off, n, pt_, pk):
                if off < 1024:
                    w = off // 512
                    S.op('dve', 'tensor_tensor', ['bb'], [pk, 'fq_t'], out=fq_t, in0=pt_[:, 0:512],
                         in1=bb[:, 1544 + off:1544 + off + 512], op=ALU.add)
                    for h in range(4):
                        S.op('act', 'activation', ['fq_t'], ['junk', ('sm_i', h)], out=junk[:, 0:128],
                             in_=fq_t[:, h * 128:(h + 1) * 128], func=AF.Square, accum_out=sm[:, 128 + h:129 + h])
                    S.op('act', 'activation', [('sm_i', h) for h in range(4)], ['sm_j'], out=sm[:, 132:136],
                         in_=sm[:, 128:132], func=AF.Sqrt, scale=1.0 / 128, bias=EPS)
                    S.op('dve', 'reciprocal', ['sm_j'], ['sm_j'], out=sm[:, 132:136], in_=sm[:, 132:136])
                    for h in range(4):
                        S.op('dve', 'scalar_tensor_tensor', ['sm_j', 'fq_t', 'gqk'], ['fqn'], out=fqn[:, h * 128:(h + 1) * 128],
                             in0=fq_t[:, h * 128:(h + 1) * 128], scalar=sm[:, 132 + h:133 + h], in1=gqk[:, w, :],
                             op0=ALU.mult, op1=ALU.mult)
                    for h in range(4):
                        S.op('pe', 'transpose', ['fqn', 'ident_b'], ['PB'], out=PB[:, h * 128:(h + 1) * 128],
                             in_=fqn[:, h * 128:(h + 1) * 128], identity=ident_b[:])
                    S.op('act', 'activation', [], ['PB', 'fT'], out=fT[:, :, j * 128:(j + 1) * 128],
                         in_=PB[:, 0:512].rearrange("p (c s) -> p c s", c=4), func=AF.Copy)
                    if j == 3:
                        dst = QTd if w == 0 else KTd
                        S.dma('pool', d_f, dst[:, :, t * 512:(t + 1) * 512].rearrange("h p s -> p h s"), fT,
                              R=['fT'], W=[('QK', w)])
                elif off == 1024:
                    S.op('dve', 'tensor_tensor', ['bb'], [pk, 'fv_aug'], out=fv_aug[:, :, j, 0:128],
                         in0=pt_[:, 0:512].rearrange("p (h e) -> p h e", h=4),
                         in1=bb[:, 1544 + 1024:1544 + 1536].rearrange("p (h e) -> p h e", h=4), op=ALU.add)
                    if j == 3:
                        S.dma('pool', d_f2, Vd[:, :, t * 4:(t + 1) * 4, :].rearrange("h p j e -> p h j e"), fv_aug,
                              R=['fv_aug'], W=['Vd'])
                else:
                    S.op('dve', 'tensor_tensor', ['bb'], [pk, 'sm_k'], out=sm[:, 136:140], in0=pt_[:, 0:4],
                         in1=bb[:, 1544 + 1536:1544 + 1540], op=ALU.add)
                    S.op('act', 'activation', ['sm_k'], ['sm_k'], out=sm[:, 136:140], in_=sm[:, 136:140], func=AF.Exp, scale=-1.0)
                    S.op('act', 'activation', ['sm_k'], ['sm_k'], out=sm[:, 136:140], in_=sm[:, 136:140], func=AF.Ln, bias=1.0)
                    S.op('pe', 'matmul', ['sm_k', 'cst_f'], ['PA'], PA[:, 0:4], lhsT=tri_f, rhs=sm[:, 136:140], start=True, stop=True)
                    S.op('pe', 'matmul', ['sm_k', 'cst_f'], ['PA'], PA[:, 4:8], lhsT=ones_f, rhs=sm[:, 136:140], start=True, stop=True)
                    if j == 0:
                        S.op('dve', 'tensor_scalar', ['Fcar'], ['rI'], out=rI[:, t, :], in0=Fcar[:], scalar1=-1.0, scalar2=None,
                             op0=ALU.mult)
                    S.op('dve', 'tensor_tensor', ['Fcar'], ['PA', 'Fall'], out=Fall[:, t * 4 + j, :], in0=PA[:, 0:4], in1=Fcar[:],
                         op=ALU.add)
                    S.op('dve', 'tensor_tensor', [], ['PA', 'Fcar'], out=Fcar[:], in0=PA[:, 4:8], in1=Fcar[:], op=ALU.add)
            proj_tok(WIN, kin, C_FQ, 1540, ev_ftok)

            S.barrier()
            ryT = vF(0, [128, 4, 512])
            xc = vF(2048, [128, 4, 512])
            lr = vF(4096, [128, 512])
            li = vF(4608, [128, 512])
            la_ = vF(5120, [128, 512])
            lu = vF(5632, [128, 512])
            lh = vF(6144, [128, 512])
            xcb = vB(0, [128, 4, 512])

            def ev_rx(mc, m, pt_, pk):
                if mc < 4:
                    S.op('act', 'activation', ['bfe'], [pk, 'rxy'], out=rxy[:, mc, 3:515], in_=pt_[:, :], func=AF.Identity,
                         bias=bfe[:, 8 + mc:9 + mc])
                else:
                    S.op('act', 'activation', ['bfe'], [pk, 'ryT'], out=ryT[:, mc - 4, :], in_=pt_[:, :], func=AF.Identity,
                         bias=bfe[:, 8 + mc:9 + mc])
            proj_feat(WIN, kin, C_RX, 1024, ev_rx)
            for c in range(4):
                S.op('dve', 'tensor_scalar', ['rxy'] + LP, ['xc'], out=xc[:, c, :], in0=rxy[:, c, 0:512],
                     scalar1=lpar[:, c:c + 1], scalar2=lpar[:, 16 + c:17 + c], op0=ALU.mult, op1=ALU.add)
                for k in range(1, 4):
                    S.op('dve', 'scalar_tensor_tensor', ['rxy'] + LP, ['xc'], out=xc[:, c, :], in0=rxy[:, c, k:k + 512],
                         scalar=lpar[:, 4 * k + c:4 * k + c + 1], in1=xc[:, c, :], op0=ALU.mult, op1=ALU.add)
            S.op('pool', 'tensor_copy', ['xc'], ['xcb'], out=xcb, in_=xc)
            S.op('pool', 'tensor_copy', [], ['rxy'], out=rxy[:, :, 0:3], in_=rxy[:, :, 512:515])
            for c in range(4):
                pj = next_pj()
                S.op('pe', 'matmul', ['wbd', 'xcb'], [('PJ', pj)], PJ[pj][:, :], lhsT=wbd[:, 0, c, :], rhs=xcb[:, c, :],
                     start=True, stop=True)
                S.op('act', 'activation', LP, [('PJ', pj), 'lr'], out=lr, in_=PJ[pj][:, :], func=AF.Sigmoid,
                     bias=lpar[:, 20 + c:21 + c])
                pj = next_pj()
                S.op('pe', 'matmul', ['wbd', 'xcb'], [('PJ', pj)], PJ[pj][:, :], lhsT=wbd[:, 1, c, :], rhs=xcb[:, c, :],
                     start=True, stop=True)
                S.op('act', 'activation', LP, [('PJ', pj), 'li'], out=li, in_=PJ[pj][:, :], func=AF.Sigmoid,
                     bias=lpar[:, 24 + c:25 + c])
                S.op('act', 'activation', ['lr'] + LP, ['la_'], out=la_, in_=lr, func=AF.Exp, scale=lpar[:, 32 + c:33 + c])
                S.op('act', 'activation', ['lr'] + LP, ['lu'], out=lu, in_=lr, func=AF.Exp, scale=lpar[:, 36 + c:37 + c])
                S.op('dve', 'tensor_scalar', ['lu'], ['lu'], out=lu, in0=lu, scalar1=-1.0, scalar2=1.0, op0=ALU.mult, op1=ALU.add)
                S.op('dve', 'tensor_scalar', ['lu'], ['lu'], out=lu, in0=lu, scalar1=0.0, scalar2=None, op0=ALU.max)
                S.op('act', 'activation', ['lu'], ['lu'], out=lu, in_=lu, func=AF.Sqrt)
                S.op('dve', 'tensor_tensor', ['li', 'xc'], ['li'], out=li, in0=li, in1=xc[:, c, :], op=ALU.mult)
                S.op('dve', 'tensor_tensor', ['li', 'lu'], ['lu'], out=lu, in0=lu, in1=li, op=ALU.mult)
                S.op('dve', 'tensor_tensor_scan', ['la_', 'lu', 'lcar'], ['lh'], out=lh, data0=la_, data1=lu,
                     initial=lcar[:, c:c + 1], op0=ALU.mult, op1=ALU.add)
                S.op('pool', 'tensor_copy', ['lh'], ['lcar'], out=lcar[:, c:c + 1], in_=lh[:, 511:512])
                yv = ryT[:, c, :]
                S.op('pool', 'tensor_tensor', ['ryT'], ['li'], out=li, in0=yv, in1=yv, op=ALU.mult)
                S.op('dve', 'tensor_scalar', ['li'], ['li'], out=li, in0=li, scalar1=0.044715, scalar2=1.0, op0=ALU.mult,
                     op1=ALU.add)
                S.op('dve', 'tensor_tensor', ['li', 'ryT'], ['li'], out=li, in0=li, in1=yv, op=ALU.mult)
                S.op('act', 'activation', ['li'], ['li'], out=li, in_=li, func=AF.Sigmoid, scale=1.5957691216)
                S.op('dve', 'tensor_tensor', ['li', 'ryT'], ['li'], out=li, in0=li, in1=yv, op=ALU.mult)
                S.op('dve', 'tensor_tensor', ['li', 'lh'], ['yTs'], out=yTs[:, c, :], in0=li, in1=lh, op=ALU.mult)
            yT_store(2, t)

            S.barrier()
            gqT = vF(0, [128, 2, 512])
            gkT = vF(1024, [128, 2, 512])
            gsp = vF(2048, [128, 2, 512])
            gcs = vF(3072, [128, 2, 512])
            geq = vF(4096, [128, 2, 512])
            gek = vF(5120, [128, 2, 512])
            gsil = vF(6144, [128, 4, 512])
            qtT = vB(0, [128, 2, 512])
            ktT = vB(1024, [128, 2, 512])
            kt_tok = vB(2048, [128, 256])
            gv_t = vB(2304, [128, 4, 512])
            Ab = vB(4352, [128, 4, 128])
            gaT = vB(4864, [128, 512])
            S.dma('sp', d_p, gbc[:], P["gla_norm_g"][l].partition_broadcast(128), W=['gbc'])

            def ev_gq(mc, m, pt_, pk):
                dst = gqT if mc < 2 else gkT
                S.op('act', 'activation', ['bfe'], [pk, ('gqk_', mc)], out=dst[:, mc % 2, :], in_=pt_[:, :], func=AF.Identity,
                     bias=bfe[:, 16 + mc:17 + mc])
            proj_feat(WIN, kin, C_GQ, 512, ev_gq)

            def ev_ga(mc, m, pt_, pk):
                S.op('act', 'activation', ['bfe'], [pk, 'gaT'], out=gaT[0:16, :], in_=pt_[0:16, :], func=AF.Identity,
                     bias=bfe[0:16, 20:21])
            proj_feat(WIN, kin, C_GA, 16, ev_ga)

            def ev_gtok(j, off, n, pt_, pk):
                if off == 0:
                    S.op('dve', 'tensor_tensor', ['bb'], [pk, 'gv_t'], out=gv_t[:, j, :], in0=pt_[:, 0:512],
                         in1=bb[:, 3084:3596], op=ALU.add)
                else:
                    S.op('dve', 'tensor_tensor', ['bb'], [pk, 'gsil'], out=gsil[:, j, :], in0=pt_[:, 0:512],
                         in1=bb[:, 3596:4108], op=ALU.add)
                    S.op('act', 'activation', ['gsil'], ['junk'], out=junk[:, 0:512], in_=gsil[:, j, :], func=AF.Sigmoid)
                    S.op('dve', 'tensor_tensor', ['junk'], ['gsil'], out=gsil[:, j, :], in0=gsil[:, j, :],
                         in1=junk[:, 0:512], op=ALU.mult)
                    S.op('pool', 'tensor_tensor', ['gbc'], ['gsil'], out=gsil[:, j, :], in0=gsil[:, j, :], in1=gbc[:],
                         op=ALU.mult)
            proj_tok(WIN, kin, C_GV, 1024, ev_gtok)
            for c in range(2):
                pj = next_pj()
                S.op('pe', 'matmul', ['wal', 'gaT'], [('PJ', pj)], PJ[pj][:, :], lhsT=wal[:, c * 128:(c + 1) * 128],
                     rhs=gaT[0:16, :], start=True, stop=True)
                S.op('act', 'activation', ['gparb'], [('PJ', pj), 'gsp'], out=gsp[:, c, :], in_=PJ[pj][:, :], func=AF.Exp,
                     scale=-1.0, bias=gpar[:, 2 + c:3 + c])
                S.op('act', 'activation', ['gsp'], ['gsp'], out=gsp[:, c, :], in_=gsp[:, c, :], func=AF.Ln, bias=1.0)
                for j in range(4):
                    js = slice(j * 128, (j + 1) * 128)
                    S.op('dve', 'tensor_tensor_scan', ['gsp', 'cst_f'], ['gcs'], out=gcs[:, c, js], data0=ones_f,
                         data1=gsp[:, c, js], initial=0.0, op0=ALU.mult, op1=ALU.add)
                S.op('act', 'activation', ['gcs'], ['geq'], out=geq[:, c, :], in_=gcs[:, c, :], func=AF.Exp, scale=-1.0 / 16)
                S.op('act', 'activation', ['gcs'], ['gek'], out=gek[:, c, :], in_=gcs[:, c, :], func=AF.Exp, scale=1.0 / 16)
                S.op('dve', 'scalar_tensor_tensor', [('gqk_', c), 'geq'], ['qtT'], out=qtT[:, c, :], in0=gqT[:, c, :],
                     scalar=0.125, in1=geq[:, c, :], op0=ALU.mult, op1=ALU.mult)
                S.op('dve', 'tensor_tensor', [('gqk_', 2 + c), 'gek'], ['ktT'], out=ktT[:, c, :], in0=gkT[:, c, :],
                     in1=gek[:, c, :], op=ALU.mult)
            if t + 1 < NT:
                norm_from_xt(g1, 'g1', (t + 1) % 2, (t + 1) % 2)
            PAR = [PA, PS]
            PARK = ['PA', 'PS']
            for j in range(4):
                js = slice(j * 128, (j + 1) * 128)
                for h in range(4):
                    c, par = h // 2, h % 2
                    po_ = par * 64
                    S.op('pe', 'matmul', ['ktT', 'qtT'], [PARK[par]], PAR[par][:, c * 128:(c + 1) * 128],
                         lhsT=ktT[po_:po_ + 64, c, js], rhs=qtT[po_:po_ + 64, c, js], start=True, stop=True)
                for par in range(2):
                    S.op('dve', 'tensor_tensor', ['mask4'], [PARK[par], 'Ab'], out=Ab[:, 2 * par:2 * par + 2, :],
                         in0=PAR[par][:, 0:256].rearrange("p (h s) -> p h s", h=2), in1=mask4[:, 0:2, :], op=ALU.mult)
                for h in range(4):
                    c, par = h // 2, h % 2
                    po_ = par * 64
                    o_ap = PO[par][:, c * 128:(c + 1) * 128]
                    S.op('pe', 'matmul', ['Ab', 'gv_t'], [('PO', par)], o_ap, lhsT=Ab[:, 2 * par + c, :],
                         rhs=gv_t[:, j, h * 128:(h + 1) * 128], start=(c == 0), stop=False, skip_group_check=True)
                    S.op('pe', 'matmul', ['qtT', 'Sbf'], [('PO', par)], o_ap, lhsT=qtT[po_:po_ + 64, c, js],
                         rhs=Sbf[po_:po_ + 64, c, :], start=False, stop=True, skip_group_check=True)
                for c in range(2):
                    S.op('pe', 'transpose', ['ktT', 'ident_b'], ['PB'], out=PB[:, c * 128:(c + 1) * 128], in_=ktT[:, c, js],
                         identity=ident_b[:])
                S.op('act', 'activation', [], ['PB', 'kt_tok'], out=kt_tok, in_=PB[:, 0:256], func=AF.Copy)
                for h in range(4):
                    c, po_ = h // 2, (h % 2) * 64
                    S.op('pe', 'matmul', ['kt_tok', 'gv_t'], ['PS'], PS[po_:po_ + 64, c * 128:(c + 1) * 128],
                         lhsT=kt_tok[:, h * 64:(h + 1) * 64], rhs=gv_t[:, j, h * 128:(h + 1) * 128], start=(h < 2), stop=True,
                         skip_group_check=True)
                for c in range(2):
                    S.op('dve', 'tensor_tensor', [], ['PS', 'Sst'], out=Sst[:, c, :], in0=PS[:, c * 128:(c + 1) * 128],
                         in1=Sst[:, c, :], op=ALU.add)
                    S.op('dve', 'tensor_scalar', ['geq'], ['Sst'], out=Sst[:, c, :], in0=Sst[:, c, :],
                         scalar1=geq[:, c, j * 128 + 127:j * 128 + 128], scalar2=None, op0=ALU.mult)
                for h in range(4):
                    c, par = h // 2, h % 2
                    S.op('act', 'activation', [], [('PO', par), 'junk', ('sm_l', h)], out=junk[:, 0:128],
                         in_=PO[par][:, c * 128:(c + 1) * 128], func=AF.Square, accum_out=sm[:, 140 + h:141 + h])
                S.op('pool', 'tensor_copy', ['Sst'], ['Sbf'], out=Sbf[:], in_=Sst[:])
                S.op('act', 'activation', [('sm_l', h) for h in range(4)], ['sm_m'], out=sm[:, 144:148], in_=sm[:, 140:144],
                     func=AF.Sqrt, scale=1.0 / 128, bias=EPS)
                S.op('dve', 'reciprocal', ['sm_m'], ['sm_m'], out=sm[:, 144:148], in_=sm[:, 144:148])
                for h in range(4):
                    c, par = h // 2, h % 2
                    S.op('dve', 'scalar_tensor_tensor', ['sm_m', 'gsil'], [('PO', par), 'ytok'], out=ytok[:, h * 128:(h + 1) * 128],
                         in0=PO[par][:, c * 128:(c + 1) * 128], scalar=sm[:, 144 + h:145 + h], in1=gsil[:, j, h * 128:(h + 1) * 128],
                         op0=ALU.mult, op1=ALU.mult)
                tok_to_yTs(j)
            yT_store(3, t)

        S.barrier()
        KTs = vB(0, [128, S_])
        Vs = vB(S_, [128, NB, 130])
        o0 = S_ + NB * 130
        QTs = vB(o0, [128, 512])
        Pb = vB(o0 + 512, [128, 2, 512])
        ob = vB(o0 + 1536, [128, 512])
        biasI = vF(0, [128, NB])
        rd = vF(NB, [128, 4])
        d_k = d_f
        d_v = d_f2
        d_q = d_p
        OB = [PO[0], PO[1], PA, PS]
        OK = [('PO', 0), ('PO', 1), 'PA', 'PS']
        pcnt = 0
        for h in range(4):
            S.dma('sp', d_k, KTs, KTd[h], R=[('QK', 1)], W=['KTs'])
            S.dma('sp', d_v, Vs, Vd[h], R=['Vd'], W=['Vs'])
            for I in range(NT):
                S.dma('sp', d_q, QTs, QTd[h, :, I * 512:(I + 1) * 512], R=[('QK', 0)], W=['QTs'])
                nj = 4 * I + 4
                S.op('dve', 'tensor_scalar', ['Fall', 'rI'], ['biasI'], out=biasI[:, 0:nj], in0=Fall[:, 0:nj, h],
                     scalar1=rI[:, I, h:h + 1], scalar2=None, op0=ALU.add)
                for j in range(nj):
                    jj = j - 4 * I
                    q0 = 0 if jj <= 0 else jj * 128
                    nq = 512 - q0
                    pj = pcnt % 2
                    pcnt += 1
                    S.op('pe', 'matmul', ['KTs', 'QTs'], [('PJ', pj)], PJ[pj][:, 0:nq], lhsT=KTs[:, j * 128:(j + 1) * 128],
                         rhs=QTs[:, q0:512], start=True, stop=True)
                    S.op('act', 'activation', ['biasI'], [('PJ', pj), ('Pb', pj)], out=Pb[:, pj, 0:nq], in_=PJ[pj][:, 0:nq],
                         func=AF.Exp, bias=biasI[:, j:j + 1])
                    if jj >= 0:
                        S.op('pool', 'tensor_tensor', ['mask4'], [('Pb', pj)], out=Pb[:, pj, 0:128], in0=Pb[:, pj, 0:128],
                             in1=mask4[:, 0, :], op=ALU.mult)
                    for qs in range(q0 // 128, 4):
                        last = (j == 4 * I + qs)
                        S.op('pe', 'matmul', [('Pb', pj), 'Vs'], [OK[qs]], OB[qs][:, 0:130],
                             lhsT=Pb[:, pj, qs * 128 - q0:(qs + 1) * 128 - q0], rhs=Vs[:, j, :], start=(j == 0), stop=last)
                for qs in range(4):
                    S.op('dve', 'reciprocal', [], [OK[qs], ('rd', qs)], out=rd[:, qs:qs + 1], in_=OB[qs][:, 128:129])
                    S.op('dve', 'tensor_scalar', [('rd', qs)], [OK[qs], 'ob'], out=ob[:, qs * 128:(qs + 1) * 128],
                         in0=OB[qs][:, 0:128], scalar1=rd[:, qs:qs + 1], scalar2=None, op0=ALU.mult)
                for qs in range(4):
                    S.op('pe', 'transpose', ['ob', 'ident_b'], ['PB'], out=PB[:, qs * 128:(qs + 1) * 128],
                         in_=ob[:, qs * 128:(qs + 1) * 128], identity=ident_b[:])
                S.op('act', 'activation', [], ['PB', 'yTs'], out=yTs[:, 0, :], in_=PB[:, 0:512], func=AF.Copy)
                S.dma('pool', d_y, yT[4 + h, :, I * 512:(I + 1) * 512], yTs[:, 0, :], R=['yTs'], W=[('yT', 1)])

        S.barrier()
        ybr = vB(0, [128, 16, 512])
        mgb = vB(8192, [128, 8, 512])
        aT = vB(12288, [128, 16, 512])
        mg = vF(0, [128, 8, 512])
        sg = vF(4096, [128, 512])
        rl = vF(4608, [128, 512])
        d_yb = d_f
        d_o = d_f2
        accs = [PO[0], PO[1], PA, PS]
        acck = [('PO', 0), ('PO', 1), 'PA', 'PS']
        load_x(x_src, 0, 0)
        for t in range(NT):
            xi = t % 2
            xt = xts[xi]
            xk = ('xt', xi)
            cur['hi'] = 0
            hT = hTs[0]
            norm_from_xt(g1, 'g1', xi, 0)
            if t + 1 < NT:
                load_x(x_src, t + 1, (t + 1) % 2)
            S.dma('sp', d_yb, ybr, yT[:, :, t * 512:(t + 1) * 512].rearrange("c p s -> p c s"),
                  R=[('yT', b) for b in range(4)], W=['ybr'])
            for n in range(4):
                for half in range(2):
                    i = load_w(wb_br[l][n * 512:(n + 1) * 512, half * 512:(half + 1) * 512], 512, ('wb_br', l), kch=4)
                    gi = load_w(WIN[:, C_GT + n * 1024 + half * 512:C_GT + n * 1024 + half * 512 + 512], 512, kin)
                    for m in range(4):
                        dc = half * 4 + m
                        pj = next_pj()
                        for kc in range(8):
                            S.op('pe', 'matmul', [('wsl', gi), ('hT', 0, kc)], [('PJ', pj)], PJ[pj][:, :],
                                 lhsT=wsl[gi][:, kc, m * 128:(m + 1) * 128], rhs=hTs[0][:, kc, :], start=(kc == 0), stop=(kc == 7))
                        S.op('act', 'activation', ['bfe'], [('PJ', pj), 'sg'], out=sg, in_=PJ[pj][:, :], func=AF.Sigmoid,
                             bias=bfe[:, 21 + n * 8 + dc:22 + n * 8 + dc])
                        pj2 = next_pj()
                        for kc in range(4):
                            S.op('pe', 'matmul', [('wsl', i), 'ybr'], [('PJ', pj2)], PJ[pj2][:, :],
                                 lhsT=wsl[i][:, kc, m * 128:(m + 1) * 128], rhs=ybr[:, n * 4 + kc, :], start=(kc == 0),
                                 stop=(kc == 3))
                        if n == 0:
                            S.op('dve', 'tensor_tensor', ['sg'], [('PJ', pj2), ('mg', dc)], out=mg[:, dc, :], in0=PJ[pj2][:, :],
                                 in1=sg, op=ALU.mult)
                        else:
                            S.op('dve', 'tensor_tensor', [], [('PJ', pj2), 'sg'], out=sg, in0=PJ[pj2][:, :], in1=sg,
                                 op=ALU.mult)
                            S.op('pool', 'tensor_tensor', ['sg'], [('mg', dc)], out=mg[:, dc, :], in0=mg[:, dc, :], in1=sg,
                                 op=ALU.add)
            for dc in range(8):
                S.op('pool', 'tensor_copy', [('mg', dc)], ['mgb'], out=mgb[:, dc, :], in_=mg[:, dc, :])
            for half in range(2):
                wi = load_w(wb_o[l][:, half * 512:(half + 1) * 512], 512, ('wb_o', l))
                for j in range(4):
                    pj = next_pj()
                    for kc in range(8):
                        S.op('pe', 'matmul', [('wsl', wi), 'mgb'], [('PJ', pj)], PJ[pj][:, :],
                             lhsT=mgb[:, kc, j * 128:(j + 1) * 128], rhs=wsl[wi][:, kc, :], start=(kc == 0), stop=(kc == 7))
                    S.op('dve', 'tensor_tensor', [], [('PJ', pj), xk], out=xt[:, j, half * 512:(half + 1) * 512],
                         in0=PJ[pj][:, :], in1=xt[:, j, half * 512:(half + 1) * 512], op=ALU.add)
            norm_from_xt(g2, 'g2', xi, 1)
            for fh in range(2):
                for grp in range(4):
                    wi = load_w(wb_up[l][:, fh * 2048 + grp * 512:fh * 2048 + (grp + 1) * 512], 512, ('wb_up', l))
                    for m in range(4):
                        fc = grp * 4 + m
                        pj = next_pj()
                        for kc in range(8):
                            S.op('pe', 'matmul', [('wsl', wi), ('hT', 1, kc)], [('PJ', pj)], PJ[pj][:, :],
                                 lhsT=wsl[wi][:, kc, m * 128:(m + 1) * 128], rhs=hTs[1][:, kc, :], start=(kc == 0), stop=(kc == 7))
                        S.op('act', 'activation', [], [('PJ', pj), 'rl'], out=rl, in_=PJ[pj][:, :], func=AF.Relu)
                        S.op('pool', 'tensor_tensor', ['rl'], [('aT', fc)], out=aT[:, fc, :], in0=rl, in1=rl, op=ALU.mult)
                for half in range(2):
                    for grp in range(2):
                        r0 = fh * 2048 + grp * 1024
                        wi = load_w(wb_dn[l][r0:r0 + 1024, half * 512:(half + 1) * 512], 512, ('wb_dn', l))
                        for j in range(4):
                            for kc in range(8):
                                fc = grp * 8 + kc
                                S.op('pe', 'matmul', [('wsl', wi), ('aT', fc)], [acck[j]], accs[j][:, :],
                                     lhsT=aT[:, fc, j * 128:(j + 1) * 128], rhs=wsl[wi][:, kc, :], start=(fc == 0), stop=(fc == 15))
                    for j in range(4):
                        S.op('dve', 'tensor_tensor', [], [acck[j], xk], out=xt[:, j, half * 512:(half + 1) * 512],
                             in0=accs[j][:, :], in1=xt[:, j, half * 512:(half + 1) * 512], op=ALU.add)
            S.dma('pool', d_o, x_dst[t * 512:(t + 1) * 512, :].rearrange("(j p) d -> p j d", p=128), xt[:],
                  R=[xk], W=[('xs', id(x_dst))])

    S.barrier()
    S.emit()
    print("sbuf remaining", nc.sbuf_bytes_remaining, flush=True)
    print("instr counts", S.ecnt, "waits", S.nwaits, "dsems", S.ndsem, flush=True)
    return nc, es


def make_consts():
    c = np.zeros((128, 384), np.float32)
    c[:, 0:128] = np.eye(128, dtype=np.float32)
    c[:, 128:256] = np.triu(np.ones((128, 128), np.float32))
    c[:, 256:384] = 1.0
    return c


_CACHE = {}


def run(inputs, dbg=False):
    x = np.ascontiguousarray(inputs["x"], dtype=np.float32)
    B, S_, _ = x.shape
    NL = inputs["w_in"].shape[0]
    key = (S_, NL, dbg)
    if key not in _CACHE:
        _CACHE[key] = build(S_, NL, dbg)
    nc, _es = _CACHE[key]
    cst = make_consts()
    in_maps = []
    for b in range(B):
        m = {"x": x[b], "cst": cst}
        for k in PNAMES:
            m[k] = np.ascontiguousarray(inputs[k], dtype=np.float32)
        in_maps.append(m)
    res = run_bass_kernel_spmd(nc, in_maps, core_ids=list(range(B)))
    return res


def kernel(**inputs):
    res = run(inputs)
    return np.stack([r["out"] for r in res.results], axis=0).astype(np.float32)
```

```python
import os
import numpy as np
import concourse.bass as bass
import concourse.mybir as mybir
from concourse.bass_utils import run_bass_kernel_spmd
from contextlib import ExitStack

F32 = mybir.dt.float32
BF16 = mybir.dt.bfloat16
AF = mybir.ActivationFunctionType
ALU = mybir.AluOpType

D = 1024
NIN = 10268
DFF = 4096
EPS = 1e-6
C_MQ, C_MK, C_MV, C_MO, C_MI, C_MF = 0, 512, 1024, 1536, 2048, 2052
C_FQ, C_FK, C_FV, C_FF = 2056, 2568, 3080, 3592
C_RX, C_RY = 3596, 4108
C_GQ, C_GK, C_GV, C_GG, C_GA = 4620, 4876, 5132, 5644, 6156
C_GT = 6172


class DSem:
    def __init__(self, h):
        self.h = h
        self.count = 0


class Sync:
    ENG = ('pe', 'act', 'dve', 'pool', 'sp')

    def __init__(self, nc, es):
        self.nc = nc
        self.es = es
        self.esem = {k: es.enter_context(nc.semaphore('s_' + k)) for k in self.ENG}
        self.ecnt = {k: 0 for k in self.ENG}
        self.seen = {k: {} for k in self.ENG}
        self.prog = {k: [] for k in self.ENG}
        self.lastw = {}
        self.readers = {}
        self.nwaits = 0
        self.ndsem = 0
        self.dsems = []

    def dsem(self, name=None, nobar=False):
        self.ndsem += 1
        d = DSem(self.es.enter_context(self.nc.semaphore(name or ('d%d' % self.ndsem))))
        if not nobar:
            self.dsems.append(d)
        return d

    def barrier(self):
        for e in self.ENG:
            for o in self.ENG:
                if o != e and self.ecnt[o] > 0:
                    self._wait(e, (self.esem[o], self.ecnt[o]))
            for d in self.dsems:
                if d.count > 0:
                    self._wait(e, (d.h, d.count))

    def _wait(self, eng, tok):
        sem, val = tok
        if eng == 'pe' and sem is self.esem['pe']:
            return
        s = self.seen[eng]
        if s.get(id(sem), 0) >= val:
            return
        self.prog[eng].append(('w', sem, val))
        self.nwaits += 1
        s[id(sem)] = val

    def _deps(self, eng, R, W):
        for k in R:
            t = self.lastw.get(k)
            if t:
                self._wait(eng, t)
        for k in W:
            t = self.lastw.get(k)
            if t:
                self._wait(eng, t)
            for t in self.readers.get(k, {}).values():
                self._wait(eng, t)

    def _record(self, tok, R, W):
        for k in W:
            self.lastw[k] = tok
            self.readers[k] = {}
        for k in R:
            self.readers.setdefault(k, {})[id(tok[0])] = tok

    def op(self, eng, meth, R, W, *args, **kw):
        self._deps(eng, R, W)
        self.ecnt[eng] += 1
        self.prog[eng].append(('i', meth, args, kw, self.esem[eng], 1))
        self._record((self.esem[eng], self.ecnt[eng]), R, W)

    def dma(self, q, ds, out, in_, R=(), W=(), **kw):
        self._deps(q, R, W)
        ds.count += 16
        self.prog[q].append(('i', 'dma_start', (), dict(out=out, in_=in_, **kw), ds.h, 16))
        self._record((ds.h, ds.count), R, W)

    def wait_all(self, eng, keys):
        for k in keys:
            t = self.lastw.get(k)
            if t:
                self._wait(eng, t)

    def emit(self):
        nc = self.nc

        def replay(name, e):
            for it in self.prog[name]:
                if it[0] == 'w':
                    e.wait_ge(it[1], it[2])
                else:
                    _, meth, args, kw, sem, inc = it
                    getattr(e, meth)(*args, **kw).then_inc(sem, inc)

        with nc.Block() as block:
            @block.sync
            def _(e):
                replay('sp', e)

            @block.tensor
            def _(e):
                replay('pe', e)

            @block.scalar
            def _(e):
                replay('act', e)

            @block.vector
            def _(e):
                replay('dve', e)

            @block.gpsimd
            def _(e):
                replay('pool', e)


PNAMES = ["norm1_g", "w_in", "b_in", "ml_norm_g", "fx_qnorm_g", "fx_knorm_g", "lru_conv_w", "lru_conv_b",
          "lru_wa", "lru_ba", "lru_wx", "lru_bx", "lru_lambda", "gla_w_alpha", "gla_b_alpha", "gla_norm_g",
          "w_branch", "w_o", "norm2_g", "w_up", "w_down"]


def build(S_, NL, dbg=False):
    NT = S_ // 512
    NB = S_ // 128
    nc = bass.Bass("TRN2", target_bir_lowering=False)
    es = ExitStack()
    es.enter_context(nc.allow_non_contiguous_dma(reason="small param loads"))
    S = Sync(nc, es)

    def dram(name, shape, dt, kind="Internal"):
        return nc.dram_tensor(name, shape, dt, kind=kind).ap()

    x_in = dram("x", [S_, D], F32, "ExternalInput")
    cst = dram("cst", [128, 384], F32, "ExternalInput")
    shp = dict(norm1_g=[NL, D], w_in=[NL, D, NIN], b_in=[NL, NIN], ml_norm_g=[NL, 512], fx_qnorm_g=[NL, 128],
               fx_knorm_g=[NL, 128], lru_conv_w=[NL, 4, 512], lru_conv_b=[NL, 512], lru_wa=[NL, 8, 64, 64],
               lru_ba=[NL, 512], lru_wx=[NL, 8, 64, 64], lru_bx=[NL, 512], lru_lambda=[NL, 512],
               gla_w_alpha=[NL, 16, 256], gla_b_alpha=[NL, 256], gla_norm_g=[NL, 512],
               w_branch=[NL, 4, 512, D], w_o=[NL, D, D], norm2_g=[NL, D], w_up=[NL, D, DFF], w_down=[NL, DFF, D])
    P = {k: dram(k, shp[k], F32, "ExternalInput") for k in PNAMES}
    out = dram("out", [S_, D], F32, "ExternalOutput")
    dbg_y = dram("dbg_y", [16, 128, S_], BF16, "ExternalOutput") if dbg else None

    wb_in = dram("wb_in", [NL, D, NIN], BF16)
    wb_br = dram("wb_br", [NL, 2048, D], BF16)
    wb_o = dram("wb_o", [NL, D, D], BF16)
    wb_up = dram("wb_up", [NL, D, DFF], BF16)
    wb_dn = dram("wb_dn", [NL, DFF, D], BF16)
    xs = [dram("xs0", [S_, D], F32), dram("xs1", [S_, D], F32)]
    yT = dbg_y if dbg else dram("yT", [16, 128, S_], BF16)
    QTd = dram("QTd", [4, 128, S_], BF16)
    KTd = dram("KTd", [4, 128, S_], BF16)
    Vd = dram("Vd", [4, 128, NB, 130], BF16)

    def sb(name, shape, dt):
        return es.enter_context(nc.sbuf_tensor(name, shape, dt))

    def ps(name, shape, dt):
        return es.enter_context(nc.psum_tensor(name, shape, dt))

    def cast_w(src, dst, key):
        ds = S.dsem(nobar=True)
        n = 1
        for s_ in src.shape:
            n *= s_
        rows = n // 2048
        pat = " ".join("abcd"[:len(src.shape)])
        sv = src.rearrange(f"{pat} -> ({pat})").rearrange("(r c) -> r c", c=2048)
        pat = " ".join("abcd"[:len(dst.shape)])
        dv = dst.rearrange(f"{pat} -> ({pat})").rearrange("(r c) -> r c", c=2048)
        r0 = 0
        while r0 < rows:
            r1 = min(rows, r0 + 2048)
            S.dma('pool', ds, dv[r0:r1, :], sv[r0:r1, :], W=[key])
            r0 = r1

    for l in range(NL):
        cast_w(P["w_in"][l], wb_in[l], ('wb_in', l))
        cast_w(P["w_branch"][l], wb_br[l], ('wb_br', l))
        cast_w(P["w_o"][l], wb_o[l], ('wb_o', l))
        cast_w(P["w_up"][l], wb_up[l], ('wb_up', l))
        cast_w(P["w_down"][l], wb_dn[l], ('wb_dn', l))

    cst_f = sb("cst_f", [128, 384], F32)
    d_c = S.dsem()
    S.dma('sp', d_c, cst_f[:], cst, W=['cst_f'])
    ident_f = cst_f[:, 0:128]
    tri_f = cst_f[:, 128:256]
    ones_f = cst_f[:, 256:384]
    ident_b = sb("ident_b", [128, 128], BF16)
    mask4 = sb("mask4", [128, 4, 128], BF16)
    S.op('dve', 'tensor_copy', ['cst_f'], ['ident_b'], out=ident_b[:], in_=ident_f)
    for h in range(4):
        S.op('dve', 'tensor_copy', ['cst_f'], ['mask4'], out=mask4[:, h, :], in_=tri_f)

    xts = [sb("xt0", [128, 4, D], F32), sb("xt1", [128, 4, D], F32)]
    junk = sb("junk", [128, D], F32)
    hTs = [sb("hT0", [128, 8, 512], BF16), sb("hT1", [128, 8, 512], BF16)]
    NW = 3
    wsl = [sb("wsl%d" % i, [128, 8, 512], BF16) for i in range(NW)]
    d_wsl = [S.dsem() for _ in range(NW)]
    d_xs = [S.dsem(), S.dsem()]
    cur = dict(hi=0)
    st1 = sb("st1", [128, 16], F32)
    bfe = sb("bfe", [128, 64], F32)
    g1 = sb("g1", [128, 8], F32)
    g2 = sb("g2", [128, 8], F32)
    sm = sb("sm", [128, 160], F32)
    yTs = sb("yTs", [128, 4, 512], BF16)
    ytok = sb("ytok", [128, 512], BF16)
    yTs2 = sb("yTs2", [128, 4, 512], BF16)
    ytok2 = sb("ytok2", [128, 512], BF16)
    junk2 = sb("junk2", [128, 128], F32)
    gbc2 = sb("gbc2", [128, 512], F32)
    d_y2 = S.dsem()
    d_g2 = S.dsem()
    Fall = sb("Fall", [128, NB, 4], F32)
    rI = sb("rI", [128, NT, 4], F32)
    d_p = S.dsem()
    d_y = S.dsem()
    bb = sb("bb", [128, 4108], F32)
    mif = sb("mif", [128, 4, 8], F32)
    Cst = sb("Cst", [128, 4, 130], F32)
    Cbf = sb("Cbf", [128, 4, 130], BF16)
    gbc = sb("gbc", [128, 512], F32)
    gqk = sb("gqk", [128, 2, 128], F32)
    Fcar = sb("Fcar", [128, 4], F32)
    rxy = sb("rxy", [128, 4, 515], F32)
    lcar = sb("lcar", [128, 4], F32)
    lpar = sb("lpar", [128, 64], F32)
    wbd = sb("wbd", [128, 2, 4, 128], BF16)
    wal_f = sb("wal_f", [16, 256], F32)
    wal = sb("wal", [16, 256], BF16)
    gpar = sb("gpar", [128, 8], F32)
    Sst = sb("Sst", [128, 2, 128], F32)
    Sbf = sb("Sbf", [128, 2, 128], BF16)
    d_f = S.dsem()
    d_f2 = S.dsem()
    bigB = sb("bigB", [128, 20480], BF16)
    bigF = sb("bigF", [128, 8192], F32)

    def vB(off, shape):
        n = 1
        for s_ in shape[1:]:
            n *= s_
        v = bigB[:, off:off + n]
        if len(shape) == 3:
            v = v.rearrange("p (a b) -> p a b", a=shape[1])
        elif len(shape) == 4:
            v = v.rearrange("p (a b c) -> p a b c", a=shape[1], b=shape[2])
        return v

    def vF(off, shape):
        n = 1
        for s_ in shape[1:]:
            n *= s_
        v = bigF[:, off:off + n]
        if len(shape) == 3:
            v = v.rearrange("p (a b) -> p a b", a=shape[1])
        return v

    PJ = [ps("PJ0", [128, 512], F32), ps("PJ1", [128, 512], F32)]
    PT = ps("PT", [128, 512], F32)
    PB = ps("PB", [128, 1024], BF16)
    PA = ps("PA", [128, 512], F32)
    PO = [ps("PO0", [128, 512], F32), ps("PO1", [128, 512], F32)]
    PS = ps("PS", [128, 512], F32)

    state = dict(wslot=0, pj=0)

    def load_w(src_ap, ncols, rkey, kch=8):
        i = state['wslot']
        state['wslot'] = (i + 1) % NW
        S.dma('sp', d_wsl[i], wsl[i][:, 0:kch, 0:ncols], src_ap.rearrange("(c p) n -> p c n", p=128),
              R=[rkey], W=[('wsl', i)])
        return i

    def next_pj():
        i = state['pj']
        state['pj'] ^= 1
        return i

    def norm_from_xt(gt, gkey, xi, hi):
        xt = xts[xi]
        xk = ('xt', xi)
        hT = hTs[hi]
        for j in range(4):
            S.op('act', 'activation', [xk], ['junk', ('st1', j)], out=junk[:], in_=xt[:, j, :], func=AF.Square,
                 accum_out=st1[:, j:j + 1])
        S.op('act', 'activation', [('st1', j) for j in range(4)], ['st1b'], out=st1[:, 4:8], in_=st1[:, 0:4],
             func=AF.Sqrt, scale=1.0 / D, bias=EPS)
        S.op('dve', 'reciprocal', ['st1b'], ['st1c'], out=st1[:, 8:12], in_=st1[:, 4:8])
        for j in range(4):
            S.op('dve', 'tensor_scalar', [xk, 'st1c'], ['junk'], out=junk[:], in0=xt[:, j, :],
                 scalar1=st1[:, 8 + j:9 + j], scalar2=None, op0=ALU.mult)
            for half in range(2):
                for q in range(4):
                    c = half * 4 + q
                    S.op('pe', 'transpose', ['junk', 'cst_f'], ['PT'], out=PT[:, q * 128:(q + 1) * 128],
                         in_=junk[:, c * 128:(c + 1) * 128], identity=ident_f)
                for q in range(4):
                    c = half * 4 + q
                    S.op('dve', 'tensor_scalar', [gkey], ['PT', ('hT', hi, c)], out=hT[:, c, j * 128:(j + 1) * 128],
                         in0=PT[:, q * 128:(q + 1) * 128], scalar1=gt[:, c:c + 1], scalar2=None, op0=ALU.mult)

    def load_x(src_dram, t, xi):
        S.dma('sp', d_xs[xi], xts[xi][:], src_dram[t * 512:(t + 1) * 512, :].rearrange("(j p) d -> p j d", p=128),
              R=[('xs', id(src_dram))], W=[('xt', xi)])

    def proj_feat(wsrc, wkey, c0, ncols, evac):
        done = 0
        while done < ncols:
            n = min(512, ncols - done)
            wi = load_w(wsrc[:, c0 + done:c0 + done + n], n, wkey)
            mc = 0
            while mc * 128 < n:
                m = min(128, n - mc * 128)
                pj = next_pj()
                for kc in range(8):
                    S.op('pe', 'matmul', [('wsl', wi), ('hT', cur['hi'], kc)], [('PJ', pj)], PJ[pj][0:m, :],
                         lhsT=wsl[wi][:, kc, mc * 128:mc * 128 + m], rhs=hTs[cur['hi']][:, kc, :], start=(kc == 0), stop=(kc == 7))
                evac((done + mc * 128) // 128, m, PJ[pj], ('PJ', pj))
                mc += 1
            done += n

    def proj_tok(wsrc, wkey, c0, ncols, evac):
        done = 0
        while done < ncols:
            n = min(512, ncols - done)
            wi = load_w(wsrc[:, c0 + done:c0 + done + n], n, wkey)
            for j in range(4):
                pj = next_pj()
                for kc in range(8):
                    S.op('pe', 'matmul', [('wsl', wi), ('hT', cur['hi'], kc)], [('PJ', pj)], PJ[pj][:, 0:n],
                         lhsT=hTs[cur['hi']][:, kc, j * 128:(j + 1) * 128], rhs=wsl[wi][:, kc, 0:n], start=(kc == 0), stop=(kc == 7))
                evac(j, done, n, PJ[pj], ('PJ', pj))
            done += n

    def yT_store(br, t, alt=False):
        ys, yk, dsy = (yTs2, 'yTs2', d_y2) if alt else (yTs, 'yTs', d_y)
        S.dma('pool', dsy, yT[br * 4:(br + 1) * 4, :, t * 512:(t + 1) * 512].rearrange("c p s -> p c s"),
              ys[:], R=[yk], W=[('yT', br)])

    def tok_to_yTs(j, alt=False):
        ys, yk, yt_, ytk = (yTs2, 'yTs2', ytok2, 'ytok2') if alt else (yTs, 'yTs', ytok, 'ytok')
        for c in range(4):
            S.op('pe', 'transpose', [ytk, 'ident_b'], ['PB'], out=PB[:, c * 128:(c + 1) * 128],
                 in_=yt_[:, c * 128:(c + 1) * 128], identity=ident_b[:])
        S.op('act', 'activation', [], ['PB', yk], out=ys[:, :, j * 128:(j + 1) * 128],
             in_=PB[:, 0:512].rearrange("p (c s) -> p c s", c=4), func=AF.Copy)

    for l in range(NL):
        x_src = x_in if l == 0 else xs[(l - 1) % 2]
        x_dst = out if l == NL - 1 else xs[l % 2]
        WIN = wb_in[l]
        kin = ('wb_in', l)
        S.barrier()
        bi = P["b_in"][l]
        S.dma('sp', d_p, g1[:], P["norm1_g"][l].rearrange("(c p) -> p c", p=128), W=['g1'])
        S.dma('sp', d_p, g2[:], P["norm2_g"][l].rearrange("(c p) -> p c", p=128), W=['g2'])
        S.dma('sp', d_p, bb[:, 0:1544], bi[512:2056].partition_broadcast(128), W=['bb'])
        S.dma('sp', d_p, bb[:, 1544:3084], bi[2056:3596].partition_broadcast(128), W=['bb'])
        S.dma('sp', d_p, bb[:, 3084:4108], bi[5132:6156].partition_broadcast(128), W=['bb'])
        S.dma('sp', d_p, bfe[:, 0:8], bi[0:1024].rearrange("(c p) -> p c", p=128), W=['bfe'])
        S.dma('sp', d_p, bfe[:, 8:16], bi[C_RX:C_RX + 1024].rearrange("(c p) -> p c", p=128), W=['bfe'])
        S.dma('sp', d_p, bfe[:, 16:20], bi[C_GQ:C_GQ + 512].rearrange("(c p) -> p c", p=128), W=['bfe'])
        S.dma('sp', d_p, bfe[0:16, 20:21], bi[C_GA:C_GA + 16].rearrange("(p c) -> p c", c=1), W=['bfe'])
        S.dma('sp', d_p, bfe[:, 21:53], bi[C_GT:C_GT + 4096].rearrange("(c p) -> p c", p=128), W=['bfe'])
        S.dma('sp', d_p, gqk[:, 0, :], P["fx_qnorm_g"][l].partition_broadcast(128), W=['gqk'])
        S.dma('sp', d_p, gqk[:, 1, :], P["fx_knorm_g"][l].partition_broadcast(128), W=['gqk'])
        for k_ in range(4):
            S.dma('sp', d_p, lpar[:, 4 * k_:4 * k_ + 4], P["lru_conv_w"][l, k_].rearrange("(c p) -> p c", p=128), W=['lpar'])
        for i_, nm in enumerate(["lru_conv_b", "lru_ba", "lru_bx", "lru_lambda"]):
            S.dma('sp', d_p, lpar[:, 16 + 4 * i_:20 + 4 * i_], P[nm][l].rearrange("(c p) -> p c", p=128), W=['lpar'])
        LP = ['lpar', 'lpard', 'lpare']
        wbd_f = junk[:, :].rearrange("p (w c k) -> p w c k", w=2, c=4)
        S.op('pool', 'memset', [], ['junk'], junk[:], 0.0)
        for wi_, nm in enumerate(["lru_wa", "lru_wx"]):
            for g in range(8):
                c, po = g // 2, (g % 2) * 64
                S.dma('sp', d_p, wbd_f[po:po + 64, wi_, c, po:po + 64], P[nm][l, g], R=[], W=['junk'])
        S.dma('sp', d_p, wal_f[:], P["gla_w_alpha"][l], W=['wal_f'])
        S.dma('sp', d_p, gpar[:, 0:2], P["gla_b_alpha"][l].rearrange("(c p) -> p c", p=128), W=['gpar'])
        S.barrier()
        S.op('pool', 'tensor_scalar', ['gqk'], ['gqk'], out=gqk[:, 0, :], in0=gqk[:, 0, :], scalar1=float(128 ** -0.5),
             scalar2=None, op0=ALU.mult)
        S.op('act', 'activation', ['lpar'], ['lparb'], out=lpar[:, 40:44], in_=lpar[:, 28:32], func=AF.Exp, scale=-1.0)
        S.op('act', 'activation', ['lparb'], ['lparc'], out=lpar[:, 44:48], in_=lpar[:, 40:44], func=AF.Ln, bias=1.0)
        S.op('dve', 'tensor_scalar', ['lparc'], ['lpard'], out=lpar[:, 32:36], in0=lpar[:, 44:48], scalar1=-8.0,
             scalar2=None, op0=ALU.mult)
        S.op('dve', 'tensor_scalar', ['lparc'], ['lpare'], out=lpar[:, 36:40], in0=lpar[:, 44:48], scalar1=-16.0,
             scalar2=None, op0=ALU.mult)
        S.op('pool', 'tensor_copy', ['junk'], ['wbd'], out=wbd[:], in_=wbd_f)
        S.op('pool', 'tensor_copy', ['wal_f'], ['wal'], out=wal[:], in_=wal_f[:])
        S.op('pool', 'tensor_scalar', ['gpar'], ['gparb'], out=gpar[:, 2:4], in0=gpar[:, 0:2], scalar1=-1.0,
             scalar2=None, op0=ALU.mult)
        for tl, key in [(Cst, 'Cst'), (Cbf, 'Cbf'), (Sst, 'Sst'), (Sbf, 'Sbf'), (lcar, 'lcar'), (Fcar, 'Fcar'), (rxy, 'rxy')]:
            S.op('pool', 'memset', [], [key], tl[:], 0.0)
        S.barrier()

        load_x(x_src, 0, 0)
        norm_from_xt(g1, 'g1', 0, 0)
        for t in range(NT):
            cur['hi'] = t % 2
            if t + 1 < NT:
                load_x(x_src, t + 1, (t + 1) % 2)

            S.barrier()
            gqT = vF(0, [128, 2, 512])
            gkT = vF(1024, [128, 2, 512])
            gsp = vF(2048, [128, 2, 512])
            gcs = vF(3072, [128, 2, 512])
            geq = vF(4096, [128, 2, 512])
            gek = vF(5120, [128, 2, 512])
            gsil = vF(6144, [128, 4, 512])
            qtT = vB(0, [128, 2, 512])
            ktT = vB(1024, [128, 2, 512])
            kt_tok = vB(2048, [128, 256])
            gv_t = vB(2304, [128, 4, 512])
            Ab = vB(4352, [128, 4, 128])
            gaT = vB(4864, [128, 512])
            S.dma('sp', d_g2, gbc2[:], P["gla_norm_g"][l].partition_broadcast(128), W=['gbc2'])

            def ev_gq(mc, m, pt_, pk):
                dst = gqT if mc < 2 else gkT
                S.op('act', 'activation', ['bfe'], [pk, ('gqk_', mc)], out=dst[:, mc % 2, :], in_=pt_[:, :], func=AF.Identity,
                     bias=bfe[:, 16 + mc:17 + mc])
            proj_feat(WIN, kin, C_GQ, 512, ev_gq)

            def ev_ga(mc, m, pt_, pk):
                S.op('act', 'activation', ['bfe'], [pk, 'gaT'], out=gaT[0:16, :], in_=pt_[0:16, :], func=AF.Identity,
                     bias=bfe[0:16, 20:21])
            proj_feat(WIN, kin, C_GA, 16, ev_ga)

            def ev_gtok(j, off, n, pt_, pk):
                if off == 0:
                    S.op('dve', 'tensor_tensor', ['bb'], [pk, 'gv_t'], out=gv_t[:, j, :], in0=pt_[:, 0:512],
                         in1=bb[:, 3084:3596], op=ALU.add)
                else:
                    S.op('dve', 'tensor_tensor', ['bb'], [pk, 'gsil'], out=gsil[:, j, :], in0=pt_[:, 0:512],
                         in1=bb[:, 3596:4108], op=ALU.add)
                    S.op('act', 'activation', ['gsil'], ['junk'], out=junk[:, 0:512], in_=gsil[:, j, :], func=AF.Sigmoid)
                    S.op('dve', 'tensor_tensor', ['junk'], ['gsil'], out=gsil[:, j, :], in0=gsil[:, j, :],
                         in1=junk[:, 0:512], op=ALU.mult)
                    S.op('pool', 'tensor_tensor', ['gbc2'], ['gsil'], out=gsil[:, j, :], in0=gsil[:, j, :], in1=gbc2[:],
                         op=ALU.mult)
            proj_tok(WIN, kin, C_GV, 1024, ev_gtok)
            for c in range(2):
                pj = next_pj()
                S.op('pe', 'matmul', ['wal', 'gaT'], [('PJ', pj)], PJ[pj][:, :], lhsT=wal[:, c * 128:(c + 1) * 128],
                     rhs=gaT[0:16, :], start=True, stop=True)
                S.op('act', 'activation', ['gparb'], [('PJ', pj), 'gsp'], out=gsp[:, c, :], in_=PJ[pj][:, :], func=AF.Exp,
                     scale=-1.0, bias=gpar[:, 2 + c:3 + c])
                S.op('act', 'activation', ['gsp'], ['gsp'], out=gsp[:, c, :], in_=gsp[:, c, :], func=AF.Ln, bias=1.0)
                for j in range(4):
                    js = slice(j * 128, (j + 1) * 128)
                    S.op('dve', 'tensor_tensor_scan', ['gsp', 'cst_f'], ['gcs'], out=gcs[:, c, js], data0=ones_f,
                         data1=gsp[:, c, js], initial=0.0, op0=ALU.mult, op1=ALU.add)
                S.op('act', 'activation', ['gcs'], ['geq'], out=geq[:, c, :], in_=gcs[:, c, :], func=AF.Exp, scale=-1.0 / 16)
                S.op('act', 'activation', ['gcs'], ['gek'], out=gek[:, c, :], in_=gcs[:, c, :], func=AF.Exp, scale=1.0 / 16)
                S.op('dve', 'scalar_tensor_tensor', [('gqk_', c), 'geq'], ['qtT'], out=qtT[:, c, :], in0=gqT[:, c, :],
                     scalar=0.125, in1=geq[:, c, :], op0=ALU.mult, op1=ALU.mult)
                S.op('dve', 'tensor_tensor', [('gqk_', 2 + c), 'gek'], ['ktT'], out=ktT[:, c, :], in0=gkT[:, c, :],
                     in1=gek[:, c, :], op=ALU.mult)
            S.barrier()
            mqT = vB(5376, [128, 4, 512])
            mkT = vB(7424, [128, 4, 512])
            mk_tok = vB(9472, [128, 4, 512])
            mv_aug = vB(11520, [128, 4, 4, 130])
            STb = vB(13600, [128, 4, 128])
            kd = vB(14112, [128, 4, 128])
            gs_ml = vF(0, [128, 4, 512])
            ytmp = vF(2048, [128, 4, 130])
            S.op('pool', 'memset', [], ['mv_aug'], mv_aug, 1.0)
            S.dma('sp', d_p, gbc[:], P["ml_norm_g"][l].partition_broadcast(128), W=['gbc'])

            def ev_mq(mc, m, pt_, pk):
                if mc < 4:
                    S.op('dve', 'tensor_scalar', ['bfe'], [pk, ('mqk', mc)], out=mqT[:, mc, :], in0=pt_[:, :],
                         scalar1=bfe[:, mc:mc + 1], scalar2=float(128 ** -0.5), op0=ALU.add, op1=ALU.mult)
                else:
                    S.op('act', 'activation', ['bfe'], [pk, ('mqk', mc)], out=mkT[:, mc - 4, :], in_=pt_[:, :],
                         func=AF.Identity, bias=bfe[:, mc:mc + 1])
            proj_feat(WIN, kin, C_MQ, 1024, ev_mq)

            def ev_mtok(j, off, n, pt_, pk):
                if off == 0:
                    S.op('dve', 'tensor_tensor', ['bb'], [pk, 'mk_tok'], out=mk_tok[:, j, :], in0=pt_[:, 0:512],
                         in1=bb[:, 0:512], op=ALU.add)
                elif off == 512:
                    S.op('dve', 'tensor_tensor', ['bb'], [pk, 'mv_aug'], out=mv_aug[:, j, :, 0:128],
                         in0=pt_[:, 0:512].rearrange("p (h e) -> p h e", h=4),
                         in1=bb[:, 512:1024].rearrange("p (h e) -> p h e", h=4), op=ALU.add)
                elif off == 1024:
                    S.op('dve', 'tensor_tensor', ['bb'], [pk, 'gs_ml'], out=gs_ml[:, j, :], in0=pt_[:, 0:512],
                         in1=bb[:, 1024:1536], op=ALU.add)
                    S.op('act', 'activation', ['gs_ml'], ['gs_ml'], out=gs_ml[:, j, :], in_=gs_ml[:, j, :], func=AF.Sigmoid)
                    S.op('pool', 'tensor_tensor', ['gs_ml', 'gbc'], ['gs_ml'], out=gs_ml[:, j, :], in0=gs_ml[:, j, :],
                         in1=gbc[:], op=ALU.mult)
                else:
                    S.op('dve', 'tensor_tensor', ['bb'], [pk, 'mif'], out=mif[:, j, :], in0=pt_[:, 0:8],
                         in1=bb[:, 1536:1544], op=ALU.add)
            proj_tok(WIN, kin, C_MK, 1544, ev_mtok)

            S.op('act', 'activation', ['mif'], ['sm_a'], out=sm[:, 0:16].rearrange("p (j h) -> p j h", h=4),
                 in_=mif[:, :, 4:8], func=AF.Exp, scale=-1.0)
            S.op('act', 'activation', ['sm_a'], ['sm_a'], out=sm[:, 0:16], in_=sm[:, 0:16], func=AF.Ln, bias=1.0)
            S.op('pe', 'matmul', ['sm_a', 'cst_f'], ['PA'], PA[:, 0:16], lhsT=tri_f, rhs=sm[:, 0:16], start=True, stop=True)
            S.op('pe', 'matmul', ['sm_a', 'cst_f'], ['PA'], PA[:, 16:32], lhsT=ones_f, rhs=sm[:, 0:16], start=True, stop=True)
            S.op('dve', 'tensor_copy', [], ['PA', 'sm_b'], out=sm[:, 16:48], in_=PA[:, 0:32])
            S.op('act', 'activation', ['sm_b'], ['sm_c'], out=sm[:, 48:80], in_=sm[:, 16:48], func=AF.Exp, scale=-1.0)
            S.op('dve', 'tensor_tensor', ['sm_b', 'mif'], ['sm_d'], out=sm[:, 96:112].rearrange("p (j h) -> p j h", h=4),
                 in0=sm[:, 16:32].rearrange("p (j h) -> p j h", h=4), in1=mif[:, :, 0:4], op=ALU.add)
            S.op('dve', 'tensor_tensor', ['sm_d', 'sm_b'], ['sm_e'], out=sm[:, 80:96], in0=sm[:, 96:112], in1=sm[:, 32:48],
                 op=ALU.subtract)
            S.op('act', 'activation', ['sm_e'], ['sm_e'], out=sm[:, 80:96], in_=sm[:, 80:96], func=AF.Exp)
            S.op('act', 'activation', ['sm_d'], ['sm_d'], out=sm[:, 96:112], in_=sm[:, 96:112], func=AF.Exp)
            if t + 1 < NT:
                norm_from_xt(g1, 'g1', (t + 1) % 2, (t + 1) % 2)
            PAR = [PJ[0], PJ[1]]
            PARK = [('PJ', 0), ('PJ', 1)]
            def ml_chain(j):
                js = slice(j * 128, (j + 1) * 128)
                for h in range(4):
                    S.op('pe', 'matmul', [('mqk', h), ('mqk', 4 + h)], ['PA'], PA[:, h * 128:(h + 1) * 128],
                         lhsT=mkT[:, h, js], rhs=mqT[:, h, js], start=True, stop=True)
                for h in range(4):
                    col = 96 + j * 4 + h
                    S.op('dve', 'scalar_tensor_tensor', ['sm_d', 'mask4'], ['PA', 'STb'], out=STb[:, h, :],
                         in0=PA[:, h * 128:(h + 1) * 128], scalar=sm[:, col:col + 1], in1=mask4[:, h, :],
                         op0=ALU.mult, op1=ALU.mult)
                for h in range(4):
                    pk = ('PO', h // 2)
                    o_ap = PO[h // 2][:, (h % 2) * 130:(h % 2) * 130 + 130]
                    S.op('pe', 'matmul', ['STb', 'mv_aug'], [pk], o_ap, lhsT=STb[:, h, :], rhs=mv_aug[:, j, h, :],
                         start=(h % 2 == 0), stop=False, skip_group_check=True)
                    S.op('pe', 'matmul', [('mqk', h), 'Cbf'], [pk], o_ap, lhsT=mqT[:, h, js], rhs=Cbf[:, h, :],
                         start=False, stop=True, skip_group_check=True)
                for h in range(4):
                    col = 80 + j * 4 + h
                    S.op('pool', 'tensor_scalar', ['mk_tok', 'sm_e'], ['kd'], out=kd[:, h, :],
                         in0=mk_tok[:, j, h * 128:(h + 1) * 128], scalar1=sm[:, col:col + 1], scalar2=None, op0=ALU.mult)
                for h in range(4):
                    S.op('pe', 'matmul', ['kd', 'mv_aug'], ['PS'], PS[:, 0:130], lhsT=kd[:, h, :], rhs=mv_aug[:, j, h, :],
                         start=True, stop=True)
                    col = 64 + j * 4 + h
                    S.op('dve', 'scalar_tensor_tensor', ['sm_c'], ['PS', 'Cst'], out=Cst[:, h, :], in0=Cst[:, h, :],
                         scalar=sm[:, col:col + 1], in1=PS[:, 0:130], op0=ALU.mult, op1=ALU.add)
                for hp in range(2):
                    S.op('act', 'activation', [], [('PO', hp), 'ytmp'], out=ytmp[:, 2 * hp:2 * hp + 2, :],
                         in_=PO[hp][:, 0:260].rearrange("p (h e) -> p h e", h=2), func=AF.Copy)
                S.op('pool', 'tensor_copy', ['Cst'], ['Cbf'], out=Cbf[:], in_=Cst[:])
                eb4 = sm[:, 48 + j * 4:52 + j * 4]
                S.op('dve', 'tensor_tensor', ['ytmp', 'sm_c'], ['sm_f'], out=sm[:, 112:116], in0=ytmp[:, :, 128], in1=eb4,
                     op=ALU.mult)
                S.op('act', 'activation', ['sm_f'], ['sm_f'], out=sm[:, 112:116], in_=sm[:, 112:116], func=AF.Abs)
                S.op('dve', 'tensor_scalar', ['sm_f'], ['sm_f'], out=sm[:, 112:116], in0=sm[:, 112:116], scalar1=1.0,
                     scalar2=None, op0=ALU.max)
                S.op('dve', 'reciprocal', ['sm_f'], ['sm_f'], out=sm[:, 112:116], in_=sm[:, 112:116])
                S.op('dve', 'tensor_tensor', ['sm_f', 'sm_c'], ['sm_f'], out=sm[:, 112:116], in0=sm[:, 112:116], in1=eb4,
                     op=ALU.mult)
                for h in range(4):
                    S.op('dve', 'tensor_scalar', ['sm_f', 'ytmp'], ['ytmp'], out=ytmp[:, h, 0:128], in0=ytmp[:, h, 0:128],
                         scalar1=sm[:, 112 + h:113 + h], scalar2=None, op0=ALU.mult)
                for h in range(4):
                    S.op('act', 'activation', ['ytmp'], ['junk', ('sm_g', h)], out=junk[:, 0:128], in_=ytmp[:, h, 0:128],
                         func=AF.Square, accum_out=sm[:, 116 + h:117 + h])
                S.op('act', 'activation', [('sm_g', h) for h in range(4)], ['sm_h'], out=sm[:, 120:124], in_=sm[:, 116:120],
                     func=AF.Sqrt, scale=1.0 / 128, bias=EPS)
                S.op('dve', 'reciprocal', ['sm_h'], ['sm_h'], out=sm[:, 120:124], in_=sm[:, 120:124])
                for h in range(4):
                    S.op('dve', 'scalar_tensor_tensor', ['sm_h', 'ytmp', 'gs_ml'], ['ytok'], out=ytok[:, h * 128:(h + 1) * 128],
                         in0=ytmp[:, h, 0:128], scalar=sm[:, 120 + h:121 + h], in1=gs_ml[:, j, h * 128:(h + 1) * 128],
                         op0=ALU.mult, op1=ALU.mult)
                tok_to_yTs(j)
            def gla_chain(j):
                js = slice(j * 128, (j + 1) * 128)
                for h in range(4):
                    c, par = h // 2, h % 2
                    po_ = par * 64
                    S.op('pe', 'matmul', ['ktT', 'qtT'], [PARK[par]], PAR[par][:, c * 128:(c + 1) * 128],
                         lhsT=ktT[po_:po_ + 64, c, js], rhs=qtT[po_:po_ + 64, c, js], start=True, stop=True)
                for par in range(2):
                    S.op('dve', 'tensor_tensor', ['mask4'], [PARK[par], 'Ab'], out=Ab[:, 2 * par:2 * par + 2, :],
                         in0=PAR[par][:, 0:256].rearrange("p (h s) -> p h s", h=2), in1=mask4[:, 0:2, :], op=ALU.mult)
                for h in range(4):
                    c, par = h // 2, h % 2
                    po_ = par * 64
                    o_ap = PJ[par][:, c * 128:(c + 1) * 128]
                    S.op('pe', 'matmul', ['Ab', 'gv_t'], [('PJ', par)], o_ap, lhsT=Ab[:, 2 * par + c, :],
                         rhs=gv_t[:, j, h * 128:(h + 1) * 128], start=(c == 0), stop=False, skip_group_check=True)
                    S.op('pe', 'matmul', ['qtT', 'Sbf'], [('PJ', par)], o_ap, lhsT=qtT[po_:po_ + 64, c, js],
                         rhs=Sbf[po_:po_ + 64, c, :], start=False, stop=True, skip_group_check=True)
                for c in range(2):
                    S.op('pe', 'transpose', ['ktT', 'ident_b'], ['PB'], out=PB[:, c * 128:(c + 1) * 128], in_=ktT[:, c, js],
                         identity=ident_b[:])
                S.op('act', 'activation', [], ['PB', 'kt_tok'], out=kt_tok, in_=PB[:, 0:256], func=AF.Copy)
                for h in range(4):
                    c, po_ = h // 2, (h % 2) * 64
                    S.op('pe', 'matmul', ['kt_tok', 'gv_t'], ['PT'], PT[po_:po_ + 64, c * 128:(c + 1) * 128],
                         lhsT=kt_tok[:, h * 64:(h + 1) * 64], rhs=gv_t[:, j, h * 128:(h + 1) * 128], start=(h < 2), stop=True,
                         skip_group_check=True)
                for c in range(2):
                    S.op('dve', 'tensor_tensor', [], ['PT', 'Sst'], out=Sst[:, c, :], in0=PT[:, c * 128:(c + 1) * 128],
                         in1=Sst[:, c, :], op=ALU.add)
                    S.op('dve', 'tensor_scalar', ['geq'], ['Sst'], out=Sst[:, c, :], in0=Sst[:, c, :],
                         scalar1=geq[:, c, j * 128 + 127:j * 128 + 128], scalar2=None, op0=ALU.mult)
                for h in range(4):
                    c, par = h // 2, h % 2
                    S.op('act', 'activation', [], [('PJ', par), 'junk2', ('sm_l', h)], out=junk2[:, 0:128],
                         in_=PJ[par][:, c * 128:(c + 1) * 128], func=AF.Square, accum_out=sm[:, 140 + h:141 + h])
                S.op('pool', 'tensor_copy', ['Sst'], ['Sbf'], out=Sbf[:], in_=Sst[:])
                S.op('act', 'activation', [('sm_l', h) for h in range(4)], ['sm_m'], out=sm[:, 144:148], in_=sm[:, 140:144],
                     func=AF.Sqrt, scale=1.0 / 128, bias=EPS)
                S.op('dve', 'reciprocal', ['sm_m'], ['sm_m'], out=sm[:, 144:148], in_=sm[:, 144:148])
                for h in range(4):
                    c, par = h // 2, h % 2
                    S.op('dve', 'scalar_tensor_tensor', ['sm_m', 'gsil'], [('PJ', par), 'ytok2'], out=ytok2[:, h * 128:(h + 1) * 128],
                         in0=PJ[par][:, c * 128:(c + 1) * 128], scalar=sm[:, 144 + h:145 + h], in1=gsil[:, j, h * 128:(h + 1) * 128],
                         op0=ALU.mult, op1=ALU.mult)
                tok_to_yTs(j, alt=True)
            for j in range(4):
                ml_chain(j)
                gla_chain(j)
            yT_store(0, t)
            yT_store(3, t, alt=True)

            S.barrier()
            fqn = vB(0, [128, 512])
            fT = vB(512, [128, 4, 512])
            fv_aug = vB(2560, [128, 4, 4, 130])
            fq_t = vF(0, [128, 512])
            S.op('pool', 'memset', [], ['fv_aug'], fv_aug, 1.0)

            def ev_ftok(j, off, n, pt_, pk):
                if off < 1024:
                    w = off // 512
                    S.op('dve', 'tensor_tensor', ['bb'], [pk, 'fq_t'], out=fq_t, in0=pt_[:, 0:512],
                         in1=bb[:, 1544 + off:1544 + off + 512], op=ALU.add)
                    for h in range(4):
                        S.op('act', 'activation', ['fq_t'], ['junk', ('sm_i', h)], out=junk[:, 0:128],
                             in_=fq_t[:, h * 128:(h + 1) * 128], func=AF.Square, accum_out=sm[:, 128 + h:129 + h])
                    S.op('act', 'activation', [('sm_i', h) for h in range(4)], ['sm_j'], out=sm[:, 132:136],
                         in_=sm[:, 128:132], func=AF.Sqrt, scale=1.0 / 128, bias=EPS)
                    S.op('dve', 'reciprocal', ['sm_j'], ['sm_j'], out=sm[:, 132:136], in_=sm[:, 132:136])
                    for h in range(4):
                        S.op('dve', 'scalar_tensor_tensor', ['sm_j', 'fq_t', 'gqk'], ['fqn'], out=fqn[:, h * 128:(h + 1) * 128],
                             in0=fq_t[:, h * 128:(h + 1) * 128], scalar=sm[:, 132 + h:133 + h], in1=gqk[:, w, :],
                             op0=ALU.mult, op1=ALU.mult)
                    for h in range(4):
                        S.op('pe', 'transpose', ['fqn', 'ident_b'], ['PB'], out=PB[:, h * 128:(h + 1) * 128],
                             in_=fqn[:, h * 128:(h + 1) * 128], identity=ident_b[:])
                    S.op('act', 'activation', [], ['PB', 'fT'], out=fT[:, :, j * 128:(j + 1) * 128],
                         in_=PB[:, 0:512].rearrange("p (c s) -> p c s", c=4), func=AF.Copy)
                    if j == 3:
                        dst = QTd if w == 0 else KTd
                        S.dma('pool', d_f, dst[:, :, t * 512:(t + 1) * 512].rearrange("h p s -> p h s"), fT,
                              R=['fT'], W=[('QK', w)])
                elif off == 1024:
                    S.op('dve', 'tensor_tensor', ['bb'], [pk, 'fv_aug'], out=fv_aug[:, :, j, 0:128],
                         in0=pt_[:, 0:512].rearrange("p (h e) -> p h e", h=4),
                         in1=bb[:, 1544 + 1024:1544 + 1536].rearrange("p (h e) -> p h e", h=4), op=ALU.add)
                    if j == 3:
                        S.dma('pool', d_f2, Vd[:, :, t * 4:(t + 1) * 4, :].rearrange("h p j e -> p h j e"), fv_aug,
                              R=['fv_aug'], W=['Vd'])
                else:
                    S.op('dve', 'tensor_tensor', ['bb'], [pk, 'sm_k'], out=sm[:, 136:140], in0=pt_[:, 0:4],
                         in1=bb[:, 1544 + 1536:1544 + 1540], op=ALU.add)
                    S.op('act', 'activation', ['sm_k'], ['sm_k'], out=sm[:, 136:140], in_=sm[:, 136:140], func=AF.Exp, scale=-1.0)
                    S.op('act', 'activation', ['sm_k'], ['sm_k'], out=sm[:, 136:140], in_=sm[:, 136:140], func=AF.Ln, bias=1.0)
                    S.op('pe', 'matmul', ['sm_k', 'cst_f'], ['PA'], PA[:, 0:4], lhsT=tri_f, rhs=sm[:, 136:140], start=True, stop=True)
                    S.op('pe', 'matmul', ['sm_k', 'cst_f'], ['PA'], PA[:, 4:8], lhsT=ones_f, rhs=sm[:, 136:140], start=True, stop=True)
                    if j == 0:
                        S.op('dve', 'tensor_scalar', ['Fcar'], ['rI'], out=rI[:, t, :], in0=Fcar[:], scalar1=-1.0, scalar2=None,
                             op0=ALU.mult)
                    S.op('dve', 'tensor_tensor', ['Fcar'], ['PA', 'Fall'], out=Fall[:, t * 4 + j, :], in0=PA[:, 0:4], in1=Fcar[:],
                         op=ALU.add)
                    S.op('dve', 'tensor_tensor', [], ['PA', 'Fcar'], out=Fcar[:], in0=PA[:, 4:8], in1=Fcar[:], op=ALU.add)
            proj_tok(WIN, kin, C_FQ, 1540, ev_ftok)

            S.barrier()
            ryT = vF(0, [128, 4, 512])
            xc = vF(2048, [128, 4, 512])
            lr = vF(4096, [128, 512])
            li = vF(4608, [128, 512])
            la_ = vF(5120, [128, 512])
            lu = vF(5632, [128, 512])
            lh = vF(6144, [128, 512])
            xcb = vB(0, [128, 4, 512])

            def ev_rx(mc, m, pt_, pk):
                if mc < 4:
                    S.op('act', 'activation', ['bfe'], [pk, 'rxy'], out=rxy[:, mc, 3:515], in_=pt_[:, :], func=AF.Identity,
                         bias=bfe[:, 8 + mc:9 + mc])
                else:
                    S.op('act', 'activation', ['bfe'], [pk, 'ryT'], out=ryT[:, mc - 4, :], in_=pt_[:, :], func=AF.Identity,
                         bias=bfe[:, 8 + mc:9 + mc])
            proj_feat(WIN, kin, C_RX, 1024, ev_rx)
            for c in range(4):
                S.op('dve', 'tensor_scalar', ['rxy'] + LP, ['xc'], out=xc[:, c, :], in0=rxy[:, c, 0:512],
                     scalar1=lpar[:, c:c + 1], scalar2=lpar[:, 16 + c:17 + c], op0=ALU.mult, op1=ALU.add)
                for k in range(1, 4):
                    S.op('dve', 'scalar_tensor_tensor', ['rxy'] + LP, ['xc'], out=xc[:, c, :], in0=rxy[:, c, k:k + 512],
                         scalar=lpar[:, 4 * k + c:4 * k + c + 1], in1=xc[:, c, :], op0=ALU.mult, op1=ALU.add)
            S.op('pool', 'tensor_copy', ['xc'], ['xcb'], out=xcb, in_=xc)
            S.op('pool', 'tensor_copy', [], ['rxy'], out=rxy[:, :, 0:3], in_=rxy[:, :, 512:515])
            for c in range(4):
                pj = next_pj()
                S.op('pe', 'matmul', ['wbd', 'xcb'], [('PJ', pj)], PJ[pj][:, :], lhsT=wbd[:, 0, c, :], rhs=xcb[:, c, :],
                     start=True, stop=True)
                S.op('act', 'activation', LP, [('PJ', pj), 'lr'], out=lr, in_=PJ[pj][:, :], func=AF.Sigmoid,
                     bias=lpar[:, 20 + c:21 + c])
                pj = next_pj()
                S.op('pe', 'matmul', ['wbd', 'xcb'], [('PJ', pj)], PJ[pj][:, :], lhsT=wbd[:, 1, c, :], rhs=xcb[:, c, :],
                     start=True, stop=True)
                S.op('act', 'activation', LP, [('PJ', pj), 'li'], out=li, in_=PJ[pj][:, :], func=AF.Sigmoid,
                     bias=lpar[:, 24 + c:25 + c])
                S.op('act', 'activation', ['lr'] + LP, ['la_'], out=la_, in_=lr, func=AF.Exp, scale=lpar[:, 32 + c:33 + c])
                S.op('act', 'activation', ['lr'] + LP, ['lu'], out=lu, in_=lr, func=AF.Exp, scale=lpar[:, 36 + c:37 + c])
                S.op('dve', 'tensor_scalar', ['lu'], ['lu'], out=lu, in0=lu, scalar1=-1.0, scalar2=1.0, op0=ALU.mult, op1=ALU.add)
                S.op('dve', 'tensor_scalar', ['lu'], ['lu'], out=lu, in0=lu, scalar1=0.0, scalar2=None, op0=ALU.max)
                S.op('act', 'activation', ['lu'], ['lu'], out=lu, in_=lu, func=AF.Sqrt)
                S.op('dve', 'tensor_tensor', ['li', 'xc'], ['li'], out=li, in0=li, in1=xc[:, c, :], op=ALU.mult)
                S.op('dve', 'tensor_tensor', ['li', 'lu'], ['lu'], out=lu, in0=lu, in1=li, op=ALU.mult)
                S.op('dve', 'tensor_tensor_scan', ['la_', 'lu', 'lcar'], ['lh'], out=lh, data0=la_, data1=lu,
                     initial=lcar[:, c:c + 1], op0=ALU.mult, op1=ALU.add)
                S.op('pool', 'tensor_copy', ['lh'], ['lcar'], out=lcar[:, c:c + 1], in_=lh[:, 511:512])
                yv = ryT[:, c, :]
                S.op('pool', 'tensor_tensor', ['ryT'], ['li'], out=li, in0=yv, in1=yv, op=ALU.mult)
                S.op('dve', 'tensor_scalar', ['li'], ['li'], out=li, in0=li, scalar1=0.044715, scalar2=1.0, op0=ALU.mult,
                     op1=ALU.add)
                S.op('dve', 'tensor_tensor', ['li', 'ryT'], ['li'], out=li, in0=li, in1=yv, op=ALU.mult)
                S.op('act', 'activation', ['li'], ['li'], out=li, in_=li, func=AF.Sigmoid, scale=1.5957691216)
                S.op('dve', 'tensor_tensor', ['li', 'ryT'], ['li'], out=li, in0=li, in1=yv, op=ALU.mult)
                S.op('dve', 'tensor_tensor', ['li', 'lh'], ['yTs'], out=yTs[:, c, :], in0=li, in1=lh, op=ALU.mult)
            yT_store(2, t)


        S.barrier()
        KTs = vB(0, [128, S_])
        Vs = vB(S_, [128, NB, 130])
        o0 = S_ + NB * 130
        QTb = [vB(o0, [128, 512]), vB(o0 + 2048, [128, 512])]
        Pb = vB(o0 + 512, [128, 2, 512])
        ob = vB(o0 + 1536, [128, 512])
        d_q2 = [d_p, d_g2]
        qcnt = 0
        biasI = vF(0, [128, NB])
        rd = vF(NB, [128, 4])
        d_k = d_f
        d_v = d_f2
        d_q = d_p
        OB = [PO[0], PO[1], PA, PS]
        OK = [('PO', 0), ('PO', 1), 'PA', 'PS']
        pcnt = 0
        for h in range(4):
            S.dma('sp', d_k, KTs, KTd[h], R=[('QK', 1)], W=['KTs'])
            S.dma('sp', d_v, Vs, Vd[h], R=['Vd'], W=['Vs'])
            for I in range(NT):
                qi = qcnt % 2
                qcnt += 1
                QTs = QTb[qi]
                S.dma('sp', d_q2[qi], QTs, QTd[h, :, I * 512:(I + 1) * 512], R=[('QK', 0)], W=[('QTs', qi)])
                nj = 4 * I + 4
                S.op('dve', 'tensor_scalar', ['Fall', 'rI'], ['biasI'], out=biasI[:, 0:nj], in0=Fall[:, 0:nj, h],
                     scalar1=rI[:, I, h:h + 1], scalar2=None, op0=ALU.add)
                for j in range(nj):
                    jj = j - 4 * I
                    q0 = 0 if jj <= 0 else jj * 128
                    nq = 512 - q0
                    pj = pcnt % 2
                    pcnt += 1
                    S.op('pe', 'matmul', ['KTs', ('QTs', qi)], [('PJ', pj)], PJ[pj][:, 0:nq], lhsT=KTs[:, j * 128:(j + 1) * 128],
                         rhs=QTs[:, q0:512], start=True, stop=True)
                    S.op('act', 'activation', ['biasI'], [('PJ', pj), ('Pb', pj)], out=Pb[:, pj, 0:nq], in_=PJ[pj][:, 0:nq],
                         func=AF.Exp, bias=biasI[:, j:j + 1])
                    if jj >= 0:
                        S.op('pool', 'tensor_tensor', ['mask4'], [('Pb', pj)], out=Pb[:, pj, 0:128], in0=Pb[:, pj, 0:128],
                             in1=mask4[:, 0, :], op=ALU.mult)
                    for qs in range(q0 // 128, 4):
                        last = (j == 4 * I + qs)
                        S.op('pe', 'matmul', [('Pb', pj), 'Vs'], [OK[qs]], OB[qs][:, 0:130],
                             lhsT=Pb[:, pj, qs * 128 - q0:(qs + 1) * 128 - q0], rhs=Vs[:, j, :], start=(j == 0), stop=last)
                for qs in range(4):
                    S.op('dve', 'reciprocal', [], [OK[qs], ('rd', qs)], out=rd[:, qs:qs + 1], in_=OB[qs][:, 128:129])
                    S.op('dve', 'tensor_scalar', [('rd', qs)], [OK[qs], 'ob'], out=ob[:, qs * 128:(qs + 1) * 128],
                         in0=OB[qs][:, 0:128], scalar1=rd[:, qs:qs + 1], scalar2=None, op0=ALU.mult)
                for qs in range(4):
                    S.op('pe', 'transpose', ['ob', 'ident_b'], ['PB'], out=PB[:, qs * 128:(qs + 1) * 128],
                         in_=ob[:, qs * 128:(qs + 1) * 128], identity=ident_b[:])
                S.op('act', 'activation', [], ['PB', 'yTs'], out=yTs[:, 0, :], in_=PB[:, 0:512], func=AF.Copy)
                S.dma('pool', d_y, yT[4 + h, :, I * 512:(I + 1) * 512], yTs[:, 0, :], R=['yTs'], W=[('yT', 1)])

        S.barrier()
        ybr = vB(0, [128, 16, 512])
        mgb = vB(8192, [128, 8, 512])
        aT = vB(12288, [128, 16, 512])
        mg = vF(0, [128, 8, 512])
        sg = vF(4096, [128, 512])
        rl = vF(4608, [128, 512])
        d_yb = d_f
        d_o = d_f2
        accs = [PO[0], PO[1], PA, PS]
        acck = [('PO', 0), ('PO', 1), 'PA', 'PS']
        load_x(x_src, 0, 0)
        for t in range(NT):
            xi = t % 2
            xt = xts[xi]
            xk = ('xt', xi)
            cur['hi'] = 0
            hT = hTs[0]
            norm_from_xt(g1, 'g1', xi, 0)
            if t + 1 < NT:
                load_x(x_src, t + 1, (t + 1) % 2)
            S.dma('sp', d_yb, ybr, yT[:, :, t * 512:(t + 1) * 512].rearrange("c p s -> p c s"),
                  R=[('yT', b) for b in range(4)], W=['ybr'])
            for n in range(4):
                for half in range(2):
                    i = load_w(wb_br[l][n * 512:(n + 1) * 512, half * 512:(half + 1) * 512], 512, ('wb_br', l), kch=4)
                    gi = load_w(WIN[:, C_GT + n * 1024 + half * 512:C_GT + n * 1024 + half * 512 + 512], 512, kin)
                    for m in range(4):
                        dc = half * 4 + m
                        pj = next_pj()
                        for kc in range(8):
                            S.op('pe', 'matmul', [('wsl', gi), ('hT', 0, kc)], [('PJ', pj)], PJ[pj][:, :],
                                 lhsT=wsl[gi][:, kc, m * 128:(m + 1) * 128], rhs=hTs[0][:, kc, :], start=(kc == 0), stop=(kc == 7))
                        S.op('act', 'activation', ['bfe'], [('PJ', pj), 'sg'], out=sg, in_=PJ[pj][:, :], func=AF.Sigmoid,
                             bias=bfe[:, 21 + n * 8 + dc:22 + n * 8 + dc])
                        pj2 = next_pj()
                        for kc in range(4):
                            S.op('pe', 'matmul', [('wsl', i), 'ybr'], [('PJ', pj2)], PJ[pj2][:, :],
                                 lhsT=wsl[i][:, kc, m * 128:(m + 1) * 128], rhs=ybr[:, n * 4 + kc, :], start=(kc == 0),
                                 stop=(kc == 3))
                        if n == 0:
                            S.op('dve', 'tensor_tensor', ['sg'], [('PJ', pj2), ('mg', dc)], out=mg[:, dc, :], in0=PJ[pj2][:, :],
                                 in1=sg, op=ALU.mult)
                        else:
                            S.op('dve', 'tensor_tensor', [], [('PJ', pj2), 'sg'], out=sg, in0=PJ[pj2][:, :], in1=sg,
                                 op=ALU.mult)
                            S.op('pool', 'tensor_tensor', ['sg'], [('mg', dc)], out=mg[:, dc, :], in0=mg[:, dc, :], in1=sg,
                                 op=ALU.add)
            for dc in range(8):
                S.op('pool', 'tensor_copy', [('mg', dc)], ['mgb'], out=mgb[:, dc, :], in_=mg[:, dc, :])
            for half in range(2):
                wi = load_w(wb_o[l][:, half * 512:(half + 1) * 512], 512, ('wb_o', l))
                for j in range(4):
                    pj = next_pj()
                    for kc in range(8):
                        S.op('pe', 'matmul', [('wsl', wi), 'mgb'], [('PJ', pj)], PJ[pj][:, :],
                             lhsT=mgb[:, kc, j * 128:(j + 1) * 128], rhs=wsl[wi][:, kc, :], start=(kc == 0), stop=(kc == 7))
                    S.op('dve', 'tensor_tensor', [], [('PJ', pj), xk], out=xt[:, j, half * 512:(half + 1) * 512],
                         in0=PJ[pj][:, :], in1=xt[:, j, half * 512:(half + 1) * 512], op=ALU.add)
            norm_from_xt(g2, 'g2', xi, 1)
            for fh in range(2):
                for grp in range(4):
                    wi = load_w(wb_up[l][:, fh * 2048 + grp * 512:fh * 2048 + (grp + 1) * 512], 512, ('wb_up', l))
                    for m in range(4):
                        fc = grp * 4 + m
                        pj = next_pj()
                        for kc in range(8):
                            S.op('pe', 'matmul', [('wsl', wi), ('hT', 1, kc)], [('PJ', pj)], PJ[pj][:, :],
                                 lhsT=wsl[wi][:, kc, m * 128:(m + 1) * 128], rhs=hTs[1][:, kc, :], start=(kc == 0), stop=(kc == 7))
                        S.op('act', 'activation', [], [('PJ', pj), 'rl'], out=rl, in_=PJ[pj][:, :], func=AF.Relu)
                        S.op('pool', 'tensor_tensor', ['rl'], [('aT', fc)], out=aT[:, fc, :], in0=rl, in1=rl, op=ALU.mult)
                for half in range(2):
                    for grp in range(2):
                        r0 = fh * 2048 + grp * 1024
                        wi = load_w(wb_dn[l][r0:r0 + 1024, half * 512:(half + 1) * 512], 512, ('wb_dn', l))
                        for j in range(4):
                            for kc in range(8):
                                fc = grp * 8 + kc
                                S.op('pe', 'matmul', [('wsl', wi), ('aT', fc)], [acck[j]], accs[j][:, :],
                                     lhsT=aT[:, fc, j * 128:(j + 1) * 128], rhs=wsl[wi][:, kc, :], start=(fc == 0), stop=(fc == 15))
                    for j in range(4):
                        S.op('dve', 'tensor_tensor', [], [acck[j], xk], out=xt[:, j, half * 512:(half + 1) * 512],
                             in0=accs[j][:, :], in1=xt[:, j, half * 512:(half + 1) * 512], op=ALU.add)
            S.dma('pool', d_o, x_dst[t * 512:(t + 1) * 512, :].rearrange("(j p) d -> p j d", p=128), xt[:],
                  R=[xk], W=[('xs', id(x_dst))])

    S.barrier()
    S.emit()
    print("sbuf remaining", nc.sbuf_bytes_remaining, flush=True)
    print("instr counts", S.ecnt, "waits", S.nwaits, "dsems", S.ndsem, flush=True)
    return nc, es


def make_consts():
    c = np.zeros((128, 384), np.float32)
    c[:, 0:128] = np.eye(128, dtype=np.float32)
    c[:, 128:256] = np.triu(np.ones((128, 128), np.float32))
    c[:, 256:384] = 1.0
    return c


_CACHE = {}


def run(inputs, dbg=False):
    x = np.ascontiguousarray(inputs["x"], dtype=np.float32)
    B, S_, _ = x.shape
    NL = inputs["w_in"].shape[0]
    key = (S_, NL, dbg)
    if key not in _CACHE:
        _CACHE[key] = build(S_, NL, dbg)
    nc, _es = _CACHE[key]
    cst = make_consts()
    in_maps = []
    for b in range(B):
        m = {"x": x[b], "cst": cst}
        for k in PNAMES:
            m[k] = np.ascontiguousarray(inputs[k], dtype=np.float32)
        in_maps.append(m)
    res = run_bass_kernel_spmd(nc, in_maps, core_ids=list(range(B)))
    return res


def kernel(**inputs):
    res = run(inputs)
    return np.stack([r["out"] for r in res.results], axis=0).astype(np.float32)
```

```python
import os
import numpy as np
import concourse.bass as bass
import concourse.mybir as mybir
from concourse.bass_utils import run_bass_kernel_spmd
from contextlib import ExitStack

F32 = mybir.dt.float32
BF16 = mybir.dt.bfloat16
AF = mybir.ActivationFunctionType
ALU = mybir.AluOpType

D = 1024
NIN = 10268
DFF = 4096
EPS = 1e-6
C_MQ, C_MK, C_MV, C_MO, C_MI, C_MF = 0, 512, 1024, 1536, 2048, 2052
C_FQ, C_FK, C_FV, C_FF = 2056, 2568, 3080, 3592
C_RX, C_RY = 3596, 4108
C_GQ, C_GK, C_GV, C_GG, C_GA = 4620, 4876, 5132, 5644, 6156
C_GT = 6172


class DSem:
    def __init__(self, h):
        self.h = h
        self.count = 0


class Sync:
    ENG = ('pe', 'act', 'dve', 'pool', 'sp')

    def __init__(self, nc, es):
        self.nc = nc
        self.es = es
        self.esem = {k: es.enter_context(nc.semaphore('s_' + k)) for k in self.ENG}
        self.ecnt = {k: 0 for k in self.ENG}
        self.seen = {k: {} for k in self.ENG}
        self.prog = {k: [] for k in self.ENG}
        self.lastw = {}
        self.readers = {}
        self.nwaits = 0
        self.ndsem = 0
        self.dsems = []

    def dsem(self, name=None, nobar=False):
        self.ndsem += 1
        d = DSem(self.es.enter_context(self.nc.semaphore(name or ('d%d' % self.ndsem))))
        if not nobar:
            self.dsems.append(d)
        return d

    def barrier(self):
        for e in self.ENG:
            for o in self.ENG:
                if o != e and self.ecnt[o] > 0:
                    self._wait(e, (self.esem[o], self.ecnt[o]))
            for d in self.dsems:
                if d.count > 0:
                    self._wait(e, (d.h, d.count))

    def _wait(self, eng, tok):
        sem, val = tok
        if eng == 'pe' and sem is self.esem['pe']:
            return
        s = self.seen[eng]
        if s.get(id(sem), 0) >= val:
            return
        self.prog[eng].append(('w', sem, val))
        self.nwaits += 1
        s[id(sem)] = val

    def _deps(self, eng, R, W):
        for k in R:
            t = self.lastw.get(k)
            if t:
                self._wait(eng, t)
        for k in W:
            t = self.lastw.get(k)
            if t:
                self._wait(eng, t)
            for t in self.readers.get(k, {}).values():
                self._wait(eng, t)

    def _record(self, tok, R, W):
        for k in W:
            self.lastw[k] = tok
            self.readers[k] = {}
        for k in R:
            self.readers.setdefault(k, {})[id(tok[0])] = tok

    def op(self, eng, meth, R, W, *args, **kw):
        self._deps(eng, R, W)
        self.ecnt[eng] += 1
        self.prog[eng].append(('i', meth, args, kw, self.esem[eng], 1))
        self._record((self.esem[eng], self.ecnt[eng]), R, W)

    def dma(self, q, ds, out, in_, R=(), W=(), **kw):
        self._deps(q, R, W)
        ds.count += 16
        self.prog[q].append(('i', 'dma_start', (), dict(out=out, in_=in_, **kw), ds.h, 16))
        self._record((ds.h, ds.count), R, W)

    def wait_all(self, eng, keys):
        for k in keys:
            t = self.lastw.get(k)
            if t:
                self._wait(eng, t)

    def emit(self):
        nc = self.nc

        def replay(name, e):
            for it in self.prog[name]:
                if it[0] == 'w':
                    e.wait_ge(it[1], it[2])
                else:
                    _, meth, args, kw, sem, inc = it
                    getattr(e, meth)(*args, **kw).then_inc(sem, inc)

        with nc.Block() as block:
            @block.sync
            def _(e):
                replay('sp', e)

            @block.tensor
            def _(e):
                replay('pe', e)

            @block.scalar
            def _(e):
                replay('act', e)

            @block.vector
            def _(e):
                replay('dve', e)

            @block.gpsimd
            def _(e):
                replay('pool', e)


PNAMES = ["norm1_g", "w_in", "b_in", "ml_norm_g", "fx_qnorm_g", "fx_knorm_g", "lru_conv_w", "lru_conv_b",
          "lru_wa", "lru_ba", "lru_wx", "lru_bx", "lru_lambda", "gla_w_alpha", "gla_b_alpha", "gla_norm_g",
          "w_branch", "w_o", "norm2_g", "w_up", "w_down"]


def build(S_, NL, dbg=False):
    NT = S_ // 512
    NB = S_ // 128
    nc = bass.Bass("TRN2", target_bir_lowering=False)
    es = ExitStack()
    es.enter_context(nc.allow_non_contiguous_dma(reason="small param loads"))
    S = Sync(nc, es)

    def dram(name, shape, dt, kind="Internal"):
        return nc.dram_tensor(name, shape, dt, kind=kind).ap()

    x_in = dram("x", [S_, D], F32, "ExternalInput")
    cst = dram("cst", [128, 384], F32, "ExternalInput")
    shp = dict(norm1_g=[NL, D], w_in=[NL, D, NIN], b_in=[NL, NIN], ml_norm_g=[NL, 512], fx_qnorm_g=[NL, 128],
               fx_knorm_g=[NL, 128], lru_conv_w=[NL, 4, 512], lru_conv_b=[NL, 512], lru_wa=[NL, 8, 64, 64],
               lru_ba=[NL, 512], lru_wx=[NL, 8, 64, 64], lru_bx=[NL, 512], lru_lambda=[NL, 512],
               gla_w_alpha=[NL, 16, 256], gla_b_alpha=[NL, 256], gla_norm_g=[NL, 512],
               w_branch=[NL, 4, 512, D], w_o=[NL, D, D], norm2_g=[NL, D], w_up=[NL, D, DFF], w_down=[NL, DFF, D])
    P = {k: dram(k, shp[k], F32, "ExternalInput") for k in PNAMES}
    out = dram("out", [S_, D], F32, "ExternalOutput")
    dbg_y = dram("dbg_y", [16, 128, S_], BF16, "ExternalOutput") if dbg else None

    wb_in = dram("wb_in", [NL, D, NIN], BF16)
    wb_br = dram("wb_br", [NL, 2048, D], BF16)
    wb_o = dram("wb_o", [NL, D, D], BF16)
    wb_up = dram("wb_up", [NL, D, DFF], BF16)
    wb_dn = dram("wb_dn", [NL, DFF, D], BF16)
    xs = [dram("xs0", [S_, D], F32), dram("xs1", [S_, D], F32)]
    yT = dbg_y if dbg else dram("yT", [16, 128, S_], BF16)
    QTd = dram("QTd", [4, 128, S_], BF16)
    KTd = dram("KTd", [4, 128, S_], BF16)
    Vd = dram("Vd", [4, 128, NB, 130], BF16)

    def sb(name, shape, dt):
        return es.enter_context(nc.sbuf_tensor(name, shape, dt))

    def ps(name, shape, dt):
        return es.enter_context(nc.psum_tensor(name, shape, dt))

    def cast_w(src, dst, key):
        ds = S.dsem(nobar=True)
        n = 1
        for s_ in src.shape:
            n *= s_
        rows = n // 2048
        pat = " ".join("abcd"[:len(src.shape)])
        sv = src.rearrange(f"{pat} -> ({pat})").rearrange("(r c) -> r c", c=2048)
        pat = " ".join("abcd"[:len(dst.shape)])
        dv = dst.rearrange(f"{pat} -> ({pat})").rearrange("(r c) -> r c", c=2048)
        r0 = 0
        while r0 < rows:
            r1 = min(rows, r0 + 2048)
            S.dma('pool', ds, dv[r0:r1, :], sv[r0:r1, :], W=[key])
            r0 = r1

    for l in range(NL):
        cast_w(P["w_in"][l], wb_in[l], ('wb_in', l))
        cast_w(P["w_branch"][l], wb_br[l], ('wb_br', l))
        cast_w(P["w_o"][l], wb_o[l], ('wb_o', l))
        cast_w(P["w_up"][l], wb_up[l], ('wb_up', l))
        cast_w(P["w_down"][l], wb_dn[l], ('wb_dn', l))

    cst_f = sb("cst_f", [128, 384], F32)
    d_c = S.dsem()
    S.dma('sp', d_c, cst_f[:], cst, W=['cst_f'])
    ident_f = cst_f[:, 0:128]
    tri_f = cst_f[:, 128:256]
    ones_f = cst_f[:, 256:384]
    ident_b = sb("ident_b", [128, 128], BF16)
    mask4 = sb("mask4", [128, 4, 128], BF16)
    S.op('dve', 'tensor_copy', ['cst_f'], ['ident_b'], out=ident_b[:], in_=ident_f)
    for h in range(4):
        S.op('dve', 'tensor_copy', ['cst_f'], ['mask4'], out=mask4[:, h, :], in_=tri_f)

    xts = [sb("xt0", [128, 4, D], F32), sb("xt1", [128, 4, D], F32)]
    junk = sb("junk", [128, D], F32)
    hTs = [sb("hT0", [128, 8, 512], BF16), sb("hT1", [128, 8, 512], BF16)]
    NW = 3
    wsl = [sb("wsl%d" % i, [128, 8, 512], BF16) for i in range(NW)]
    d_wsl = [S.dsem() for _ in range(NW)]
    d_xs = [S.dsem(), S.dsem()]
    cur = dict(hi=0)
    st1 = sb("st1", [128, 16], F32)
    bfe = sb("bfe", [128, 64], F32)
    g1 = sb("g1", [128, 8], F32)
    g2 = sb("g2", [128, 8], F32)
    sm = sb("sm", [128, 160], F32)
    yTs = sb("yTs", [128, 4, 512], BF16)
    ytok = sb("ytok", [128, 512], BF16)
    yTs2 = sb("yTs2", [128, 4, 512], BF16)
    ytok2 = sb("ytok2", [128, 512], BF16)
    junk2 = sb("junk2", [128, 128], F32)
    gbc2 = sb("gbc2", [128, 512], F32)
    d_y2 = S.dsem()
    d_g2 = S.dsem()
    Fall = sb("Fall", [128, NB, 4], F32)
    rI = sb("rI", [128, NT, 4], F32)
    d_p = S.dsem()
    d_y = S.dsem()
    bb = sb("bb", [128, 4108], F32)
    mif = sb("mif", [128, 4, 8], F32)
    Cst = sb("Cst", [128, 4, 130], F32)
    Cbf = sb("Cbf", [128, 4, 130], BF16)
    gbc = sb("gbc", [128, 512], F32)
    gqk = sb("gqk", [128, 2, 128], F32)
    Fcar = sb("Fcar", [128, 4], F32)
    rxy = sb("rxy", [128, 4, 515], F32)
    lcar = sb("lcar", [128, 4], F32)
    lpar = sb("lpar", [128, 64], F32)
    wbd = sb("wbd", [128, 2, 4, 128], BF16)
    wal_f = sb("wal_f", [16, 256], F32)
    wal = sb("wal", [16, 256], BF16)
    gpar = sb("gpar", [128, 8], F32)
    Sst = sb("Sst", [128, 2, 128], F32)
    Sbf = sb("Sbf", [128, 2, 128], BF16)
    d_f = S.dsem()
    d_f2 = S.dsem()
    bigB = sb("bigB", [128, 20480], BF16)
    bigF = sb("bigF", [128, 8192], F32)

    def vB(off, shape):
        n = 1
        for s_ in shape[1:]:
            n *= s_
        v = bigB[:, off:off + n]
        if len(shape) == 3:
            v = v.rearrange("p (a b) -> p a b", a=shape[1])
        elif len(shape) == 4:
            v = v.rearrange("p (a b c) -> p a b c", a=shape[1], b=shape[2])
        return v

    def vF(off, shape):
        n = 1
        for s_ in shape[1:]:
            n *= s_
        v = bigF[:, off:off + n]
        if len(shape) == 3:
            v = v.rearrange("p (a b) -> p a b", a=shape[1])
        return v

    PJ = [ps("PJ0", [128, 512], F32), ps("PJ1", [128, 512], F32)]
    PT = ps("PT", [128, 512], F32)
    PB = ps("PB", [128, 1024], BF16)
    PA = ps("PA", [128, 512], F32)
    PO = [ps("PO0", [128, 512], F32), ps("PO1", [128, 512], F32)]
    PS = ps("PS", [128, 512], F32)

    state = dict(wslot=0, pj=0)

    def load_w(src_ap, ncols, rkey, kch=8):
        i = state['wslot']
        state['wslot'] = (i + 1) % NW
        S.dma('sp', d_wsl[i], wsl[i][:, 0:kch, 0:ncols], src_ap.rearrange("(c p) n -> p c n", p=128),
              R=[rkey], W=[('wsl', i)])
        return i

    def next_pj():
        i = state['pj']
        state['pj'] ^= 1
        return i

    def norm_from_xt(gt, gkey, xi, hi):
        xt = xts[xi]
        xk = ('xt', xi)
        hT = hTs[hi]
        for j in range(4):
            S.op('act', 'activation', [xk], ['junk', ('st1', j)], out=junk[:], in_=xt[:, j, :], func=AF.Square,
                 accum_out=st1[:, j:j + 1])
        S.op('act', 'activation', [('st1', j) for j in range(4)], ['st1b'], out=st1[:, 4:8], in_=st1[:, 0:4],
             func=AF.Sqrt, scale=1.0 / D, bias=EPS)
        S.op('dve', 'reciprocal', ['st1b'], ['st1c'], out=st1[:, 8:12], in_=st1[:, 4:8])
        for j in range(4):
            S.op('dve', 'tensor_scalar', [xk, 'st1c'], ['junk'], out=junk[:], in0=xt[:, j, :],
                 scalar1=st1[:, 8 + j:9 + j], scalar2=None, op0=ALU.mult)
            for half in range(2):
                for q in range(4):
                    c = half * 4 + q
                    S.op('pe', 'transpose', ['junk', 'cst_f'], ['PT'], out=PT[:, q * 128:(q + 1) * 128],
                         in_=junk[:, c * 128:(c + 1) * 128], identity=ident_f)
                for q in range(4):
                    c = half * 4 + q
                    S.op('dve', 'tensor_scalar', [gkey], ['PT', ('hT', hi, c)], out=hT[:, c, j * 128:(j + 1) * 128],
                         in0=PT[:, q * 128:(q + 1) * 128], scalar1=gt[:, c:c + 1], scalar2=None, op0=ALU.mult)

    def load_x(src_dram, t, xi):
        S.dma('sp', d_xs[xi], xts[xi][:], src_dram[t * 512:(t + 1) * 512, :].rearrange("(j p) d -> p j d", p=128),
              R=[('xs', id(src_dram))], W=[('xt', xi)])

    def proj_feat(wsrc, wkey, c0, ncols, evac):
        done = 0
        while done < ncols:
            n = min(512, ncols - done)
            wi = load_w(wsrc[:, c0 + done:c0 + done + n], n, wkey)
            mc = 0
            while mc * 128 < n:
                m = min(128, n - mc * 128)
                pj = next_pj()
                for kc in range(8):
                    S.op('pe', 'matmul', [('wsl', wi), ('hT', cur['hi'], kc)], [('PJ', pj)], PJ[pj][0:m, :],
                         lhsT=wsl[wi][:, kc, mc * 128:mc * 128 + m], rhs=hTs[cur['hi']][:, kc, :], start=(kc == 0), stop=(kc == 7))
                evac((done + mc * 128) // 128, m, PJ[pj], ('PJ', pj))
                mc += 1
            done += n

    def proj_tok(wsrc, wkey, c0, ncols, evac):
        done = 0
        while done < ncols:
            n = min(512, ncols - done)
            wi = load_w(wsrc[:, c0 + done:c0 + done + n], n, wkey)
            for j in range(4):
                pj = next_pj()
                for kc in range(8):
                    S.op('pe', 'matmul', [('wsl', wi), ('hT', cur['hi'], kc)], [('PJ', pj)], PJ[pj][:, 0:n],
                         lhsT=hTs[cur['hi']][:, kc, j * 128:(j + 1) * 128], rhs=wsl[wi][:, kc, 0:n], start=(kc == 0), stop=(kc == 7))
                evac(j, done, n, PJ[pj], ('PJ', pj))
            done += n

    def yT_store(br, t, alt=False):
        ys, yk, dsy = (yTs2, 'yTs2', d_y2) if alt else (yTs, 'yTs', d_y)
        S.dma('pool', dsy, yT[br * 4:(br + 1) * 4, :, t * 512:(t + 1) * 512].rearrange("c p s -> p c s"),
              ys[:], R=[yk], W=[('yT', br)])

    def tok_to_yTs(j, alt=False):
        ys, yk, yt_, ytk = (yTs2, 'yTs2', ytok2, 'ytok2') if alt else (yTs, 'yTs', ytok, 'ytok')
        for c in range(4):
            S.op('pe', 'transpose', [ytk, 'ident_b'], ['PB'], out=PB[:, c * 128:(c + 1) * 128],
                 in_=yt_[:, c * 128:(c + 1) * 128], identity=ident_b[:])
        S.op('act', 'activation', [], ['PB', yk], out=ys[:, :, j * 128:(j + 1) * 128],
             in_=PB[:, 0:512].rearrange("p (c s) -> p c s", c=4), func=AF.Copy)

    for l in range(NL):
        x_src = x_in if l == 0 else xs[(l - 1) % 2]
        x_dst = out if l == NL - 1 else xs[l % 2]
        WIN = wb_in[l]
        kin = ('wb_in', l)
        S.barrier()
        bi = P["b_in"][l]
        S.dma('sp', d_p, g1[:], P["norm1_g"][l].rearrange("(c p) -> p c", p=128), W=['g1'])
        S.dma('sp', d_p, g2[:], P["norm2_g"][l].rearrange("(c p) -> p c", p=128), W=['g2'])
        S.dma('sp', d_p, bb[:, 0:1544], bi[512:2056].partition_broadcast(128), W=['bb'])
        S.dma('sp', d_p, bb[:, 1544:3084], bi[2056:3596].partition_broadcast(128), W=['bb'])
        S.dma('sp', d_p, bb[:, 3084:4108], bi[5132:6156].partition_broadcast(128), W=['bb'])
        S.dma('sp', d_p, bfe[:, 0:8], bi[0:1024].rearrange("(c p) -> p c", p=128), W=['bfe'])
        S.dma('sp', d_p, bfe[:, 8:16], bi[C_RX:C_RX + 1024].rearrange("(c p) -> p c", p=128), W=['bfe'])
        S.dma('sp', d_p, bfe[:, 16:20], bi[C_GQ:C_GQ + 512].rearrange("(c p) -> p c", p=128), W=['bfe'])
        S.dma('sp', d_p, bfe[0:16, 20:21], bi[C_GA:C_GA + 16].rearrange("(p c) -> p c", c=1), W=['bfe'])
        S.dma('sp', d_p, bfe[:, 21:53], bi[C_GT:C_GT + 4096].rearrange("(c p) -> p c", p=128), W=['bfe'])
        S.dma('sp', d_p, gqk[:, 0, :], P["fx_qnorm_g"][l].partition_broadcast(128), W=['gqk'])
        S.dma('sp', d_p, gqk[:, 1, :], P["fx_knorm_g"][l].partition_broadcast(128), W=['gqk'])
        for k_ in range(4):
            S.dma('sp', d_p, lpar[:, 4 * k_:4 * k_ + 4], P["lru_conv_w"][l, k_].rearrange("(c p) -> p c", p=128), W=['lpar'])
        for i_, nm in enumerate(["lru_conv_b", "lru_ba", "lru_bx", "lru_lambda"]):
            S.dma('sp', d_p, lpar[:, 16 + 4 * i_:20 + 4 * i_], P[nm][l].rearrange("(c p) -> p c", p=128), W=['lpar'])
        LP = ['lpar', 'lpard', 'lpare']
        wbd_f = junk[:, :].rearrange("p (w c k) -> p w c k", w=2, c=4)
        S.op('pool', 'memset', [], ['junk'], junk[:], 0.0)
        for wi_, nm in enumerate(["lru_wa", "lru_wx"]):
            for g in range(8):
                c, po = g // 2, (g % 2) * 64
                S.dma('sp', d_p, wbd_f[po:po + 64, wi_, c, po:po + 64], P[nm][l, g], R=[], W=['junk'])
        S.dma('sp', d_p, wal_f[:], P["gla_w_alpha"][l], W=['wal_f'])
        S.dma('sp', d_p, gpar[:, 0:2], P["gla_b_alpha"][l].rearrange("(c p) -> p c", p=128), W=['gpar'])
        S.barrier()
        S.op('pool', 'tensor_scalar', ['gqk'], ['gqk'], out=gqk[:, 0, :], in0=gqk[:, 0, :], scalar1=float(128 ** -0.5),
             scalar2=None, op0=ALU.mult)
        S.op('act', 'activation', ['lpar'], ['lparb'], out=lpar[:, 40:44], in_=lpar[:, 28:32], func=AF.Exp, scale=-1.0)
        S.op('act', 'activation', ['lparb'], ['lparc'], out=lpar[:, 44:48], in_=lpar[:, 40:44], func=AF.Ln, bias=1.0)
        S.op('dve', 'tensor_scalar', ['lparc'], ['lpard'], out=lpar[:, 32:36], in0=lpar[:, 44:48], scalar1=-8.0,
             scalar2=None, op0=ALU.mult)
        S.op('dve', 'tensor_scalar', ['lparc'], ['lpare'], out=lpar[:, 36:40], in0=lpar[:, 44:48], scalar1=-16.0,
             scalar2=None, op0=ALU.mult)
        S.op('pool', 'tensor_copy', ['junk'], ['wbd'], out=wbd[:], in_=wbd_f)
        S.op('pool', 'tensor_copy', ['wal_f'], ['wal'], out=wal[:], in_=wal_f[:])
        S.op('pool', 'tensor_scalar', ['gpar'], ['gparb'], out=gpar[:, 2:4], in0=gpar[:, 0:2], scalar1=-1.0,
             scalar2=None, op0=ALU.mult)
        for tl, key in [(Cst, 'Cst'), (Cbf, 'Cbf'), (Sst, 'Sst'), (Sbf, 'Sbf'), (lcar, 'lcar'), (Fcar, 'Fcar'), (rxy, 'rxy')]:
            S.op('pool', 'memset', [], [key], tl[:], 0.0)
        S.barrier()

        load_x(x_src, 0, 0)
        norm_from_xt(g1, 'g1', 0, 0)
        for t in range(NT):
            cur['hi'] = t % 2
            if t + 1 < NT:
                load_x(x_src, t + 1, (t + 1) % 2)

            S.barrier()
            gqT = vF(0, [128, 2, 512])
            gkT = vF(1024, [128, 2, 512])
            gsp = vF(2048, [128, 2, 512])
            gcs = vF(3072, [128, 2, 512])
            geq = vF(4096, [128, 2, 512])
            gek = vF(5120, [128, 2, 512])
            gsil = vF(6144, [128, 4, 512])
            qtT = vB(0, [128, 2, 512])
            ktT = vB(1024, [128, 2, 512])
            kt_tok = vB(2048, [128, 256])
            gv_t = vB(2304, [128, 4, 512])
            Ab = vB(4352, [128, 4, 128])
            gaT = vB(4864, [128, 512])
            S.dma('sp', d_g2, gbc2[:], P["gla_norm_g"][l].partition_broadcast(128), W=['gbc2'])

            def ev_gq(mc, m, pt_, pk):
                dst = gqT if mc < 2 else gkT
                S.op('act', 'activation', ['bfe'], [pk, ('gqk_', mc)], out=dst[:, mc % 2, :], in_=pt_[:, :], func=AF.Identity,
                     bias=bfe[:, 16 + mc:17 + mc])
            proj_feat(WIN, kin, C_GQ, 512, ev_gq)

            def ev_ga(mc, m, pt_, pk):
                S.op('act', 'activation', ['bfe'], [pk, 'gaT'], out=gaT[0:16, :], in_=pt_[0:16, :], func=AF.Identity,
                     bias=bfe[0:16, 20:21])
            proj_feat(WIN, kin, C_GA, 16, ev_ga)

            def ev_gtok(j, off, n, pt_, pk):
                if off == 0:
                    S.op('dve', 'tensor_tensor', ['bb'], [pk, 'gv_t'], out=gv_t[:, j, :], in0=pt_[:, 0:512],
                         in1=bb[:, 3084:3596], op=ALU.add)
                else:
                    S.op('dve', 'tensor_tensor', ['bb'], [pk, 'gsil'], out=gsil[:, j, :], in0=pt_[:, 0:512],
                         in1=bb[:, 3596:4108], op=ALU.add)
                    S.op('act', 'activation', ['gsil'], ['junk'], out=junk[:, 0:512], in_=gsil[:, j, :], func=AF.Sigmoid)
                    S.op('dve', 'tensor_tensor', ['junk'], ['gsil'], out=gsil[:, j, :], in0=gsil[:, j, :],
                         in1=junk[:, 0:512], op=ALU.mult)
                    S.op('pool', 'tensor_tensor', ['gbc2'], ['gsil'], out=gsil[:, j, :], in0=gsil[:, j, :], in1=gbc2[:],
                         op=ALU.mult)
            proj_tok(WIN, kin, C_GV, 1024, ev_gtok)
            for c in range(2):
                pj = next_pj()
                S.op('pe', 'matmul', ['wal', 'gaT'], [('PJ', pj)], PJ[pj][:, :], lhsT=wal[:, c * 128:(c + 1) * 128],
                     rhs=gaT[0:16, :], start=True, stop=True)
                S.op('act', 'activation', ['gparb'], [('PJ', pj), 'gsp'], out=gsp[:, c, :], in_=PJ[pj][:, :], func=AF.Exp,
                     scale=-1.0, bias=gpar[:, 2 + c:3 + c])
                S.op('act', 'activation', ['gsp'], ['gsp'], out=gsp[:, c, :], in_=gsp[:, c, :], func=AF.Ln, bias=1.0)
                for j in range(4):
                    js = slice(j * 128, (j + 1) * 128)
                    S.op('dve', 'tensor_tensor_scan', ['gsp', 'cst_f'], ['gcs'], out=gcs[:, c, js], data0=ones_f,
                         data1=gsp[:, c, js], initial=0.0, op0=ALU.mult, op1=ALU.add)
                S.op('act', 'activation', ['gcs'], ['geq'], out=geq[:, c, :], in_=gcs[:, c, :], func=AF.Exp, scale=-1.0 / 16)
                S.op('act', 'activation', ['gcs'], ['gek'], out=gek[:, c, :], in_=gcs[:, c, :], func=AF.Exp, scale=1.0 / 16)
                S.op('dve', 'scalar_tensor_tensor', [('gqk_', c), 'geq'], ['qtT'], out=qtT[:, c, :], in0=gqT[:, c, :],
                     scalar=0.125, in1=geq[:, c, :], op0=ALU.mult, op1=ALU.mult)
                S.op('dve', 'tensor_tensor', [('gqk_', 2 + c), 'gek'], ['ktT'], out=ktT[:, c, :], in0=gkT[:, c, :],
                     in1=gek[:, c, :], op=ALU.mult)
            S.barrier()
            mqT = vB(5376, [128, 4, 512])
            mkT = vB(7424, [128, 4, 512])
            mk_tok = vB(9472, [128, 4, 512])
            mv_aug = vB(11520, [128, 4, 4, 130])
            STbs = [vB(13600, [128, 4, 128]), vB(14624, [128, 4, 128])]
            kd = vB(14112, [128, 4, 128])
            gs_ml = vF(0, [128, 4, 512])
            ytmp = vF(2048, [128, 4, 130])
            S.op('pool', 'memset', [], ['mv_aug'], mv_aug, 1.0)
            S.dma('sp', d_p, gbc[:], P["ml_norm_g"][l].partition_broadcast(128), W=['gbc'])

            def ev_mq(mc, m, pt_, pk):
                if mc < 4:
                    S.op('dve', 'tensor_scalar', ['bfe'], [pk, ('mqk', mc)], out=mqT[:, mc, :], in0=pt_[:, :],
                         scalar1=bfe[:, mc:mc + 1], scalar2=float(128 ** -0.5), op0=ALU.add, op1=ALU.mult)
                else:
                    S.op('act', 'activation', ['bfe'], [pk, ('mqk', mc)], out=mkT[:, mc - 4, :], in_=pt_[:, :],
                         func=AF.Identity, bias=bfe[:, mc:mc + 1])
            proj_feat(WIN, kin, C_MQ, 1024, ev_mq)

            def ev_mtok(j, off, n, pt_, pk):
                if off == 0:
                    S.op('dve', 'tensor_tensor', ['bb'], [pk, 'mk_tok'], out=mk_tok[:, j, :], in0=pt_[:, 0:512],
                         in1=bb[:, 0:512], op=ALU.add)
                elif off == 512:
                    S.op('dve', 'tensor_tensor', ['bb'], [pk, 'mv_aug'], out=mv_aug[:, j, :, 0:128],
                         in0=pt_[:, 0:512].rearrange("p (h e) -> p h e", h=4),
                         in1=bb[:, 512:1024].rearrange("p (h e) -> p h e", h=4), op=ALU.add)
                elif off == 1024:
                    S.op('dve', 'tensor_tensor', ['bb'], [pk, 'gs_ml'], out=gs_ml[:, j, :], in0=pt_[:, 0:512],
                         in1=bb[:, 1024:1536], op=ALU.add)
                    S.op('act', 'activation', ['gs_ml'], ['gs_ml'], out=gs_ml[:, j, :], in_=gs_ml[:, j, :], func=AF.Sigmoid)
                    S.op('pool', 'tensor_tensor', ['gs_ml', 'gbc'], ['gs_ml'], out=gs_ml[:, j, :], in0=gs_ml[:, j, :],
                         in1=gbc[:], op=ALU.mult)
                else:
                    S.op('dve', 'tensor_tensor', ['bb'], [pk, 'mif'], out=mif[:, j, :], in0=pt_[:, 0:8],
                         in1=bb[:, 1536:1544], op=ALU.add)
            proj_tok(WIN, kin, C_MK, 1544, ev_mtok)

            S.op('act', 'activation', ['mif'], ['sm_a'], out=sm[:, 0:16].rearrange("p (j h) -> p j h", h=4),
                 in_=mif[:, :, 4:8], func=AF.Exp, scale=-1.0)
            S.op('act', 'activation', ['sm_a'], ['sm_a'], out=sm[:, 0:16], in_=sm[:, 0:16], func=AF.Ln, bias=1.0)
            S.op('pe', 'matmul', ['sm_a', 'cst_f'], ['PA'], PA[:, 0:16], lhsT=tri_f, rhs=sm[:, 0:16], start=True, stop=True)
            S.op('pe', 'matmul', ['sm_a', 'cst_f'], ['PA'], PA[:, 16:32], lhsT=ones_f, rhs=sm[:, 0:16], start=True, stop=True)
            S.op('dve', 'tensor_copy', [], ['PA', 'sm_b'], out=sm[:, 16:48], in_=PA[:, 0:32])
            S.op('act', 'activation', ['sm_b'], ['sm_c'], out=sm[:, 48:80], in_=sm[:, 16:48], func=AF.Exp, scale=-1.0)
            S.op('dve', 'tensor_tensor', ['sm_b', 'mif'], ['sm_d'], out=sm[:, 96:112].rearrange("p (j h) -> p j h", h=4),
                 in0=sm[:, 16:32].rearrange("p (j h) -> p j h", h=4), in1=mif[:, :, 0:4], op=ALU.add)
            S.op('dve', 'tensor_tensor', ['sm_d', 'sm_b'], ['sm_e'], out=sm[:, 80:96], in0=sm[:, 96:112], in1=sm[:, 32:48],
                 op=ALU.subtract)
            S.op('act', 'activation', ['sm_e'], ['sm_e'], out=sm[:, 80:96], in_=sm[:, 80:96], func=AF.Exp)
            S.op('act', 'activation', ['sm_d'], ['sm_d'], out=sm[:, 96:112], in_=sm[:, 96:112], func=AF.Exp)
            if t + 1 < NT:
                norm_from_xt(g1, 'g1', (t + 1) % 2, (t + 1) % 2)
            PAR = [PJ[0], PJ[1]]
            PARK = [('PJ', 0), ('PJ', 1)]
            def ml_head(j):
                js = slice(j * 128, (j + 1) * 128)
                STb = STbs[j % 2]
                for h in range(4):
                    S.op('pe', 'matmul', [('mqk', h), ('mqk', 4 + h)], ['PA'], PA[:, h * 128:(h + 1) * 128],
                         lhsT=mkT[:, h, js], rhs=mqT[:, h, js], start=True, stop=True)
                for h in range(4):
                    col = 96 + j * 4 + h
                    S.op('dve', 'scalar_tensor_tensor', ['sm_d', 'mask4'], ['PA', ('STb', j % 2)], out=STb[:, h, :],
                         in0=PA[:, h * 128:(h + 1) * 128], scalar=sm[:, col:col + 1], in1=mask4[:, h, :],
                         op0=ALU.mult, op1=ALU.mult)

            def ml_chain(j):
                js = slice(j * 128, (j + 1) * 128)
                STb = STbs[j % 2]
                for h in range(4):
                    pk = ('PO', h // 2)
                    o_ap = PO[h // 2][:, (h % 2) * 130:(h % 2) * 130 + 130]
                    S.op('pe', 'matmul', [('STb', j % 2), 'mv_aug'], [pk], o_ap, lhsT=STb[:, h, :], rhs=mv_aug[:, j, h, :],
                         start=(h % 2 == 0), stop=False, skip_group_check=True)
                    S.op('pe', 'matmul', [('mqk', h), 'Cbf'], [pk], o_ap, lhsT=mqT[:, h, js], rhs=Cbf[:, h, :],
                         start=False, stop=True, skip_group_check=True)
                for h in range(4):
                    col = 80 + j * 4 + h
                    S.op('pool', 'tensor_scalar', ['mk_tok', 'sm_e'], ['kd'], out=kd[:, h, :],
                         in0=mk_tok[:, j, h * 128:(h + 1) * 128], scalar1=sm[:, col:col + 1], scalar2=None, op0=ALU.mult)
                for h in range(4):
                    S.op('pe', 'matmul', ['kd', 'mv_aug'], ['PS'], PS[:, 0:130], lhsT=kd[:, h, :], rhs=mv_aug[:, j, h, :],
                         start=True, stop=True)
                    col = 64 + j * 4 + h
                    S.op('dve', 'scalar_tensor_tensor', ['sm_c'], ['PS', 'Cst'], out=Cst[:, h, :], in0=Cst[:, h, :],
                         scalar=sm[:, col:col + 1], in1=PS[:, 0:130], op0=ALU.mult, op1=ALU.add)
                for hp in range(2):
                    S.op('act', 'activation', [], [('PO', hp), 'ytmp'], out=ytmp[:, 2 * hp:2 * hp + 2, :],
                         in_=PO[hp][:, 0:260].rearrange("p (h e) -> p h e", h=2), func=AF.Copy)
                S.op('pool', 'tensor_copy', ['Cst'], ['Cbf'], out=Cbf[:], in_=Cst[:])
                eb4 = sm[:, 48 + j * 4:52 + j * 4]
                S.op('dve', 'tensor_tensor', ['ytmp', 'sm_c'], ['sm_f'], out=sm[:, 112:116], in0=ytmp[:, :, 128], in1=eb4,
                     op=ALU.mult)
                S.op('act', 'activation', ['sm_f'], ['sm_f'], out=sm[:, 112:116], in_=sm[:, 112:116], func=AF.Abs)
                S.op('dve', 'tensor_scalar', ['sm_f'], ['sm_f'], out=sm[:, 112:116], in0=sm[:, 112:116], scalar1=1.0,
                     scalar2=None, op0=ALU.max)
                S.op('dve', 'reciprocal', ['sm_f'], ['sm_f'], out=sm[:, 112:116], in_=sm[:, 112:116])
                S.op('dve', 'tensor_tensor', ['sm_f', 'sm_c'], ['sm_f'], out=sm[:, 112:116], in0=sm[:, 112:116], in1=eb4,
                     op=ALU.mult)
                for h in range(4):
                    S.op('dve', 'tensor_scalar', ['sm_f', 'ytmp'], ['ytmp'], out=ytmp[:, h, 0:128], in0=ytmp[:, h, 0:128],
                         scalar1=sm[:, 112 + h:113 + h], scalar2=None, op0=ALU.mult)
                for h in range(4):
                    S.op('act', 'activation', ['ytmp'], ['junk', ('sm_g', h)], out=junk[:, 0:128], in_=ytmp[:, h, 0:128],
                         func=AF.Square, accum_out=sm[:, 116 + h:117 + h])
                S.op('act', 'activation', [('sm_g', h) for h in range(4)], ['sm_h'], out=sm[:, 120:124], in_=sm[:, 116:120],
                     func=AF.Sqrt, scale=1.0 / 128, bias=EPS)
                S.op('dve', 'reciprocal', ['sm_h'], ['sm_h'], out=sm[:, 120:124], in_=sm[:, 120:124])
                for h in range(4):
                    S.op('dve', 'scalar_tensor_tensor', ['sm_h', 'ytmp', 'gs_ml'], ['ytok'], out=ytok[:, h * 128:(h + 1) * 128],
                         in0=ytmp[:, h, 0:128], scalar=sm[:, 120 + h:121 + h], in1=gs_ml[:, j, h * 128:(h + 1) * 128],
                         op0=ALU.mult, op1=ALU.mult)
                tok_to_yTs(j)
            def gla_chain(j):
                js = slice(j * 128, (j + 1) * 128)
                for h in range(4):
                    c, par = h // 2, h % 2
                    po_ = par * 64
                    S.op('pe', 'matmul', ['ktT', 'qtT'], [PARK[par]], PAR[par][:, c * 128:(c + 1) * 128],
                         lhsT=ktT[po_:po_ + 64, c, js], rhs=qtT[po_:po_ + 64, c, js], start=True, stop=True)
                for par in range(2):
                    S.op('dve', 'tensor_tensor', ['mask4'], [PARK[par], 'Ab'], out=Ab[:, 2 * par:2 * par + 2, :],
                         in0=PAR[par][:, 0:256].rearrange("p (h s) -> p h s", h=2), in1=mask4[:, 0:2, :], op=ALU.mult)
                for h in range(4):
                    c, par = h // 2, h % 2
                    po_ = par * 64
                    o_ap = PJ[par][:, c * 128:(c + 1) * 128]
                    S.op('pe', 'matmul', ['Ab', 'gv_t'], [('PJ', par)], o_ap, lhsT=Ab[:, 2 * par + c, :],
                         rhs=gv_t[:, j, h * 128:(h + 1) * 128], start=(c == 0), stop=False, skip_group_check=True)
                    S.op('pe', 'matmul', ['qtT', 'Sbf'], [('PJ', par)], o_ap, lhsT=qtT[po_:po_ + 64, c, js],
                         rhs=Sbf[po_:po_ + 64, c, :], start=False, stop=True, skip_group_check=True)
                for c in range(2):
                    S.op('pe', 'transpose', ['ktT', 'ident_b'], ['PB'], out=PB[:, c * 128:(c + 1) * 128], in_=ktT[:, c, js],
                         identity=ident_b[:])
                S.op('act', 'activation', [], ['PB', 'kt_tok'], out=kt_tok, in_=PB[:, 0:256], func=AF.Copy)
                for h in range(4):
                    c, po_ = h // 2, (h % 2) * 64
                    S.op('pe', 'matmul', ['kt_tok', 'gv_t'], ['PT'], PT[po_:po_ + 64, c * 128:(c + 1) * 128],
                         lhsT=kt_tok[:, h * 64:(h + 1) * 64], rhs=gv_t[:, j, h * 128:(h + 1) * 128], start=(h < 2), stop=True,
                         skip_group_check=True)
                for c in range(2):
                    S.op('dve', 'tensor_tensor', [], ['PT', 'Sst'], out=Sst[:, c, :], in0=PT[:, c * 128:(c + 1) * 128],
                         in1=Sst[:, c, :], op=ALU.add)
                    S.op('dve', 'tensor_scalar', ['geq'], ['Sst'], out=Sst[:, c, :], in0=Sst[:, c, :],
                         scalar1=geq[:, c, j * 128 + 127:j * 128 + 128], scalar2=None, op0=ALU.mult)
                for h in range(4):
                    c, par = h // 2, h % 2
                    S.op('act', 'activation', [], [('PJ', par), 'junk2', ('sm_l', h)], out=junk2[:, 0:128],
                         in_=PJ[par][:, c * 128:(c + 1) * 128], func=AF.Square, accum_out=sm[:, 140 + h:141 + h])
                S.op('pool', 'tensor_copy', ['Sst'], ['Sbf'], out=Sbf[:], in_=Sst[:])
                S.op('act', 'activation', [('sm_l', h) for h in range(4)], ['sm_m'], out=sm[:, 144:148], in_=sm[:, 140:144],
                     func=AF.Sqrt, scale=1.0 / 128, bias=EPS)
                S.op('dve', 'reciprocal', ['sm_m'], ['sm_m'], out=sm[:, 144:148], in_=sm[:, 144:148])
                for h in range(4):
                    c, par = h // 2, h % 2
                    S.op('dve', 'scalar_tensor_tensor', ['sm_m', 'gsil'], [('PJ', par), 'ytok2'], out=ytok2[:, h * 128:(h + 1) * 128],
                         in0=PJ[par][:, c * 128:(c + 1) * 128], scalar=sm[:, 144 + h:145 + h], in1=gsil[:, j, h * 128:(h + 1) * 128],
                         op0=ALU.mult, op1=ALU.mult)
                tok_to_yTs(j, alt=True)
            ml_head(0)
            for j in range(4):
                if j + 1 < 4:
                    ml_head(j + 1)
                ml_chain(j)
                gla_chain(j)
            yT_store(0, t)
            yT_store(3, t, alt=True)

            S.barrier()
            fqn = vB(0, [128, 512])
            fT = vB(512, [128, 4, 512])
            fv_aug = vB(2560, [128, 4, 4, 130])
            fq_t = vF(0, [128, 512])
            S.op('pool', 'memset', [], ['fv_aug'], fv_aug, 1.0)

            def ev_ftok(j, off, n, pt_, pk):
                if off < 1024:
                    w = off // 512
                    S.op('dve', 'tensor_tensor', ['bb'], [pk, 'fq_t'], out=fq_t, in0=pt_[:, 0:512],
                         in1=bb[:, 1544 + off:1544 + off + 512], op=ALU.add)
                    for h in range(4):
                        S.op('act', 'activation', ['fq_t'], ['junk', ('sm_i', h)], out=junk[:, 0:128],
                             in_=fq_t[:, h * 128:(h + 1) * 128], func=AF.Square, accum_out=sm[:, 128 + h:129 + h])
                    S.op('act', 'activation', [('sm_i', h) for h in range(4)], ['sm_j'], out=sm[:, 132:136],
                         in_=sm[:, 128:132], func=AF.Sqrt, scale=1.0 / 128, bias=EPS)
                    S.op('dve', 'reciprocal', ['sm_j'], ['sm_j'], out=sm[:, 132:136], in_=sm[:, 132:136])
                    for h in range(4):
                        S.op('dve', 'scalar_tensor_tensor', ['sm_j', 'fq_t', 'gqk'], ['fqn'], out=fqn[:, h * 128:(h + 1) * 128],
                             in0=fq_t[:, h * 128:(h + 1) * 128], scalar=sm[:, 132 + h:133 + h], in1=gqk[:, w, :],
                             op0=ALU.mult, op1=ALU.mult)
                    for h in range(4):
                        S.op('pe', 'transpose', ['fqn', 'ident_b'], ['PB'], out=PB[:, h * 128:(h + 1) * 128],
                             in_=fqn[:, h * 128:(h + 1) * 128], identity=ident_b[:])
                    S.op('act', 'activation', [], ['PB', 'fT'], out=fT[:, :, j * 128:(j + 1) * 128],
                         in_=PB[:, 0:512].rearrange("p (c s) -> p c s", c=4), func=AF.Copy)
                    if j == 3:
                        dst = QTd if w == 0 else KTd
                        S.dma('pool', d_f, dst[:, :, t * 512:(t + 1) * 512].rearrange("h p s -> p h s"), fT,
                              R=['fT'], W=[('QK', w)])
                elif off == 1024:
                    S.op('dve', 'tensor_tensor', ['bb'], [pk, 'fv_aug'], out=fv_aug[:, :, j, 0:128],
                         in0=pt_[:, 0:512].rearrange("p (h e) -> p h e", h=4),
                         in1=bb[:, 1544 + 1024:1544 + 1536].rearrange("p (h e) -> p h e", h=4), op=ALU.add)
                    if j == 3:
                        S.dma('pool', d_f2, Vd[:, :, t * 4:(t + 1) * 4, :].rearrange("h p j e -> p h j e"), fv_aug,
                              R=['fv_aug'], W=['Vd'])
                else:
                    S.op('dve', 'tensor_tensor', ['bb'], [pk, 'sm_k'], out=sm[:, 136:140], in0=pt_[:, 0:4],
                         in1=bb[:, 1544 + 1536:1544 + 1540], op=ALU.add)
                    S.op('act', 'activation', ['sm_k'], ['sm_k'], out=sm[:, 136:140], in_=sm[:, 136:140], func=AF.Exp, scale=-1.0)
                    S.op('act', 'activation', ['sm_k'], ['sm_k'], out=sm[:, 136:140], in_=sm[:, 136:140], func=AF.Ln, bias=1.0)
                    S.op('pe', 'matmul', ['sm_k', 'cst_f'], ['PA'], PA[:, 0:4], lhsT=tri_f, rhs=sm[:, 136:140], start=True, stop=True)
                    S.op('pe', 'matmul', ['sm_k', 'cst_f'], ['PA'], PA[:, 4:8], lhsT=ones_f, rhs=sm[:, 136:140], start=True, stop=True)
                    if j == 0:
                        S.op('dve', 'tensor_scalar', ['Fcar'], ['rI'], out=rI[:, t, :], in0=Fcar[:], scalar1=-1.0, scalar2=None,
                             op0=ALU.mult)
                    S.op('dve', 'tensor_tensor', ['Fcar'], ['PA', 'Fall'], out=Fall[:, t * 4 + j, :], in0=PA[:, 0:4], in1=Fcar[:],
                         op=ALU.add)
                    S.op('dve', 'tensor_tensor', [], ['PA', 'Fcar'], out=Fcar[:], in0=PA[:, 4:8], in1=Fcar[:], op=ALU.add)
            proj_tok(WIN, kin, C_FQ, 1540, ev_ftok)

            S.barrier()
            ryT = vF(0, [128, 4, 512])
            xc = vF(2048, [128, 4, 512])
            lr = vF(4096, [128, 512])
            li = vF(4608, [128, 512])
            la_ = vF(5120, [128, 512])
            lu = vF(5632, [128, 512])
            lh = vF(6144, [128, 512])
            xcb = vB(0, [128, 4, 512])

            def ev_rx(mc, m, pt_, pk):
                if mc < 4:
                    S.op('act', 'activation', ['bfe'], [pk, 'rxy'], out=rxy[:, mc, 3:515], in_=pt_[:, :], func=AF.Identity,
                         bias=bfe[:, 8 + mc:9 + mc])
                else:
                    S.op('act', 'activation', ['bfe'], [pk, 'ryT'], out=ryT[:, mc - 4, :], in_=pt_[:, :], func=AF.Identity,
                         bias=bfe[:, 8 + mc:9 + mc])
            proj_feat(WIN, kin, C_RX, 1024, ev_rx)
            for c in range(4):
                S.op('dve', 'tensor_scalar', ['rxy'] + LP, ['xc'], out=xc[:, c, :], in0=rxy[:, c, 0:512],
                     scalar1=lpar[:, c:c + 1], scalar2=lpar[:, 16 + c:17 + c], op0=ALU.mult, op1=ALU.add)
                for k in range(1, 4):
                    S.op('dve', 'scalar_tensor_tensor', ['rxy'] + LP, ['xc'], out=xc[:, c, :], in0=rxy[:, c, k:k + 512],
                         scalar=lpar[:, 4 * k + c:4 * k + c + 1], in1=xc[:, c, :], op0=ALU.mult, op1=ALU.add)
            S.op('pool', 'tensor_copy', ['xc'], ['xcb'], out=xcb, in_=xc)
            S.op('pool', 'tensor_copy', [], ['rxy'], out=rxy[:, :, 0:3], in_=rxy[:, :, 512:515])
            for c in range(4):
                pj = next_pj()
                S.op('pe', 'matmul', ['wbd', 'xcb'], [('PJ', pj)], PJ[pj][:, :], lhsT=wbd[:, 0, c, :], rhs=xcb[:, c, :],
                     start=True, stop=True)
                S.op('act', 'activation', LP, [('PJ', pj), 'lr'], out=lr, in_=PJ[pj][:, :], func=AF.Sigmoid,
                     bias=lpar[:, 20 + c:21 + c])
                pj = next_pj()
                S.op('pe', 'matmul', ['wbd', 'xcb'], [('PJ', pj)], PJ[pj][:, :], lhsT=wbd[:, 1, c, :], rhs=xcb[:, c, :],
                     start=True, stop=True)
                S.op('act', 'activation', LP, [('PJ', pj), 'li'], out=li, in_=PJ[pj][:, :], func=AF.Sigmoid,
                     bias=lpar[:, 24 + c:25 + c])
                S.op('act', 'activation', ['lr'] + LP, ['la_'], out=la_, in_=lr, func=AF.Exp, scale=lpar[:, 32 + c:33 + c])
                S.op('act', 'activation', ['lr'] + LP, ['lu'], out=lu, in_=lr, func=AF.Exp, scale=lpar[:, 36 + c:37 + c])
                S.op('dve', 'tensor_scalar', ['lu'], ['lu'], out=lu, in0=lu, scalar1=-1.0, scalar2=1.0, op0=ALU.mult, op1=ALU.add)
                S.op('dve', 'tensor_scalar', ['lu'], ['lu'], out=lu, in0=lu, scalar1=0.0, scalar2=None, op0=ALU.max)
                S.op('act', 'activation', ['lu'], ['lu'], out=lu, in_=lu, func=AF.Sqrt)
                S.op('dve', 'tensor_tensor', ['li', 'xc'], ['li'], out=li, in0=li, in1=xc[:, c, :], op=ALU.mult)
                S.op('dve', 'tensor_tensor', ['li', 'lu'], ['lu'], out=lu, in0=lu, in1=li, op=ALU.mult)
                S.op('dve', 'tensor_tensor_scan', ['la_', 'lu', 'lcar'], ['lh'], out=lh, data0=la_, data1=lu,
                     initial=lcar[:, c:c + 1], op0=ALU.mult, op1=ALU.add)
                S.op('pool', 'tensor_copy', ['lh'], ['lcar'], out=lcar[:, c:c + 1], in_=lh[:, 511:512])
                yv = ryT[:, c, :]
                S.op('pool', 'tensor_tensor', ['ryT'], ['li'], out=li, in0=yv, in1=yv, op=ALU.mult)
                S.op('dve', 'tensor_scalar', ['li'], ['li'], out=li, in0=li, scalar1=0.044715, scalar2=1.0, op0=ALU.mult,
                     op1=ALU.add)
                S.op('dve', 'tensor_tensor', ['li', 'ryT'], ['li'], out=li, in0=li, in1=yv, op=ALU.mult)
                S.op('act', 'activation', ['li'], ['li'], out=li, in_=li, func=AF.Sigmoid, scale=1.5957691216)
                S.op('dve', 'tensor_tensor', ['li', 'ryT'], ['li'], out=li, in0=li, in1=yv, op=ALU.mult)
                S.op('dve', 'tensor_tensor', ['li', 'lh'], ['yTs'], out=yTs[:, c, :], in0=li, in1=lh, op=ALU.mult)
            yT_store(2, t)


        S.barrier()
        KTs = vB(0, [128, S_])
        Vs = vB(S_, [128, NB, 130])
        o0 = S_ + NB * 130
        QTb = [vB(o0, [128, 512]), vB(o0 + 2048, [128, 512])]
        Pb = vB(o0 + 512, [128, 2, 512])
        ob = vB(o0 + 1536, [128, 512])
        d_q2 = [d_p, d_g2]
        qcnt = 0
        biasI = vF(0, [128, NB])
        rd = vF(NB, [128, 4])
        d_k = d_f
        d_v = d_f2
        d_q = d_p
        OB = [PO[0], PO[1], PA, PS]
        OK = [('PO', 0), ('PO', 1), 'PA', 'PS']
        pcnt = 0
        for h in range(4):
            S.dma('sp', d_k, KTs, KTd[h], R=[('QK', 1)], W=['KTs'])
            S.dma('sp', d_v, Vs, Vd[h], R=['Vd'], W=['Vs'])
            for I in range(NT):
                qi = qcnt % 2
                qcnt += 1
                QTs = QTb[qi]
                S.dma('sp', d_q2[qi], QTs, QTd[h, :, I * 512:(I + 1) * 512], R=[('QK', 0)], W=[('QTs', qi)])
                nj = 4 * I + 4
                S.op('dve', 'tensor_scalar', ['Fall', 'rI'], ['biasI'], out=biasI[:, 0:nj], in0=Fall[:, 0:nj, h],
                     scalar1=rI[:, I, h:h + 1], scalar2=None, op0=ALU.add)
                def emit_st(j, pj):
                    jj = j - 4 * I
                    q0 = 0 if jj <= 0 else jj * 128
                    nq = 512 - q0
                    S.op('pe', 'matmul', ['KTs', ('QTs', qi)], [('PJ', pj)], PJ[pj][:, 0:nq], lhsT=KTs[:, j * 128:(j + 1) * 128],
                         rhs=QTs[:, q0:512], start=True, stop=True)
                    S.op('act', 'activation', ['biasI'], [('PJ', pj), ('Pb', pj)], out=Pb[:, pj, 0:nq], in_=PJ[pj][:, 0:nq],
                         func=AF.Exp, bias=biasI[:, j:j + 1])
                    if jj >= 0:
                        S.op('pool', 'tensor_tensor', ['mask4'], [('Pb', pj)], out=Pb[:, pj, 0:128], in0=Pb[:, pj, 0:128],
                             in1=mask4[:, 0, :], op=ALU.mult)

                def emit_pv(j, pj):
                    jj = j - 4 * I
                    q0 = 0 if jj <= 0 else jj * 128
                    for qs in range(q0 // 128, 4):
                        last = (j == 4 * I + qs)
                        S.op('pe', 'matmul', [('Pb', pj), 'Vs'], [OK[qs]], OB[qs][:, 0:130],
                             lhsT=Pb[:, pj, qs * 128 - q0:(qs + 1) * 128 - q0], rhs=Vs[:, j, :], start=(j == 0), stop=last)

                emit_st(0, pcnt % 2)
                for j in range(nj):
                    if j + 1 < nj:
                        emit_st(j + 1, (pcnt + 1) % 2)
                    emit_pv(j, pcnt % 2)
                    pcnt += 1
                for qs in range(4):
                    S.op('dve', 'reciprocal', [], [OK[qs], ('rd', qs)], out=rd[:, qs:qs + 1], in_=OB[qs][:, 128:129])
                    S.op('dve', 'tensor_scalar', [('rd', qs)], [OK[qs], 'ob'], out=ob[:, qs * 128:(qs + 1) * 128],
                         in0=OB[qs][:, 0:128], scalar1=rd[:, qs:qs + 1], scalar2=None, op0=ALU.mult)
                for qs in range(4):
                    S.op('pe', 'transpose', ['ob', 'ident_b'], ['PB'], out=PB[:, qs * 128:(qs + 1) * 128],
                         in_=ob[:, qs * 128:(qs + 1) * 128], identity=ident_b[:])
                S.op('act', 'activation', [], ['PB', 'yTs'], out=yTs[:, 0, :], in_=PB[:, 0:512], func=AF.Copy)
                S.dma('pool', d_y, yT[4 + h, :, I * 512:(I + 1) * 512], yTs[:, 0, :], R=['yTs'], W=[('yT', 1)])

        S.barrier()
        ybr = vB(0, [128, 16, 512])
        mgb = vB(8192, [128, 8, 512])
        aT = vB(12288, [128, 16, 512])
        mg = vF(0, [128, 8, 512])
        sg = vF(4096, [128, 512])
        rl = vF(4608, [128, 512])
        d_yb = d_f
        d_o = d_f2
        accs = [PO[0], PO[1], PA, PS]
        acck = [('PO', 0), ('PO', 1), 'PA', 'PS']
        load_x(x_src, 0, 0)
        for t in range(NT):
            xi = t % 2
            xt = xts[xi]
            xk = ('xt', xi)
            cur['hi'] = 0
            hT = hTs[0]
            norm_from_xt(g1, 'g1', xi, 0)
            if t + 1 < NT:
                load_x(x_src, t + 1, (t + 1) % 2)
            S.dma('sp', d_yb, ybr, yT[:, :, t * 512:(t + 1) * 512].rearrange("c p s -> p c s"),
                  R=[('yT', b) for b in range(4)], W=['ybr'])
            for n in range(4):
                for half in range(2):
                    i = load_w(wb_br[l][n * 512:(n + 1) * 512, half * 512:(half + 1) * 512], 512, ('wb_br', l), kch=4)
                    gi = load_w(WIN[:, C_GT + n * 1024 + half * 512:C_GT + n * 1024 + half * 512 + 512], 512, kin)
                    for m in range(4):
                        dc = half * 4 + m
                        pj = next_pj()
                        for kc in range(8):
                            S.op('pe', 'matmul', [('wsl', gi), ('hT', 0, kc)], [('PJ', pj)], PJ[pj][:, :],
                                 lhsT=wsl[gi][:, kc, m * 128:(m + 1) * 128], rhs=hTs[0][:, kc, :], start=(kc == 0), stop=(kc == 7))
                        S.op('act', 'activation', ['bfe'], [('PJ', pj), 'sg'], out=sg, in_=PJ[pj][:, :], func=AF.Sigmoid,
                             bias=bfe[:, 21 + n * 8 + dc:22 + n * 8 + dc])
                        pj2 = next_pj()
                        for kc in range(4):
                            S.op('pe', 'matmul', [('wsl', i), 'ybr'], [('PJ', pj2)], PJ[pj2][:, :],
                                 lhsT=wsl[i][:, kc, m * 128:(m + 1) * 128], rhs=ybr[:, n * 4 + kc, :], start=(kc == 0),
                                 stop=(kc == 3))
                        if n == 0:
                            S.op('dve', 'tensor_tensor', ['sg'], [('PJ', pj2), ('mg', dc)], out=mg[:, dc, :], in0=PJ[pj2][:, :],
                                 in1=sg, op=ALU.mult)
                        else:
                            S.op('dve', 'tensor_tensor', [], [('PJ', pj2), 'sg'], out=sg, in0=PJ[pj2][:, :], in1=sg,
                                 op=ALU.mult)
                            S.op('pool', 'tensor_tensor', ['sg'], [('mg', dc)], out=mg[:, dc, :], in0=mg[:, dc, :], in1=sg,
                                 op=ALU.add)
            for dc in range(8):
                S.op('pool', 'tensor_copy', [('mg', dc)], ['mgb'], out=mgb[:, dc, :], in_=mg[:, dc, :])
            for half in range(2):
                wi = load_w(wb_o[l][:, half * 512:(half + 1) * 512], 512, ('wb_o', l))
                for j in range(4):
                    pj = next_pj()
                    for kc in range(8):
                        S.op('pe', 'matmul', [('wsl', wi), 'mgb'], [('PJ', pj)], PJ[pj][:, :],
                             lhsT=mgb[:, kc, j * 128:(j + 1) * 128], rhs=wsl[wi][:, kc, :], start=(kc == 0), stop=(kc == 7))
                    S.op('dve', 'tensor_tensor', [], [('PJ', pj), xk], out=xt[:, j, half * 512:(half + 1) * 512],
                         in0=PJ[pj][:, :], in1=xt[:, j, half * 512:(half + 1) * 512], op=ALU.add)
            norm_from_xt(g2, 'g2', xi, 1)
            for fh in range(2):
                for grp in range(4):
                    wi = load_w(wb_up[l][:, fh * 2048 + grp * 512:fh * 2048 + (grp + 1) * 512], 512, ('wb_up', l))
                    for m in range(4):
                        fc = grp * 4 + m
                        pj = next_pj()
                        for kc in range(8):
                            S.op('pe', 'matmul', [('wsl', wi), ('hT', 1, kc)], [('PJ', pj)], PJ[pj][:, :],
                                 lhsT=wsl[wi][:, kc, m * 128:(m + 1) * 128], rhs=hTs[1][:, kc, :], start=(kc == 0), stop=(kc == 7))
                        S.op('act', 'activation', [], [('PJ', pj), 'rl'], out=rl, in_=PJ[pj][:, :], func=AF.Relu)
                        S.op('pool', 'tensor_tensor', ['rl'], [('aT', fc)], out=aT[:, fc, :], in0=rl, in1=rl, op=ALU.mult)
                for half in range(2):
                    for grp in range(2):
                        r0 = fh * 2048 + grp * 1024
                        wi = load_w(wb_dn[l][r0:r0 + 1024, half * 512:(half + 1) * 512], 512, ('wb_dn', l))
                        for j in range(4):
                            for kc in range(8):
                                fc = grp * 8 + kc
                                S.op('pe', 'matmul', [('wsl', wi), ('aT', fc)], [acck[j]], accs[j][:, :],
                                     lhsT=aT[:, fc, j * 128:(j + 1) * 128], rhs=wsl[wi][:, kc, :], start=(fc == 0), stop=(fc == 15))
                    for j in range(4):
                        S.op('dve', 'tensor_tensor', [], [acck[j], xk], out=xt[:, j, half * 512:(half + 1) * 512],
                             in0=accs[j][:, :], in1=xt[:, j, half * 512:(half + 1) * 512], op=ALU.add)
            S.dma('pool', d_o, x_dst[t * 512:(t + 1) * 512, :].rearrange("(j p) d -> p j d", p=128), xt[:],
                  R=[xk], W=[('xs', id(x_dst))])

    S.barrier()
    S.emit()
    print("sbuf remaining", nc.sbuf_bytes_remaining, flush=True)
    print("instr counts", S.ecnt, "waits", S.nwaits, "dsems", S.ndsem, flush=True)
    return nc, es


def make_consts():
    c = np.zeros((128, 384), np.float32)
    c[:, 0:128] = np.eye(128, dtype=np.float32)
    c[:, 128:256] = np.triu(np.ones((128, 128), np.float32))
    c[:, 256:384] = 1.0
    return c


_CACHE = {}


def run(inputs, dbg=False):
    x = np.ascontiguousarray(inputs["x"], dtype=np.float32)
    B, S_, _ = x.shape
    NL = inputs["w_in"].shape[0]
    key = (S_, NL, dbg)
    if key not in _CACHE:
        _CACHE[key] = build(S_, NL, dbg)
    nc, _es = _CACHE[key]
    cst = make_consts()
    in_maps = []
    for b in range(B):
        m = {"x": x[b], "cst": cst}
        for k in PNAMES:
            m[k] = np.ascontiguousarray(inputs[k], dtype=np.float32)
        in_maps.append(m)
    res = run_bass_kernel_spmd(nc, in_maps, core_ids=list(range(B)))
    return res


def kernel(**inputs):
    res = run(inputs)
    return np.stack([r["out"] for r in res.results], axis=0).astype(np.float32)
```
